# Optimizing a Trainium2 kernel written in Bass

```python
import math
import jax
import jax.numpy as jnp
from jax import lax
import numpy as np

D_MODEL = 2048
BATCH = 4
SEQ = 2048
DEPTH = 2

GRID_W = 64
CTX_LEN = 256
ROPE_BASE = 10000.0
EPS = 1e-6
Q_BLOCK = 128
F32 = jnp.float32

N_BRANCH = 3
BRANCH_W = 1024

M_HEADS = 4
M_DH = BRANCH_W // M_HEADS
M_CHUNK = 64
M_CONV = 5

DA_HEADS = 4
DA_DV = BRANCH_W // DA_HEADS
DA_DH = DA_DV // 2
DA_QK_W = DA_HEADS * 2 * DA_DH

MLA_HEADS = 8
MLA_Q_RANK = 512
MLA_KV_RANK = 256
MLA_NOPE = 128
MLA_ROPE = 64
MLA_DV = BRANCH_W // MLA_HEADS
MLA_QK = MLA_NOPE + MLA_ROPE

N_EXPERTS = 16
N_GROUPS = 4
GROUP_SIZE = N_EXPERTS // N_GROUPS
GROUP_SCORE_K = 2
TOP_K = 2
D_EXPERT = 512

OFF_MQK = 0
OFF_MV = OFF_MQK + 2 * BRANCH_W
OFF_MO = OFF_MV + BRANCH_W
OFF_MG = OFF_MO + BRANCH_W
OFF_DQ = OFF_MG + 4 * M_HEADS
OFF_DK = OFF_DQ + DA_QK_W
OFF_DV = OFF_DK + DA_QK_W
OFF_CQ = OFF_DV + BRANCH_W
OFF_CKV = OFF_CQ + MLA_Q_RANK
OFF_KR = OFF_CKV + MLA_KV_RANK
OFF_G = OFF_KR + MLA_ROPE
D_IN = OFF_G + N_BRANCH * D_MODEL

kernel_name = 'hybrid_mlstm_diffattn_mla_moe_dit'


def rms_norm(x, g=None):
    xf = x.astype(F32)
    y = (xf * lax.rsqrt(jnp.mean(xf * xf, axis=-1, keepdims=True) + EPS)).astype(x.dtype)
    return y if g is None else y * g


def modulate(x, shift, scale):
    return rms_norm(x) * (1 + scale) + shift


def split_heads(a, n_heads):
    b, t, _ = a.shape
    return a.reshape(b, t, n_heads, -1).transpose(0, 2, 1, 3)


def merge_heads(a):
    b, h, t, d = a.shape
    return a.transpose(0, 2, 1, 3).reshape(b, t, h * d)


def axial_angles(n, rot_dim):
    rows = n // GRID_W
    r = jnp.repeat(jnp.arange(rows, dtype=F32), GRID_W)
    col = jnp.tile(jnp.arange(GRID_W, dtype=F32), rows)
    n_freq = rot_dim // 4
    inv = ROPE_BASE ** (-jnp.arange(n_freq, dtype=F32) / n_freq)
    return jnp.concatenate([r[:, None] * inv, col[:, None] * inv], axis=-1)


def rope(x, ang):
    half = x.shape[-1] // 2
    cos = jnp.cos(ang).astype(x.dtype)
    sin = jnp.sin(ang).astype(x.dtype)
    x1, x2 = x[..., :half], x[..., half:]
    return jnp.concatenate([x1 * cos - x2 * sin, x1 * sin + x2 * cos], axis=-1)


def softmax32(s):
    return jax.nn.softmax(s.astype(F32), axis=-1)


def sweep_query_blocks(block_fn, qs):
    b, h, t, _ = qs[0].shape
    nb = t // Q_BLOCK
    blocks = tuple(jnp.moveaxis(q.reshape(b, h, nb, Q_BLOCK, q.shape[-1]), 2, 0) for q in qs)
    out = lax.map(block_fn, blocks)
    return jnp.moveaxis(out, 0, 2).reshape(b, h, t, out.shape[-1])


def dwconv_centered(x, w, bias):
    k = w.shape[0]
    y = lax.conv_general_dilated(x, w[:, None, :].astype(x.dtype), window_strides=(1,),
                                 padding=[(k // 2, k // 2)], dimension_numbers=('NWC', 'WIO', 'NWC'),
                                 feature_group_count=x.shape[-1])
    return y + bias.astype(x.dtype)


def mlstm_chunkwise(q, k, v, ig, lf, state):
    b, h, t, dh = q.shape
    nc = t // M_CHUNK

    def to_chunks(a):
        return jnp.moveaxis(a.reshape(b, h, nc, M_CHUNK, *a.shape[3:]), 2, 0)

    xs = tuple(to_chunks(a) for a in (q.astype(F32) * dh ** -0.5, k.astype(F32), v.astype(F32), ig, lf))
    causal = jnp.tril(jnp.ones((M_CHUNK, M_CHUNK), dtype=bool))

    def step(carry, chunk):
        c_mat, n_vec, m = carry
        qc, kc, vc, ic, fc = chunk
        bcum = jnp.cumsum(fc, axis=-1)
        logw = jnp.where(causal, bcum[..., :, None] - bcum[..., None, :] + ic[..., None, :], -jnp.inf)
        inter = bcum + m[..., None]
        m_t = jnp.maximum(inter, logw.max(-1))
        w = jnp.exp(logw - m_t[..., None]) * jnp.einsum('bhtd,bhsd->bhts', qc, kc)
        decay = jnp.exp(inter - m_t)
        num = jnp.einsum('bhts,bhsd->bhtd', w, vc) + decay[..., None] * jnp.einsum('bhvk,bhtk->bhtv', c_mat, qc)
        den = w.sum(-1) + decay * jnp.einsum('bhk,bhtk->bht', n_vec, qc)
        h_out = num / jnp.maximum(jnp.abs(den), jnp.exp(-m_t))[..., None]
        b_last = bcum[..., -1]
        src = b_last[..., None] - bcum + ic
        m_new = jnp.maximum(b_last + m, src.max(-1))
        g = jnp.exp(src - m_new[..., None])
        keep = jnp.exp(b_last + m - m_new)
        c_mat = keep[..., None, None] * c_mat + jnp.einsum('bhsv,bhsk->bhvk', g[..., None] * vc, kc)
        n_vec = keep[..., None] * n_vec + jnp.einsum('bhs,bhsk->bhk', g, kc)
        return (c_mat, n_vec, m_new), h_out

    state, hs = lax.scan(step, state, xs)
    return jnp.moveaxis(hs, 0, 2).reshape(b, h, t, dh), state


def mlstm_inputs(p, conv_w, conv_b):
    b, t, _ = p.shape
    qk = jax.nn.silu(dwconv_centered(p[..., OFF_MQK:OFF_MQK + 2 * BRANCH_W], conv_w, conv_b))
    q = split_heads(qk[..., :BRANCH_W], M_HEADS)
    k = split_heads(qk[..., BRANCH_W:], M_HEADS)
    v = split_heads(p[..., OFF_MV:OFF_MV + BRANCH_W], M_HEADS)
    o = jax.nn.sigmoid(p[..., OFF_MO:OFF_MO + BRANCH_W])
    gates = p[..., OFF_MG:OFF_MG + 4 * M_HEADS].astype(F32).reshape(b, t, 4, M_HEADS).transpose(2, 0, 3, 1)
    return q, k, v, o, gates


def mlstm_bidirectional(q, k, v, gates, state_f, state_b):
    h_f, state_f = mlstm_chunkwise(q, k, v, gates[0], jax.nn.log_sigmoid(gates[1]), state_f)
    flip = lambda a: jnp.flip(a, axis=2)
    h_b, state_b = mlstm_chunkwise(flip(q), flip(k), flip(v), flip(gates[2]),
                                   jax.nn.log_sigmoid(flip(gates[3])), state_b)
    return h_f + flip(h_b), state_f, state_b


def mlstm_output(h, o, norm_g):
    hn = rms_norm(h, norm_g.reshape(M_HEADS, 1, M_DH).astype(F32))
    return merge_heads(hn).astype(o.dtype) * o


def diff_qkv(p, q_g, k_g, ang):
    b, t, _ = p.shape
    halves = lambda a: a.reshape(b, t, DA_HEADS, 2, DA_DH).transpose(3, 0, 2, 1, 4)
    q = rms_norm(halves(p[..., OFF_DQ:OFF_DQ + DA_QK_W]), q_g)
    k = rms_norm(halves(p[..., OFF_DK:OFF_DK + DA_QK_W]), k_g)
    if ang is not None:
        q = rope(q, ang)
        k = rope(k, ang)
    v = split_heads(p[..., OFF_DV:OFF_DV + BRANCH_W], DA_HEADS)
    return q, k, v


def diff_attention(q, k, v, lam):
    scale = DA_DH ** -0.5

    def block(qb):
        q1b, q2b = qb
        p1 = softmax32(jnp.einsum('bhqd,bhkd->bhqk', q1b, k[0]) * scale)
        p2 = softmax32(jnp.einsum('bhqd,bhkd->bhqk', q2b, k[1]) * scale)
        return jnp.einsum('bhqk,bhkd->bhqd', (p1 - lam * p2).astype(v.dtype), v)

    return sweep_query_blocks(block, (q[0], q[1]))


def diff_output(o, subln_g, lam_init):
    return merge_heads(rms_norm(o, subln_g) * (1.0 - lam_init))


def mla_qkv(p, cq_g, ckv_g, w_uq, w_ukv, q_g, k_g, ang):
    cq = rms_norm(p[..., OFF_CQ:OFF_CQ + MLA_Q_RANK], cq_g)
    ckv = rms_norm(p[..., OFF_CKV:OFF_CKV + MLA_KV_RANK], ckv_g)
    q = split_heads(cq @ w_uq, MLA_HEADS)
    kv = split_heads(ckv @ w_ukv, MLA_HEADS)
    k_rope = p[..., OFF_KR:OFF_KR + MLA_ROPE][:, None]
    q_nope = rms_norm(q[..., :MLA_NOPE], q_g[:MLA_NOPE])
    q_rope = rms_norm(q[..., MLA_NOPE:], q_g[MLA_NOPE:])
    k_nope = rms_norm(kv[..., :MLA_NOPE], k_g[:MLA_NOPE])
    k_rope = rms_norm(k_rope, k_g[MLA_NOPE:])
    v = kv[..., MLA_NOPE:]
    if ang is not None:
        q_rope = rope(q_rope, ang)
        k_rope = rope(k_rope, ang)
    q = jnp.concatenate([q_nope, q_rope], axis=-1)
    k = jnp.concatenate([k_nope, jnp.broadcast_to(k_rope, k_nope.shape[:-1] + (MLA_ROPE,))], axis=-1)
    return q, k, v


def mla_attention(q, k, v):
    scale = MLA_QK ** -0.5

    def block(qb):
        p = softmax32(jnp.einsum('bhqd,bhkd->bhqk', qb[0], k) * scale)
        return jnp.einsum('bhqk,bhkd->bhqd', p.astype(v.dtype), v)

    return sweep_query_blocks(block, (q,))


def merge_branches(p, ys, w_branch, w_out):
    b, t, _ = p.shape
    gates = jax.nn.sigmoid(p[..., OFF_G:OFF_G + N_BRANCH * D_MODEL].reshape(b, t, N_BRANCH, D_MODEL))
    yb = jnp.einsum('btnc,ncd->btnd', jnp.stack(ys, axis=2), w_branch)
    return (gates * yb).sum(axis=2) @ w_out


def moe(h, router_w, router_bias, w1, w3, w2):
    shp = h.shape
    hf = h.reshape(-1, D_MODEL)
    scores = jax.nn.sigmoid((hf @ router_w).astype(F32))
    sel = scores + router_bias.astype(F32)
    group_score = lax.top_k(sel.reshape(-1, N_GROUPS, GROUP_SIZE), GROUP_SCORE_K)[0].sum(-1)
    best = jnp.argmax(group_score, axis=-1)
    in_group = (jnp.arange(N_EXPERTS) // GROUP_SIZE)[None, :] == best[:, None]
    _, idx = lax.top_k(jnp.where(in_group, sel, -jnp.inf), TOP_K)
    w_sel = jnp.take_along_axis(scores, idx, axis=-1)
    w_sel = w_sel / w_sel.sum(-1, keepdims=True)
    combine = (jax.nn.one_hot(idx, N_EXPERTS, dtype=F32) * w_sel[..., None]).sum(1)
    a = jnp.einsum('nd,edf->enf', hf, w1)
    g = jnp.einsum('nd,edf->enf', hf, w3)
    hidden = jax.nn.silu(a) * g * combine.T[:, :, None].astype(h.dtype)
    return jnp.einsum('enf,efd->nd', hidden, w2).reshape(shp)


def token_mixer(h_lat, h_ctx, lam, lam_init, w_in, b_in, m_conv_w, m_conv_b, m_norm_g,
                da_q_norm_g, da_k_norm_g, da_subln_g, mla_cq_norm_g, mla_ckv_norm_g, mla_w_uq,
                mla_w_ukv, mla_q_norm_g, mla_k_norm_g, w_branch, w_out, with_ctx_out):
    n_lat = h_lat.shape[1]
    bsz = h_ctx.shape[0]
    p_lat = h_lat @ w_in + b_in
    p_ctx = h_ctx @ w_in + b_in

    zero = (jnp.zeros((bsz, M_HEADS, M_DH, M_DH), F32), jnp.zeros((bsz, M_HEADS, M_DH), F32),
            jnp.zeros((bsz, M_HEADS), F32))
    mq, mk, mv, mo_ctx, mg = mlstm_inputs(p_ctx, m_conv_w, m_conv_b)
    hm_ctx, st_f, st_b = mlstm_bidirectional(mq, mk, mv, mg, zero, zero)
    mq, mk, mv, mo_lat, mg = mlstm_inputs(p_lat, m_conv_w, m_conv_b)
    hm_lat, _, _ = mlstm_bidirectional(mq, mk, mv, mg, st_f, st_b)

    dq_c, dk_c, dv_c = diff_qkv(p_ctx, da_q_norm_g, da_k_norm_g, None)
    dq_l, dk_l, dv_l = diff_qkv(p_lat, da_q_norm_g, da_k_norm_g, axial_angles(n_lat, DA_DH))
    od_lat = diff_attention(dq_l, jnp.concatenate([dk_l, dk_c], axis=3), jnp.concatenate([dv_l, dv_c], axis=2), lam)

    aq_c, ak_c, av_c = mla_qkv(p_ctx, mla_cq_norm_g, mla_ckv_norm_g, mla_w_uq, mla_w_ukv,
                               mla_q_norm_g, mla_k_norm_g, None)
    aq_l, ak_l, av_l = mla_qkv(p_lat, mla_cq_norm_g, mla_ckv_norm_g, mla_w_uq, mla_w_ukv,
                               mla_q_norm_g, mla_k_norm_g, axial_angles(n_lat, MLA_ROPE))
    oa_lat = mla_attention(aq_l, jnp.concatenate([ak_l, ak_c], axis=2), jnp.concatenate([av_l, av_c], axis=2))

    y_lat = merge_branches(p_lat, (mlstm_output(hm_lat, mo_lat, m_norm_g),
                                   diff_output(od_lat, da_subln_g, lam_init),
                                   merge_heads(oa_lat)), w_branch, w_out)
    if not with_ctx_out:
        return y_lat, None
    od_ctx = diff_attention(dq_c, dk_c, dv_c, lam)
    oa_ctx = mla_attention(aq_c, ak_c, av_c)
    y_ctx = merge_branches(p_ctx, (mlstm_output(hm_ctx, mo_ctx, m_norm_g),
                                   diff_output(od_ctx, da_subln_g, lam_init),
                                   merge_heads(oa_ctx)), w_branch, w_out)
    return y_lat, y_ctx


def setup_inputs(seed: int = 0) -> dict:
    key = jax.random.key(seed)
    ks = jax.random.split(key, 32)
    nrm = lambda k, shape, s: jax.random.normal(k, shape, F32) * s
    gain = lambda k, shape: 1.0 + 0.02 * jax.random.normal(k, shape, F32)
    f_bias = jnp.linspace(3.0, 6.0, M_HEADS)
    b_in = nrm(ks[7], (DEPTH, D_IN), 0.02)
    b_in = b_in.at[:, OFF_MG + M_HEADS:OFF_MG + 2 * M_HEADS].add(f_bias)
    b_in = b_in.at[:, OFF_MG + 3 * M_HEADS:OFF_MG + 4 * M_HEADS].add(f_bias)
    return {
        'x': nrm(ks[0], (BATCH, SEQ, D_MODEL), 1.0),
        'c': nrm(ks[1], (BATCH, D_MODEL), 1.0),
        'ctx': nrm(ks[2], (BATCH, CTX_LEN, D_MODEL), 1.0),
        'c_ctx': nrm(ks[3], (D_MODEL,), 1.0),
        'w_ada': nrm(ks[4], (DEPTH, D_MODEL, 6 * D_MODEL), 0.5 * D_MODEL ** -0.5),
        'b_ada': nrm(ks[5], (DEPTH, 6 * D_MODEL), 0.02),
        'w_in': nrm(ks[6], (DEPTH, D_MODEL, D_IN), D_MODEL ** -0.5),
        'b_in': b_in,
        'm_conv_w': nrm(ks[8], (DEPTH, M_CONV, 2 * BRANCH_W), M_CONV ** -0.5),
        'm_conv_b': nrm(ks[9], (DEPTH, 2 * BRANCH_W), 0.02),
        'm_norm_g': gain(ks[10], (DEPTH, BRANCH_W)),
        'da_q_norm_g': gain(ks[11], (DEPTH, DA_DH)),
        'da_k_norm_g': gain(ks[12], (DEPTH, DA_DH)),
        'da_lambda': nrm(ks[13], (DEPTH, 4, DA_DH), 0.1),
        'da_subln_g': gain(ks[14], (DEPTH, DA_DV)),
        'mla_cq_norm_g': gain(ks[15], (DEPTH, MLA_Q_RANK)),
        'mla_ckv_norm_g': gain(ks[16], (DEPTH, MLA_KV_RANK)),
        'mla_w_uq': nrm(ks[17], (DEPTH, MLA_Q_RANK, MLA_HEADS * MLA_QK), MLA_Q_RANK ** -0.5),
        'mla_w_ukv': nrm(ks[18], (DEPTH, MLA_KV_RANK, MLA_HEADS * (MLA_NOPE + MLA_DV)), MLA_KV_RANK ** -0.5),
        'mla_q_norm_g': gain(ks[19], (DEPTH, MLA_QK)),
        'mla_k_norm_g': gain(ks[20], (DEPTH, MLA_QK)),
        'w_branch': nrm(ks[21], (DEPTH, N_BRANCH, BRANCH_W, D_MODEL), BRANCH_W ** -0.5),
        'w_out': nrm(ks[22], (DEPTH, D_MODEL, D_MODEL), D_MODEL ** -0.5),
        'moe_w1': nrm(ks[23], (DEPTH, N_EXPERTS, D_MODEL, D_EXPERT), D_MODEL ** -0.5),
        'moe_w3': nrm(ks[24], (DEPTH, N_EXPERTS, D_MODEL, D_EXPERT), D_MODEL ** -0.5),
        'moe_w2': nrm(ks[25], (DEPTH, N_EXPERTS, D_EXPERT, D_MODEL), D_EXPERT ** -0.5),
        'router_w': nrm(ks[26], (D_MODEL, N_EXPERTS), D_MODEL ** -0.5),
        'router_bias': nrm(ks[27], (N_EXPERTS,), 0.01),
    }


def reference(x, c, ctx, c_ctx, w_ada, b_ada, w_in, b_in, m_conv_w, m_conv_b, m_norm_g,
              da_q_norm_g, da_k_norm_g, da_lambda, da_subln_g, mla_cq_norm_g, mla_ckv_norm_g,
              mla_w_uq, mla_w_ukv, mla_q_norm_g, mla_k_norm_g, w_branch, w_out,
              moe_w1, moe_w3, moe_w2, router_w, router_bias):
    x_lat, x_ctx = x, ctx
    for l in range(DEPTH):
        last = l == DEPTH - 1
        mod_lat = (jax.nn.silu(c) @ w_ada[l] + b_ada[l])[:, None, :]
        mod_ctx = jax.nn.silu(c_ctx) @ w_ada[l] + b_ada[l]
        sh1, sc1, g1, sh2, sc2, g2 = jnp.split(mod_lat, 6, axis=-1)
        csh1, csc1, cg1, csh2, csc2, cg2 = jnp.split(mod_ctx, 6, axis=-1)
        lam_init = 0.8 - 0.6 * math.exp(-0.3 * l)
        lam_p = da_lambda[l].astype(F32)
        lam = jnp.exp(jnp.sum(lam_p[0] * lam_p[1])) - jnp.exp(jnp.sum(lam_p[2] * lam_p[3])) + lam_init
        y_lat, y_ctx = token_mixer(
            modulate(x_lat, sh1, sc1), modulate(x_ctx, csh1, csc1), lam, lam_init,
            w_in[l], b_in[l], m_conv_w[l], m_conv_b[l], m_norm_g[l],
            da_q_norm_g[l], da_k_norm_g[l], da_subln_g[l], mla_cq_norm_g[l], mla_ckv_norm_g[l],
            mla_w_uq[l], mla_w_ukv[l], mla_q_norm_g[l], mla_k_norm_g[l], w_branch[l], w_out[l],
            not last)
        x_lat = x_lat + g1 * y_lat
        x_lat = x_lat + g2 * moe(modulate(x_lat, sh2, sc2), router_w, router_bias,
                                 moe_w1[l], moe_w3[l], moe_w2[l])
        if not last:
            x_ctx = x_ctx + cg1 * y_ctx
            x_ctx = x_ctx + cg2 * moe(modulate(x_ctx, csh2, csc2), router_w, router_bias,
                                      moe_w1[l], moe_w3[l], moe_w2[l])
    return x_lat
```

```python
import contextlib
import math
import numpy as np
import concourse.bass as bass
import concourse.mybir as mybir
from concourse.bass_utils import run_bass_kernel_spmd

F32 = mybir.dt.float32
BF16 = mybir.dt.bfloat16
AF = mybir.ActivationFunctionType
ALU = mybir.AluOpType
AX = mybir.AxisListType

ENGS = ("pe", "act", "dve", "pool", "sp")
N_DMA_SEMS = 8

D = 2048
NKC = D // 128
DEPTH = 2
NT_C = 2
NT_L = 16
NT = NT_C + NT_L
NTOK = NT * 128
D_IN = 14160
OFF_MQK, OFF_MV, OFF_MO, OFF_MG = 0, 2048, 3072, 4096
OFF_DQ, OFF_DK, OFF_DV = 4112, 5136, 6160
OFF_CQ, OFF_CKV, OFF_KR, OFF_G = 7184, 7696, 7952, 8016
EPS = 1e-6
N_EXP = 16
INPUT_SHAPES = {
    "xs": [NTOK, D], "cvec": [2, D], "consts": [128, 512], "consts2": [4, 512],
    "rope_da2": [NTOK, 256], "rope_mla": [NTOK, 64],
    "w_ada": [DEPTH, D, 6 * D], "b_ada": [DEPTH, 6 * D],
    "w_in": [DEPTH, D, D_IN], "b_in": [DEPTH, D_IN],
    "m_conv_w": [DEPTH, 5, 2048], "m_conv_b": [DEPTH, 2048], "m_norm_g": [DEPTH, 1024],
    "da_q_norm_g": [DEPTH, 128], "da_k_norm_g": [DEPTH, 128], "da_lambda": [DEPTH, 4, 128],
    "da_subln_g": [DEPTH, 256], "mla_cq_norm_g": [DEPTH, 512], "mla_ckv_norm_g": [DEPTH, 256],
    "mla_w_uq": [DEPTH, 512, 1536], "mla_w_ukv": [DEPTH, 256, 2048],
    "mla_q_norm_g": [DEPTH, 192], "mla_k_norm_g": [DEPTH, 192],
    "w_branch": [DEPTH, 3, 1024, 2048], "w_out": [DEPTH, 2048, 2048],
    "moe_w1": [DEPTH, 16, 2048, 512], "moe_w3": [DEPTH, 16, 2048, 512], "moe_w2": [DEPTH, 16, 512, 2048],
    "router_w": [2048, 16], "router_bias": [16],
}


class Pipe:
    def __init__(self, nstages):
        self.n = nstages
        self.items = []

    def push(self, stages):
        self.items.append(stages)
        i = len(self.items) - 1
        for s in range(self.n):
            j = i - s
            if j >= 0 and s < len(self.items[j]):
                self.items[j][s]()

    def drain(self):
        last = len(self.items) - 1
        for extra in range(1, self.n):
            for s in range(extra, self.n):
                j = last + extra - s
                if 0 <= j <= last and s < len(self.items[j]):
                    self.items[j][s]()
        self.items = []


class Op:
    def __init__(self, eng, fn, dma):
        self.eng = eng
        self.fn = fn
        self.deps = []
        self.needed = False
        self.idx = -1
        self.dma = dma
        self.prev_dma = None
        self.cnt = 0


class Rec:
    def __init__(self):
        self.call = None

    def __getattr__(self, name):
        def f(*a, **k):
            self.call = (name, a, k)
            return self
        return f


class Sched:
    PID = 0
    GLOBAL = None

    def __init__(self, nc, same_engine_sync=True):
        self.nc = nc
        self.ops = {e: [] for e in ENGS}
        self.res = {}
        self.stack = contextlib.ExitStack()
        self.same_engine_sync = same_engine_sync
        self.dma_count = {e: 0 for e in ENGS}
        self.dma_last = {}
        self.seen = {e: {e2: -1 for e2 in ENGS} for e in ENGS}
        self.seen_dma = {e: {} for e in ENGS}
        Sched.PID += 1
        self.pid = Sched.PID

    def sb(self, name, shape, dt=F32):
        return self.stack.enter_context(self.nc.sbuf_tensor("p%d_%s" % (self.pid, name), list(shape), dt))

    def ps(self, name, shape, dt=F32):
        return self.stack.enter_context(self.nc.psum_tensor("p%d_%s" % (self.pid, name), list(shape), dt))

    def add(self, eng, fn, reads=(), writes=(), dma=False):
        rec = Rec()
        fn(rec)
        op = Op(eng, rec.call, dma)
        lst = self.ops[eng]
        op.idx = len(lst)
        deps = []
        for k in reads:
            r = self.res.get(k)
            if r is not None and r[0] is not None:
                deps.append(r[0])
        for k in writes:
            r = self.res.get(k)
            if r is not None:
                if r[0] is not None:
                    deps.append(r[0])
                deps.extend(r[1])
        seen = self.seen[eng]
        sd = self.seen_dma[eng]
        out = []
        for d in deps:
            if d is op:
                continue
            if d.eng == eng and not d.dma:
                if eng == "pe" or (not self.same_engine_sync and not dma):
                    continue
            if d.dma:
                key = (d.eng, d.dma_slot)
                if sd.get(key, -1) >= d.dma_seq:
                    continue
                sd[key] = d.dma_seq
                out.append(d)
            else:
                if seen[d.eng] >= d.idx:
                    continue
                seen[d.eng] = d.idx
                d.needed = True
                out.append(d)
        op.deps = out
        if dma:
            c = self.dma_count[eng]
            self.dma_count[eng] = c + 1
            op.dma_slot = c % N_DMA_SEMS
            op.dma_seq = c // N_DMA_SEMS
            prev = self.dma_last.get((eng, op.dma_slot))
            op.prev_dma = prev
            self.dma_last[(eng, op.dma_slot)] = op
            if prev is not None:
                sd[(eng, op.dma_slot)] = max(sd.get((eng, op.dma_slot), -1), prev.dma_seq)
        lst.append(op)
        for k in reads:
            r = self.res.setdefault(k, [None, []])
            r[1].append(op)
        for k in writes:
            self.res[k] = [op, []]
        return op

    def emit(self):
        nc = self.nc
        G = Sched.GLOBAL
        if G is None:
            G = Sched.GLOBAL = {"stack": contextlib.ExitStack(), "sems": {}, "dsems": {}, "base": {e: 0 for e in ENGS}, "dbase": {}}
        gs = G["stack"]
        for e in ENGS:
            if e not in G["sems"]:
                G["sems"][e] = gs.enter_context(nc.semaphore("gs_" + e))
        for e in ENGS:
            if self.dma_count[e] > 0:
                for j in range(N_DMA_SEMS):
                    if (e, j) not in G["dsems"]:
                        G["dsems"][(e, j)] = gs.enter_context(nc.semaphore("gd_%s_%d" % (e, j)))
                        G["dbase"][(e, j)] = 0
        sems, dsems, base, dbase = G["sems"], G["dsems"], G["base"], G["dbase"]
        final_cnt = {}
        for e in ENGS:
            c = base[e]
            last = None
            for op in self.ops[e]:
                if not op.dma:
                    last = op
            if last is not None:
                last.needed = True
            for op in self.ops[e]:
                if op.dma:
                    continue
                if op.needed:
                    c += 1
                op.cnt = c
            final_cnt[e] = c
        dval = lambda d: 16 * (dbase[(d.eng, d.dma_slot)] + d.dma_seq + 1)
        block = self.stack.enter_context(nc.Block())
        engmap = {"pe": block.tensor, "act": block.scalar, "dve": block.vector,
                  "pool": block.gpsimd, "sp": block.sync}

        def make(e):
            def body(eng):
                for op in self.ops[e]:
                    for d in op.deps:
                        if d.dma:
                            eng.wait_ge(dsems[(d.eng, d.dma_slot)], dval(d))
                        else:
                            eng.wait_ge(sems[d.eng], d.cnt)
                    if op.dma:
                        if op.prev_dma is not None:
                            eng.wait_ge(dsems[(e, op.prev_dma.dma_slot)], dval(op.prev_dma))
                        ins = getattr(eng, op.fn[0])(*op.fn[1], **op.fn[2])
                        ins.then_inc(dsems[(e, op.dma_slot)], 16)
                    else:
                        ins = getattr(eng, op.fn[0])(*op.fn[1], **op.fn[2])
                        if op.needed:
                            ins.then_inc(sems[e], 1)
                for e2 in ENGS:
                    if final_cnt[e2] > base[e2]:
                        eng.wait_ge(sems[e2], final_cnt[e2])
                for (e2, j), last in self.dma_last.items():
                    eng.wait_ge(dsems[(e2, j)], dval(last))
            return body

        for e in ENGS:
            engmap[e](make(e))
        for e in ENGS:
            base[e] = final_cnt[e]
        for (e2, j), last in self.dma_last.items():
            dbase[(e2, j)] += last.dma_seq + 1

    def close(self):
        self.stack.close()


def modoff(l, r, seg):
    return ((l * 2 + r) * 6 + seg) * D


class MK:
    def __init__(self, nc, dbg=()):
        self.nc = nc
        self.dbg = set(dbg)
        self.g = contextlib.ExitStack()
        self.T = {}
        Sx = self.scr
        Sx("modrow", [DEPTH, 2, 6 * D], F32)
        Sx("xres", [NTOK, D], F32)
        Sx("hTd", [128, NKC, NTOK], BF16)
        Sx("mqkT", [16, 128, NTOK], BF16)
        Sx("mk_tm", [NTOK, 1024], BF16)
        Sx("mv", [NTOK, 1024], BF16)
        Sx("mo", [NTOK, 1024], BF16)
        Sx("mg", [NTOK, 16], F32)
        Sx("dqT", [8, 128, NTOK], BF16)
        Sx("dkT", [8, 128, NTOK], BF16)
        Sx("dv", [NTOK, 1024], BF16)
        Sx("cqT", [4, 128, NTOK], BF16)
        Sx("ckvT", [2, 128, NTOK], BF16)
        Sx("akrT", [64, NTOK], BF16)
        Sx("gT", [48, 128, NTOK], BF16)
        Sx("aqTn", [8, 128, NTOK], BF16)
        Sx("aqTr", [8, 64, NTOK], BF16)
        Sx("akT", [8, 128, NTOK], BF16)
        Sx("av", [NTOK, 1024], BF16)
        Sx("ysT", [3, 8, 128, NTOK], BF16)
        Sx("hmf", [NTOK, 1024], F32)
        Sx("h2Td", [128, NKC, NTOK], BF16)
        Sx("mTd", [NKC, 128, NTOK], BF16)
        Sx("combd", [NTOK, 16], F32)
        Sx("hmb", [NTOK, 1024], F32)

    def tens(self, name):
        if name not in self.T:
            self.T[name] = self.nc.dram_tensor(name, list(INPUT_SHAPES[name]), F32, kind="ExternalInput")
        return self.T[name]

    def scr(self, name, shape, dt):
        kind = "ExternalOutput" if name in self.dbg else "Internal"
        self.T[name] = self.nc.dram_tensor(name, list(shape), dt, kind=kind)

    def ap(self, name):
        return self.tens(name).ap()

    def bcast_rows(self, name, offset, width, nparts=128):
        return bass.AP(self.tens(name), offset, [[0, nparts], [1, width]])

    def load_consts(self):
        nc = self.nc
        self.ident_f = self.g.enter_context(nc.sbuf_tensor("ident_f", [128, 128], F32))
        self.ident_b = self.g.enter_context(nc.sbuf_tensor("ident_b", [128, 128], BF16))
        self.eps_col = self.g.enter_context(nc.sbuf_tensor("eps_col", [128, 1], F32))
        S = Sched(nc)
        S.add("pool", lambda e: e.memset(self.eps_col[:], EPS), writes=["epsc"])
        S.add("sp", lambda e: e.dma_start(out=self.ident_f[:], in_=self.ap("consts")[:, 0:128]),
              writes=["idf"], dma=True)
        S.add("dve", lambda e: e.tensor_copy(out=self.ident_b[:], in_=self.ident_f[:]), reads=["idf"], writes=["idb"])
        for t in range(0, NT, 6):
            S.add("sp", lambda e, t=t: e.dma_start(out=self.ap("xres")[t * 128:(t + 6) * 128, :],
                                                   in_=self.ap("xs")[t * 128:(t + 6) * 128, :]), dma=True)
        S.emit()
        S.close()

    def phase_ada(self, l):
        nc = self.nc
        S = Sched(nc)
        cv = S.sb("cv", [2, D], F32)
        sv = S.sb("sv", [2, D], F32)
        sT = S.sb("sT", [128, NKC, 2], BF16)
        pT = S.ps("pT", [128, NKC, 2], F32)
        wb = [S.sb("wada%d" % i, [128, NKC, 512], BF16) for i in range(2)]
        bb = [S.sb("bada%d" % i, [2, 512], F32) for i in range(2)]
        ob = [S.sb("oada%d" % i, [2, 512], F32) for i in range(2)]
        pm = [S.ps("pm%d" % i, [128, 512], F32) for i in range(2)]
        S.add("sp", lambda e: e.dma_start(out=cv[:], in_=self.ap("cvec")), writes=["cv"], dma=True)
        S.add("act", lambda e: e.activation(out=sv[:], in_=cv[:], func=AF.Silu), reads=["cv"], writes=["sv"])
        for k in range(NKC):
            S.add("pe", lambda e, k=k: e.transpose(out=pT[:, k, :], in_=sv[:, k * 128:(k + 1) * 128],
                                                   identity=self.ident_f[0:2, 0:2]),
                  reads=["sv"], writes=["pT"])
        S.add("dve", lambda e: e.tensor_copy(out=sT[:], in_=pT[:]), reads=["pT"], writes=["sT"])
        wl = self.ap("w_ada")[l]
        nblk = 6 * D // 512
        for j in range(nblk):
            i = j % 2
            S.add("pool", lambda e, j=j, i=i: e.dma_start(
                out=wb[i][:], in_=wl[:, j * 512:(j + 1) * 512].rearrange("(k p) w -> p k w", p=128)),
                writes=[("wb", i)], dma=True)
            S.add("sp", lambda e, j=j, i=i: e.dma_start(
                out=bb[i][:], in_=self.bcast_rows("b_ada", l * 6 * D + j * 512, 512, 2)),
                writes=[("bb", i)], dma=True)
            for k in range(NKC):
                S.add("pe", lambda e, k=k, i=i: e.matmul(pm[i][0:2, :], sT[:, k, :], wb[i][:, k, :],
                                                         start=(k == 0), stop=(k == NKC - 1)),
                      reads=["sT", ("wb", i)], writes=[("pm", i)])
            S.add("dve", lambda e, i=i: e.tensor_tensor(out=ob[i][:], in0=pm[i][0:2, :], in1=bb[i][:], op=ALU.add),
                  reads=[("pm", i), ("bb", i)], writes=[("ob", i)])
            S.add("sp", lambda e, j=j, i=i: e.dma_start(out=self.ap("modrow")[l][:, j * 512:(j + 1) * 512], in_=ob[i][:]),
                  reads=[("ob", i)], dma=True)
        S.emit()
        S.close()

    def load_mod_tiles(self, S, l, segs, tag):
        out = {}
        for r in (0, 1):
            for seg, add1 in segs:
                tl = S.sb("mod%s_%d_%d" % (tag, r, seg), [128, D], F32)
                key = ("mod", r, seg)
                S.add("sp", lambda e, tl=tl, r=r, seg=seg: e.dma_start(
                    out=tl[:], in_=self.bcast_rows("modrow", modoff(l, r, seg), D)), writes=[key], dma=True)
                if add1:
                    S.add("pool", lambda e, tl=tl: e.tensor_scalar_add(tl[:], tl[:], 1.0), reads=[key], writes=[key])
                out[(r, seg)] = tl
        return out

    def rstd(self, S, ap, key, n):
        S.add("act", lambda e: e.activation(out=ap, in_=ap, func=AF.Sqrt, scale=1.0 / n, bias=self.eps_col[:, 0:1]),
              reads=[key], writes=[key])
        S.add("dve", lambda e: e.reciprocal(out=ap, in_=ap), reads=[key], writes=[key])

    def rms_modulate(self, S, xt, xkey, ss, sskey, junk, sc, sckey, sh, shkey, xm32, xmb, xmkey):
        S.add("act", lambda e: e.activation(out=junk[:], in_=xt[:], func=AF.Square, accum_out=ss),
              reads=[xkey, sskey], writes=["junk", sskey])
        self.rstd(S, ss, sskey, D)
        S.add("dve", lambda e: e.scalar_tensor_tensor(out=xm32[:], in0=xt[:], scalar=ss, in1=sc[:],
                                                      op0=ALU.mult, op1=ALU.mult),
              reads=[xkey, sskey, sckey], writes=["xm32"])
        S.add("pool", lambda e: e.tensor_tensor(out=xmb[:], in0=xm32[:], in1=sh[:], op=ALU.add),
              reads=["xm32", shkey], writes=[xmkey])

    def phase_mod1(self, l):
        nc = self.nc
        S = Sched(nc)
        mods = self.load_mod_tiles(S, l, [(1, True), (0, False)], "a")
        xts = [S.sb("xt%d" % i, [128, D], F32) for i in range(2)]
        junk = S.sb("junk", [128, D], BF16)
        xm32 = S.sb("xm32", [128, D], F32)
        xmbs = [S.sb("xmb%d" % i, [128, D], BF16) for i in range(2)]
        ssb = S.sb("ssb", [128, NT], F32)
        hst = [S.sb("hst%d" % i, [128, NKC, 128], BF16) for i in range(2)]
        pts = [S.ps("pt%d" % i, [128, 8, 128], BF16) for i in range(4)]
        S.add("dve", lambda e: e.memset(ssb[:], 0.0), writes=[("ss", t) for t in range(NT)])
        pipe = Pipe(2)

        def make_item(t):
            r = 1 if t < NT_C else 0
            i = t % 2
            xt = xts[i]

            def s0():
                S.add("sp", lambda e: e.dma_start(out=xt[:], in_=self.ap("xres")[t * 128:(t + 1) * 128, :]),
                      writes=[("xt", i)], dma=True)
                self.rms_modulate(S, xt, ("xt", i), ssb[:, t:t + 1], ("ss", t), junk,
                                  mods[(r, 1)], ("mod", r, 1), mods[(r, 0)], ("mod", r, 0), xm32, xmbs[i], ("xmb", i))

            def s1():
                for j in range(NKC):
                    pt = pts[2 * i + j // 8]
                    S.add("pe", lambda e, pt=pt, j=j: e.transpose(out=pt[:, j % 8, :], in_=xmbs[i][:, j * 128:(j + 1) * 128],
                                                                  identity=self.ident_b[:]),
                          reads=[("xmb", i)], writes=[("pt", 2 * i + j // 8)])
                S.add("act", lambda e: e.copy(out=hst[i][:, 0:8, :], in_=pts[2 * i][:]),
                      reads=[("pt", 2 * i)], writes=[("hst", i, 0)])
                S.add("dve", lambda e: e.tensor_copy(out=hst[i][:, 8:16, :], in_=pts[2 * i + 1][:]),
                      reads=[("pt", 2 * i + 1)], writes=[("hst", i, 1)])
                S.add("sp", lambda e: e.dma_start(out=self.ap("hTd")[:, :, t * 128:(t + 1) * 128], in_=hst[i][:]),
                      reads=[("hst", i, 0), ("hst", i, 1)], dma=True)

            return [s0, s1]

        for t in range(NT):
            pipe.push(make_item(t))
        pipe.drain()
        S.emit()
        S.close()

    def rows_to_cols(self, S, name, src_ap_rows, nrows, nchunks, pst, pstkey, eng="dve"):
        rt = S.sb(name + "_r", [nrows, nchunks * 128], F32)
        ct = S.sb(name + "_c", [128, nchunks, nrows], F32)
        S.add("sp", lambda e: e.dma_start(out=rt[:], in_=src_ap_rows), writes=[name + "_r"], dma=True)
        for c in range(nchunks):
            S.add("pe", lambda e, c=c: e.transpose(out=pst[:, c * nrows:(c + 1) * nrows], in_=rt[:, c * 128:(c + 1) * 128],
                                                   identity=self.ident_f[0:nrows, 0:nrows]),
                  reads=[name + "_r"], writes=[pstkey])
        S.add(eng, lambda e: e.tensor_copy(out=ct[:].rearrange("p c r -> p (c r)"), in_=pst[:, 0:nchunks * nrows]),
              reads=[pstkey], writes=[name + "_c"])
        return ct

    def phase_inproj(self, l, groups=None):
        nc = self.nc
        S = Sched(nc)
        hT = S.sb("hT", [128, NKC, NTOK], BF16)
        wbs = [S.sb("wblk%d" % i, [128, NKC, 512], BF16) for i in range(2)]
        bts = [S.sb("bt%d" % i, [128, 512], F32) for i in range(2)]
        pa = [S.ps("pa%d" % i, [128, 512], F32) for i in range(5)]
        pt = [S.ps("ptb%d" % i, [128, 8, 128], BF16) for i in range(2)]
        pmisc = S.ps("pmisc", [128, 512], F32)
        st32 = [S.sb("st32_%d" % i, [128, 512], F32) for i in range(2)]
        stb = [S.sb("stb%d" % i, [128, 512], BF16) for i in range(3)]
        NROT = 2
        tmpR = [[S.sb("tmp%d_%d" % (i, r_), [128, 512], F32) for i in range(3)] for r_ in range(NROT)]
        ss4R = [S.sb("ss4_%d" % r_, [128, 8], F32) for r_ in range(NROT)]
        rot = {"i": 0}
        tmp = tmpR[0]
        ss4 = ss4R[0]
        trs = [S.sb("trs%d" % i, [128, 4, 128], BF16) for i in range(4)]
        w_l = self.ap("w_in")[l]
        st = {"wcnt": 0, "pa": 0, "stb": 0, "st32": 0, "pt": 0, "trs": 0}
        for h in range(0, NT, 6):
            S.add("sp", lambda e, h=h: e.dma_start(out=hT[:, :, h * 128:(h + 6) * 128],
                                                   in_=self.ap("hTd")[:, :, h * 128:(h + 6) * 128]),
                  writes=[("hT", h // 6)], dma=True)
        hkeys = [("hT", i) for i in range(3)]

        plan = []
        _do = lambda g: groups is None or g in groups
        for gname, off in (("mv", OFF_MV), ("dv", OFF_DV), ("mo", OFF_MO)):
            if _do(gname):
                plan += [(off, 512, True), (off + 512, 512, True)]
        if _do("mg"):
            plan.append((OFF_MG, 16, True))
        for gname, off in (("dq", OFF_DQ), ("dk", OFF_DK)):
            if _do(gname):
                plan += [(off, 512, True), (off + 512, 512, True)]
        if _do("cq"):
            plan.append((OFF_CQ, 512, True))
        if _do("kr"):
            plan.append((OFF_CKV, 320, True))
        if _do("g"):
            plan += [(OFF_G + c0, 512, False) for c0 in range(0, 6144, 512)]
        if _do("mqk"):
            plan += [(OFF_MQK + c0, 512, False) for c0 in range(0, 2048, 512)]
        issued = {"n": 0}

        def issue_w(k):
            c0, W, bias = plan[k]
            i = k % 2
            S.add("pool", lambda e: e.dma_start(out=wbs[i][:, :, 0:W],
                                                in_=w_l[:, c0:c0 + W].rearrange("(k p) w -> p k w", p=128)),
                  writes=[("wb", i)], dma=True)
            if bias:
                S.add("sp", lambda e: e.dma_start(out=bts[i][:, 0:W], in_=self.bcast_rows("b_in", l * D_IN + c0, W)),
                      writes=[("bt", i)], dma=True)

        def load_w(c0, W, bias=True):
            k = st["wcnt"]
            st["wcnt"] += 1
            assert plan[k] == (c0, W, bias), (plan[k], c0, W, bias)
            while issued["n"] <= min(k + 1, len(plan) - 1):
                issue_w(issued["n"])
                issued["n"] += 1
            return k % 2

        def nxt(k, n):
            i = st[k] % n
            st[k] += 1
            return i

        def mm_tm(t, wi, W):
            p = nxt("pa", 5)
            for k in range(NKC):
                S.add("pe", lambda e, k=k: e.matmul(pa[p][:, 0:W], hT[:, k, t * 128:(t + 1) * 128], wbs[wi][:, k, 0:W],
                                                    start=(k == 0), stop=(k == NKC - 1)),
                      reads=[("hT", t // 6), ("wb", wi)], writes=[("pa", p)])
            return p

        def store(eng, dst_ap, src_ap, rkeys):
            S.add(eng, lambda e: e.dma_start(out=dst_ap, in_=src_ap), reads=rkeys, dma=True)

        do = lambda g: groups is None or g in groups

        for gname, off, width, dst in (("mv", OFF_MV, 1024, "mv"), ("dv", OFF_DV, 1024, "dv"), ("mo", OFF_MO, 1024, "mo")):
            if not do(gname):
                continue
            for c0 in range(0, width, 512):
                wi = load_w(off + c0, 512)
                for t in range(NT):
                    p = mm_tm(t, wi, 512)
                    b = nxt("stb", 3)
                    if gname == "mo":
                        a = nxt("st32", 2)
                        S.add("dve", lambda e, p=p, a=a: e.tensor_tensor(out=st32[a][:], in0=pa[p][:], in1=bts[wi][:], op=ALU.add),
                              reads=[("pa", p), ("bt", wi)], writes=[("st32", a)])
                        S.add("act", lambda e, a=a, b=b: e.activation(out=stb[b][:], in_=st32[a][:], func=AF.Sigmoid),
                              reads=[("st32", a)], writes=[("stb", b)])
                    else:
                        S.add("dve", lambda e, p=p, b=b: e.tensor_tensor(out=stb[b][:], in0=pa[p][:], in1=bts[wi][:], op=ALU.add),
                              reads=[("pa", p), ("bt", wi)], writes=[("stb", b)])
                    store("sp", self.ap(dst)[t * 128:(t + 1) * 128, c0:c0 + 512], stb[b][:], [("stb", b)])
        if do("mg"):
            wi = load_w(OFF_MG, 16)
            for t in range(NT):
                p = mm_tm(t, wi, 16)
                a = nxt("st32", 2)
                S.add("dve", lambda e, p=p, a=a: e.tensor_tensor(out=st32[a][:, 0:16], in0=pa[p][:, 0:16], in1=bts[wi][:, 0:16], op=ALU.add),
                      reads=[("pa", p), ("bt", wi)], writes=[("st32", a)])
                store("sp", self.ap("mg")[t * 128:(t + 1) * 128, :], st32[a][:, 0:16], [("st32", a)])

        def transposes_out(src_b, skey, ngrp, gw, dst_ap_fn):
            q = nxt("pt", 2)
            for g_ in range(ngrp):
                S.add("pe", lambda e, g_=g_: e.transpose(out=pt[q][0:gw, g_, :], in_=src_b[:, g_ * gw:(g_ + 1) * gw],
                                                         identity=self.ident_b[:]),
                      reads=[skey], writes=[("pt", q)])
            r = nxt("trs", 4)
            S.add("act", lambda e: e.copy(out=trs[r][0:gw, 0:ngrp, :], in_=pt[q][0:gw, 0:ngrp, :]),
                  reads=[("pt", q)], writes=[("trs", r)])
            store("sp", dst_ap_fn(), trs[r][0:gw, 0:ngrp, :], [("trs", r)])

        if do("kr"):
            rml = S.sb("rml", [128, NT, 64], F32)
            S.add("sp", lambda e: e.dma_start(out=rml[:], in_=self.ap("rope_mla").rearrange("(t p) c -> p t c", p=128)),
                  writes=["rml"], dma=True)

        def rope(src, skey, ngrp, half, cos_ap, sin_ap, dstb, dkey):
            x1 = src[:, :, 0:half]
            x2 = src[:, :, half:2 * half]
            cb = cos_ap.unsqueeze(1).to_broadcast([128, ngrp, half])
            sb_ = sin_ap.unsqueeze(1).to_broadcast([128, ngrp, half])
            ta = tmp[0][:, 0:ngrp * half].rearrange("p (g h) -> p g h", g=ngrp)
            tb = tmp[1][:, 0:ngrp * half].rearrange("p (g h) -> p g h", g=ngrp)
            S.add("pool", lambda e: e.tensor_tensor(out=ta, in0=x1, in1=cb, op=ALU.mult), reads=[skey, "rml"], writes=[("tmp0", rot["i"])])
            S.add("pool", lambda e: e.tensor_tensor(out=tb, in0=x2, in1=sb_, op=ALU.mult), reads=[skey, "rml"], writes=[("tmp1", rot["i"])])
            S.add("dve", lambda e: e.tensor_tensor(out=dstb[:, :, 0:half], in0=ta, in1=tb, op=ALU.subtract),
                  reads=[("tmp0", rot["i"]), ("tmp1", rot["i"])], writes=[dkey])
            S.add("pool", lambda e: e.tensor_tensor(out=ta, in0=x1, in1=sb_, op=ALU.mult), reads=[skey, "rml"], writes=[("tmp0", rot["i"])])
            S.add("dve", lambda e: e.tensor_tensor(out=tb, in0=x2, in1=cb, op=ALU.mult), reads=[skey, "rml"], writes=[("tmp1", rot["i"])])
            S.add("dve", lambda e: e.tensor_tensor(out=dstb[:, :, half:2 * half], in0=ta, in1=tb, op=ALU.add),
                  reads=[("tmp0", rot["i"]), ("tmp1", rot["i"])], writes=[dkey])

        if do("dq") or do("dk"):
            rdt = [S.sb("rdt%d" % i, [128, 256], F32) for i in range(2)]
            ones_b = S.sb("ones_b", [1, 128], BF16)
            S.add("pool", lambda e: e.memset(ones_b[:], 1.0), writes=["ones_b"])
            brow = [S.sb("brow%d" % i, [1, 512], BF16) for i in range(2)]
            junkq = S.sb("junkq", [128, 4, 128], BF16)
            stq = S.sb("stq18", [128, NT, 512], BF16)
            ssq4 = [S.sb("ssq4_%d" % i, [128, 4], F32) for i in range(4)]
            xg4 = [S.sb("xg4_%d" % i, [128, 512], F32) for i in range(2)]
            tq1 = [S.sb("tq1_%d" % i, [128, 512], F32) for i in range(2)]
            tq2 = [S.sb("tq2_%d" % i, [128, 512], F32) for i in range(1)]
            qc = {"n": 0}
        for gname, off, gain, scale, dst in (("dq", OFF_DQ, "da_q_norm_g", 128 ** -0.5, "dqT"), ("dk", OFF_DK, "da_k_norm_g", 1.0, "dkT")):
            if not do(gname):
                continue
            gb = S.sb("gb_" + gname, [128, 128], F32)
            S.add("sp", lambda e, gb=gb, gain=gain: e.dma_start(out=gb[:], in_=self.bcast_rows(gain, l * 128, 128)),
                  writes=["gb_" + gname], dma=True)
            S.add("pool", lambda e, gb=gb, scale=scale: e.tensor_scalar_mul(gb[:], gb[:], float(scale)),
                  reads=["gb_" + gname], writes=["gb_" + gname])
            for c0 in range(0, 1024, 512):
                wi = load_w(off + c0, 512)
                S.add("pool", lambda e: e.dma_start(out=brow[wi][:], in_=bass.AP(self.tens("b_in"), l * D_IN + off + c0, [[0, 1], [1, 512]])),
                      writes=[("brow", wi)], dma=True)
                for t in range(NT):
                    n_ = qc["n"]
                    qc["n"] += 1
                    r4, r3, r2 = n_ % 4, n_ % 2, 0
                    r1 = n_ % 2
                    ss = ssq4[r4]
                    xg = xg4[r3]
                    rd = rdt[r3]
                    p = nxt("pa", 5)
                    S.add("sp", lambda e: e.dma_start(out=rd[:], in_=self.ap("rope_da2")[t * 128:(t + 1) * 128, :]), writes=[("rdt", r3)], dma=True)
                    for k in range(NKC):
                        S.add("pe", lambda e, k=k: e.matmul(pa[p][:], hT[:, k, t * 128:(t + 1) * 128], wbs[wi][:, k, :],
                                                            start=(k == 0), stop=False),
                              reads=[("hT", t // 6), ("wb", wi)], writes=[("pa", p)])
                    S.add("pe", lambda e: e.matmul(pa[p][:], ones_b[:], brow[wi][:], start=False, stop=True),
                          reads=["ones_b", ("brow", wi)], writes=[("pa", p)])
                    S.add("dve", lambda e: e.memset(ss[:], 0.0), reads=[("ssq", r4)], writes=[("ssq", r4)])
                    for g_ in range(4):
                        S.add("act", lambda e, g_=g_: e.activation(out=junkq[:, g_, :], in_=pa[p][:, g_ * 128:(g_ + 1) * 128], func=AF.Square,
                                                                   accum_out=ss[:, g_:g_ + 1]),
                              reads=[("pa", p), ("ssq", r4)], writes=[("junkq", g_), ("ssqc", r4, g_)])
                    S.add("act", lambda e: e.activation(out=ss[:], in_=ss[:], func=AF.Sqrt, scale=1.0 / 128, bias=self.eps_col[:, 0:1]),
                          reads=[("ssqc", r4, g_) for g_ in range(4)] + [("ssq", r4)], writes=[("ssq", r4)] + [("ssqc", r4, g_) for g_ in range(4)])
                    x3 = xg[:].rearrange("p (g h) -> p g h", g=4)
                    S.add("dve", lambda e, x3=x3, gb=gb: e.tensor_tensor(out=x3, in0=pa[p][:].rearrange("p (g h) -> p g h", g=4),
                                                                         in1=gb[:].unsqueeze(1).to_broadcast([128, 4, 128]), op=ALU.mult),
                          reads=[("pa", p), "gb_" + gname] + [("ssqc", r4, g_) for g_ in range(4)], writes=[("xg", r3)])
                    S.add("dve", lambda e: e.reciprocal(out=ss[:], in_=ss[:]), reads=[("ssq", r4)], writes=[("ssq", r4)])
                    t1 = tq1[r1][:].rearrange("p (g h) -> p g h", g=4)
                    t2 = tq2[r2][:].rearrange("p (g h) -> p g h", g=4)
                    S.add("dve", lambda e: e.tensor_tensor(out=t1, in0=x3, in1=rd[:, 0:128].unsqueeze(1).to_broadcast([128, 4, 128]), op=ALU.mult),
                          reads=[("xg", r3), ("rdt", r3)], writes=[("tq1", r1)])
                    S.add("pool", lambda e: e.tensor_tensor(out=t2[:, :, 0:64], in0=x3[:, :, 64:128],
                                                            in1=rd[:, 128:192].unsqueeze(1).to_broadcast([128, 4, 64]), op=ALU.mult),
                          reads=[("xg", r3), ("rdt", r3)], writes=[("tq2", r2)])
                    S.add("pool", lambda e: e.tensor_tensor(out=t2[:, :, 64:128], in0=x3[:, :, 0:64],
                                                            in1=rd[:, 192:256].unsqueeze(1).to_broadcast([128, 4, 64]), op=ALU.mult),
                          reads=[("xg", r3), ("rdt", r3)], writes=[("tq2", r2)])
                    S.add("pool", lambda e: e.tensor_tensor(out=t1, in0=t1, in1=t2, op=ALU.add),
                          reads=[("tq1", r1), ("tq2", r2)], writes=[("tq1", r1)])
                    S.add("pool", lambda e: e.tensor_tensor(out=stq[:, t, :].rearrange("p (g h) -> p g h", g=4), in0=t1,
                                                            in1=ss[:].unsqueeze(2).to_broadcast([128, 4, 128]), op=ALU.mult),
                          reads=[("tq1", r1), ("ssq", r4)], writes=[("stq", t)])
                g0 = c0 // 128
                for t in range(NT):
                    transposes_out(stq[:, t, :], ("stq", t), 4, 128,
                                   lambda g0=g0, t=t, dst=dst: self.ap(dst)[g0:g0 + 4, :, t * 128:(t + 1) * 128].rearrange("g p n -> p g n"))

        def bias_mm_tile(t, wi, W, p):
            for k in range(NKC):
                S.add("pe", lambda e, k=k: e.matmul(pa[p][:, 0:W], hT[:, k, t * 128:(t + 1) * 128], wbs[wi][:, k, 0:W], start=(k == 0), stop=False),
                      reads=[("hT", t // 6), ("wb", wi)], writes=[("pa", p)])
            S.add("pe", lambda e: e.matmul(pa[p][:, 0:W], ones_b[:], brow[wi][:, 0:W], start=False, stop=True),
                  reads=["ones_b", ("brow", wi)], writes=[("pa", p)])

        if do("cq"):
            gcq = S.sb("gcq", [128, 512], F32)
            S.add("sp", lambda e: e.dma_start(out=gcq[:], in_=self.bcast_rows("mla_cq_norm_g", l * 512, 512)), writes=["gcq"], dma=True)
            wi = load_w(OFF_CQ, 512)
            S.add("pool", lambda e: e.dma_start(out=brow[wi][:], in_=bass.AP(self.tens("b_in"), l * D_IN + OFF_CQ, [[0, 1], [1, 512]])),
                  writes=[("brow", wi)], dma=True)
            for t in range(NT):
                n_ = qc["n"]
                qc["n"] += 1
                r4 = n_ % 4
                ss = ssq4[r4]
                p = nxt("pa", 5)
                bias_mm_tile(t, wi, 512, p)
                S.add("dve", lambda e: e.memset(ss[:], 0.0), reads=[("ssq", r4)], writes=[("ssq", r4)])
                S.add("act", lambda e: e.activation(out=junkq[:].rearrange("p g h -> p (g h)"), in_=pa[p][:], func=AF.Square, accum_out=ss[:, 0:1]),
                      reads=[("pa", p), ("ssq", r4)], writes=[("junkq", g_) for g_ in range(4)] + [("ssq", r4)])
                S.add("act", lambda e: e.activation(out=ss[:, 0:1], in_=ss[:, 0:1], func=AF.Sqrt, scale=1.0 / 512, bias=self.eps_col[:, 0:1]),
                      reads=[("ssq", r4)], writes=[("ssq", r4)])
                S.add("dve", lambda e: e.reciprocal(out=ss[:, 0:1], in_=ss[:, 0:1]), reads=[("ssq", r4)], writes=[("ssq", r4)])
                S.add("dve", lambda e: e.scalar_tensor_tensor(out=stq[:, t, :], in0=pa[p][:], scalar=ss[:, 0:1], in1=gcq[:], op0=ALU.mult, op1=ALU.mult),
                      reads=[("pa", p), ("ssq", r4), "gcq"], writes=[("stq", t)])
            for t in range(NT):
                transposes_out(stq[:, t, :], ("stq", t), 4, 128,
                               lambda t=t: self.ap("cqT")[:, :, t * 128:(t + 1) * 128].rearrange("g p n -> p g n"))

        if do("kr"):
            gkv = S.sb("gkv", [128, 320], F32)
            S.add("sp", lambda e: e.dma_start(out=gkv[:, 0:256], in_=self.bcast_rows("mla_ckv_norm_g", l * 256, 256)), writes=["gkv"], dma=True)
            S.add("sp", lambda e: e.dma_start(out=gkv[:, 256:320], in_=self.bcast_rows("mla_k_norm_g", l * 192 + 128, 64)), writes=["gkv"], dma=True)
            wi = load_w(OFF_CKV, 320)
            S.add("pool", lambda e: e.dma_start(out=brow[wi][:, 0:320], in_=bass.AP(self.tens("b_in"), l * D_IN + OFF_CKV, [[0, 1], [1, 320]])),
                  writes=[("brow", wi)], dma=True)
            for t in range(NT):
                n_ = qc["n"]
                qc["n"] += 1
                r4 = n_ % 4
                r2_ = n_ % 2
                ss = ssq4[r4]
                kx = xg4[r2_]
                p = nxt("pa", 5)
                bias_mm_tile(t, wi, 320, p)
                S.add("dve", lambda e: e.memset(ss[:], 0.0), reads=[("ssq", r4)], writes=[("ssq", r4)])
                S.add("act", lambda e: e.activation(out=junkq[:, 0:2, :].rearrange("p g h -> p (g h)"), in_=pa[p][:, 0:256], func=AF.Square, accum_out=ss[:, 0:1]),
                      reads=[("pa", p), ("ssq", r4)], writes=[("junkq", 0), ("junkq", 1), ("ssqa", r4)])
                S.add("act", lambda e: e.activation(out=junkq[:, 2, 0:64], in_=pa[p][:, 256:320], func=AF.Square, accum_out=ss[:, 1:2]),
                      reads=[("pa", p), ("ssq", r4)], writes=[("junkq", 2), ("ssqb", r4)])
                S.add("act", lambda e: e.activation(out=ss[:, 0:1], in_=ss[:, 0:1], func=AF.Sqrt, scale=1.0 / 256, bias=self.eps_col[:, 0:1]),
                      reads=[("ssqa", r4), ("ssqb", r4), ("ssq", r4)], writes=[("ssq", r4), ("ssqa", r4)])
                S.add("act", lambda e: e.activation(out=ss[:, 1:2], in_=ss[:, 1:2], func=AF.Sqrt, scale=1.0 / 64, bias=self.eps_col[:, 0:1]),
                      reads=[("ssq", r4), ("ssqb", r4)], writes=[("ssq", r4), ("ssqb", r4)])
                S.add("dve", lambda e: e.reciprocal(out=ss[:, 0:2], in_=ss[:, 0:2]), reads=[("ssq", r4)], writes=[("ssq", r4)])
                S.add("dve", lambda e: e.scalar_tensor_tensor(out=stq[:, t, 0:256], in0=pa[p][:, 0:256], scalar=ss[:, 0:1], in1=gkv[:, 0:256],
                                                              op0=ALU.mult, op1=ALU.mult),
                      reads=[("pa", p), ("ssq", r4), "gkv"], writes=[("stq", t)])
                S.add("dve", lambda e: e.scalar_tensor_tensor(out=kx[:, 0:64], in0=pa[p][:, 256:320], scalar=ss[:, 1:2], in1=gkv[:, 256:320],
                                                              op0=ALU.mult, op1=ALU.mult),
                      reads=[("pa", p), ("ssq", r4), "gkv"], writes=[("xg", r2_)])
                x1, x2 = kx[:, 0:32], kx[:, 32:64]
                cs, sn = rml[:, t, 0:32], rml[:, t, 32:64]
                ta, tb_ = tq1[0][:, 0:32], tq2[0][:, 0:32]
                pk = lambda fn, rd_, wr_: S.add("pool", fn, reads=rd_, writes=wr_)
                pk(lambda e: e.tensor_tensor(out=ta, in0=x1, in1=cs, op=ALU.mult), [("xg", r2_), "rml"], [("tq1", 0)])
                pk(lambda e: e.tensor_tensor(out=tb_, in0=x2, in1=sn, op=ALU.mult), [("xg", r2_), "rml"], [("tq2", 0)])
                pk(lambda e: e.tensor_tensor(out=stq[:, t, 256:288], in0=ta, in1=tb_, op=ALU.subtract), [("tq1", 0), ("tq2", 0)], [("stq", t)])
                pk(lambda e: e.tensor_tensor(out=ta, in0=x1, in1=sn, op=ALU.mult), [("xg", r2_), "rml"], [("tq1", 0)])
                pk(lambda e: e.tensor_tensor(out=tb_, in0=x2, in1=cs, op=ALU.mult), [("xg", r2_), "rml"], [("tq2", 0)])
                pk(lambda e: e.tensor_tensor(out=stq[:, t, 288:320], in0=ta, in1=tb_, op=ALU.add), [("tq1", 0), ("tq2", 0)], [("stq", t)])
            for t in range(NT):
                transposes_out(stq[:, t, :], ("stq", t), 2, 128,
                               lambda t=t: self.ap("ckvT")[:, :, t * 128:(t + 1) * 128].rearrange("g p n -> p g n"))
                q = nxt("pt", 2)
                S.add("pe", lambda e, q=q: e.transpose(out=pt[q][0:64, 0, :], in_=stq[:, t, 256:320], identity=self.ident_b[:]),
                      reads=[("stq", t)], writes=[("pt", q)])
                r = nxt("trs", 4)
                S.add("act", lambda e, r=r, q=q: e.copy(out=trs[r][0:64, 0, :], in_=pt[q][0:64, 0, :]), reads=[("pt", q)], writes=[("trs", r)])
                store("sp", self.ap("akrT")[:, t * 128:(t + 1) * 128], trs[r][0:64, 0, :], [("trs", r)])

        tb = [(0, 512), (512, 512), (1024, 512), (1536, 512), (2048, 256)]

        def mm_fm(wi, j, t0, n):
            p = nxt("pa", 5)
            for k in range(NKC):
                S.add("pe", lambda e, k=k: e.matmul(pa[p][:, 0:n], wbs[wi][:, k, j * 128:(j + 1) * 128], hT[:, k, t0:t0 + n],
                                                    start=(k == 0), stop=(k == NKC - 1)),
                      reads=hkeys + [("wb", wi)], writes=[("pa", p)])
            return p

        if do("g"):
            bcg = self.rows_to_cols(S, "bcg", bass.AP(self.tens("b_in"), l * D_IN + OFF_G, [[128, 48], [1, 128]]), 48, 1, pmisc, "pmisc")
            for c0 in range(0, 6144, 512):
                wi = load_w(OFF_G + c0, 512, bias=False)
                for j in range(4):
                    ch = c0 // 128 + j
                    for (t0, n) in tb:
                        p = mm_fm(wi, j, t0, n)
                        b = nxt("stb", 3)
                        S.add("act", lambda e, p=p, b=b, n=n, ch=ch: e.activation(out=stb[b][:, 0:n], in_=pa[p][:, 0:n], func=AF.Sigmoid,
                                                                                  bias=bcg[:, 0, ch:ch + 1]),
                              reads=[("pa", p), "bcg_c"], writes=[("stb", b)])
                        store("sp", self.ap("gT")[ch, :, t0:t0 + n], stb[b][:, 0:n], [("stb", b)])

        if do("mqk"):
            bcq = self.rows_to_cols(S, "bcq", bass.AP(self.tens("b_in"), l * D_IN + OFF_MQK, [[128, 16], [1, 128]]), 16, 1, pmisc, "pmisc")
            cvb = self.rows_to_cols(S, "cvb", bass.AP(self.tens("m_conv_b"), l * 2048, [[128, 16], [1, 128]]), 16, 1, pmisc, "pmisc")
            cvw = self.rows_to_cols(S, "cvw", self.ap("m_conv_w")[l], 5, 16, pmisc, "pmisc")
            pre = [S.sb("pre%d" % i, [128, 2304 + 8], BF16) for i in range(2)]
            sl = [S.sb("sil%d" % i, [128, 2304], BF16) for i in range(2)]
            mks = [S.sb("mks%d" % i, [128, 6, 128], BF16) for i in range(2)]
            dg = [S.sb("dg%d" % i, [128, 5, 128], BF16) for i in range(2)]
            for i in range(2):
                S.add("pool", lambda e, i=i: e.memset(pre[i][:], 0.0), writes=[("pre", i)])
            oblocks = [(0, 256, 2)] + [(256 + 512 * j, 512, 262 + 512 * j) for j in range(4)]
            for c0 in range(0, 2048, 512):
                wi = load_w(OFF_MQK + c0, 512, bias=False)
                for j in range(4):
                    ch = c0 // 128 + j
                    i = ch % 2
                    for jj in range(5):
                        S.add("dve", lambda e, jj=jj: e.tensor_scalar(out=dg[i][:, jj, :], in0=self.ident_b[:], scalar1=cvw[:, ch, jj:jj + 1],
                                                                       scalar2=None, op0=ALU.mult),
                              reads=["cvw_c"], writes=[("dg", i)])
                    for (t0, n) in tb:
                        p = mm_fm(wi, j, t0, n)
                        po = (2 + t0) if t0 < 256 else (262 + t0 - 256)
                        if t0 == 0:
                            S.add("act", lambda e, p=p: e.activation(out=pre[i][:, 2:258], in_=pa[p][:, 0:256], func=AF.Identity,
                                                                     bias=bcq[:, 0, ch:ch + 1]),
                                  reads=[("pa", p), "bcq_c"], writes=[("pre", i)])
                            S.add("act", lambda e, p=p: e.activation(out=pre[i][:, 262:518], in_=pa[p][:, 256:512], func=AF.Identity,
                                                                     bias=bcq[:, 0, ch:ch + 1]),
                                  reads=[("pa", p), "bcq_c"], writes=[("pre", i)])
                        elif (t0 // 512) % 2 == 1:
                            S.add("act", lambda e, p=p, po=po, n=n: e.activation(out=pre[i][:, po:po + n], in_=pa[p][:, 0:n],
                                                                                 func=AF.Identity, bias=bcq[:, 0, ch:ch + 1]),
                                  reads=[("pa", p), "bcq_c"], writes=[("pre", i)])
                        else:
                            S.add("dve", lambda e, p=p, po=po, n=n: e.tensor_scalar(out=pre[i][:, po:po + n], in0=pa[p][:, 0:n],
                                                                                    scalar1=bcq[:, 0, ch:ch + 1], scalar2=None, op0=ALU.add),
                                  reads=[("pa", p), "bcq_c"], writes=[("pre", i)])
                    for (ts, n, po) in oblocks:
                        p = nxt("pa", 5)
                        for jj in range(5):
                            S.add("pe", lambda e, jj=jj: e.matmul(pa[p][:, 0:n], dg[i][:, jj, :], pre[i][:, po - 2 + jj:po - 2 + jj + n],
                                                                  start=(jj == 0), stop=(jj == 4)),
                                  reads=[("dg", i), ("pre", i)], writes=[("pa", p)])
                        S.add("act", lambda e, p=p, ts=ts, n=n: e.activation(out=sl[i][:, ts:ts + n], in_=pa[p][:, 0:n], func=AF.Silu,
                                                                             bias=cvb[:, 0, ch:ch + 1]),
                              reads=[("pa", p), "cvb_c"], writes=[("sl", i)])
                    if ch < 8:
                        S.add("pool", lambda e, i=i: e.tensor_scalar_mul(sl[i][:], sl[i][:], 1.0 / 16.0), reads=[("sl", i)], writes=[("sl", i)])
                    store("sp", self.ap("mqkT")[ch], sl[i][:], [("sl", i)])
                    if ch >= 8:
                        for t3 in range(0, NT, 6):
                            q = nxt("pt", 2)
                            for tt in range(6):
                                t = t3 + tt
                                S.add("pe", lambda e, q=q, tt=tt, t=t, i=i: e.transpose(out=pt[q][:, tt, :], in_=sl[i][:, t * 128:(t + 1) * 128],
                                                                                        identity=self.ident_b[:]),
                                      reads=[("sl", i)], writes=[("pt", q)])
                            m = nxt("trs", 2)
                            S.add("dve", lambda e, q=q, m=m: e.tensor_copy(out=mks[m][:], in_=pt[q][:, 0:6, :]), reads=[("pt", q)], writes=[("mks", m)])
                            store("sp", self.ap("mk_tm")[t3 * 128:(t3 + 6) * 128, (ch - 8) * 128:(ch - 7) * 128].rearrange("(t p) c -> p t c", p=128),
                                  mks[m][:], [("mks", m)])
        S.emit()
        S.close()


    def phase_mla_up(self, l):
        nc = self.nc
        S = Sched(nc)
        cqT = S.sb("cqTs", [128, 4, NTOK], BF16)
        ckvT = S.sb("ckvTs", [128, 2, NTOK], BF16)
        wuq = S.sb("wuq", [128, 4, 1536], BF16)
        wukv = S.sb("wukv", [128, 2, 2048], BF16)
        gq = S.sb("gq", [128, 192], F32)
        gk = S.sb("gk", [128, 128], F32)
        rml = S.sb("rml2", [128, NT, 64], F32)
        pq = [S.ps("pq%d" % i, [128, 512], F32) for i in range(4)]
        pt = [S.ps("ptm%d" % i, [128, 8, 128], BF16) for i in range(3)]
        st = [S.sb("stq%d" % i, [128, 2048], F32) for i in range(2)]
        sq = S.sb("sqq", [128, 2048], F32)
        qb = [S.sb("qb%d" % i, [128, 2048], BF16) for i in range(2)]
        ssq = S.sb("ssq", [128, 16], F32)
        tmpa = S.sb("tmpa", [128, 256], F32)
        tmpb = S.sb("tmpb", [128, 256], F32)
        trn = [S.sb("trn%d" % i, [128, 8, 128], BF16) for i in range(2)]
        trr = [S.sb("trr%d" % i, [64, 8, 128], BF16) for i in range(2)]
        S.add("sp", lambda e: e.dma_start(out=cqT[:], in_=self.ap("cqT").rearrange("g p n -> p g n")), writes=["cqT"], dma=True)
        S.add("sp", lambda e: e.dma_start(out=ckvT[:], in_=self.ap("ckvT").rearrange("g p n -> p g n")), writes=["ckvT"], dma=True)
        S.add("pool", lambda e: e.dma_start(out=wuq[:], in_=self.ap("mla_w_uq")[l].rearrange("(k p) w -> p k w", p=128)), writes=["wuq"], dma=True)
        S.add("pool", lambda e: e.dma_start(out=wukv[:], in_=self.ap("mla_w_ukv")[l].rearrange("(k p) w -> p k w", p=128)), writes=["wukv"], dma=True)
        S.add("sp", lambda e: e.dma_start(out=gq[:], in_=self.bcast_rows("mla_q_norm_g", l * 192, 192)), writes=["gq"], dma=True)
        S.add("sp", lambda e: e.dma_start(out=gk[:], in_=self.bcast_rows("mla_k_norm_g", l * 192, 128)), writes=["gk"], dma=True)
        S.add("pool", lambda e: e.tensor_scalar_mul(gq[:], gq[:], 192.0 ** -0.5), reads=["gq"], writes=["gq"])
        S.add("sp", lambda e: e.dma_start(out=rml[:], in_=self.ap("rope_mla").rearrange("(t p) c -> p t c", p=128)), writes=["rml"], dma=True)
        pipeu = Pipe(2)

        def make_q(t):
            i = 0
            tok = slice(t * 128, (t + 1) * 128)
            s3 = st[i][:, 0:1536].rearrange("p (h d) -> p h d", h=8)
            q3 = sq[:, 0:1536].rearrange("p (h d) -> p h d", h=8)
            b3 = qb[i][:, 0:1536].rearrange("p (h d) -> p h d", h=8)

            def s0():
                    for cb in range(3):
                        for k in range(4):
                            S.add("pe", lambda e, cb=cb, k=k: e.matmul(pq[cb][:], cqT[:, k, tok], wuq[:, k, cb * 512:(cb + 1) * 512],
                                                                       start=(k == 0), stop=(k == 3)),
                                  reads=["cqT", "wuq"], writes=[("pq", cb)])
                        eng = ("act", "dve", "act")[cb]
                        if eng == "act":
                            S.add("act", lambda e, cb=cb: e.copy(out=st[i][:, cb * 512:(cb + 1) * 512], in_=pq[cb][:]),
                                  reads=[("pq", cb)], writes=[("st", i)])
                        else:
                            S.add("dve", lambda e, cb=cb: e.tensor_copy(out=st[i][:, cb * 512:(cb + 1) * 512], in_=pq[cb][:]),
                                  reads=[("pq", cb)], writes=[("st", i)])
                    s3 = st[i][:, 0:1536].rearrange("p (h d) -> p h d", h=8)
                    q3 = sq[:, 0:1536].rearrange("p (h d) -> p h d", h=8)
                    b3 = qb[i][:, 0:1536].rearrange("p (h d) -> p h d", h=8)
                    S.add("pool", lambda e: e.tensor_tensor(out=sq[:, 0:1536], in0=st[i][:, 0:1536], in1=st[i][:, 0:1536], op=ALU.mult),
                          reads=[("st", i)], writes=["sq"])
                    S.add("dve", lambda e: e.tensor_reduce(out=ssq[:, 0:8], in_=q3[:, :, 0:128], axis=AX.X, op=ALU.add), reads=["sq"], writes=["ssq"])
                    S.add("dve", lambda e: e.tensor_reduce(out=ssq[:, 8:16], in_=q3[:, :, 128:192], axis=AX.X, op=ALU.add), reads=["sq"], writes=["ssq"])
                    self.rstd(S, ssq[:, 0:8], "ssq", 128)
                    self.rstd(S, ssq[:, 8:16], "ssq", 64)
                    S.add("pool", lambda e: e.tensor_tensor(out=s3[:, :, 0:128], in0=s3[:, :, 0:128],
                                                            in1=ssq[:, 0:8].unsqueeze(2).to_broadcast([128, 8, 128]), op=ALU.mult),
                          reads=[("st", i), "ssq"], writes=[("st", i)])
                    S.add("pool", lambda e: e.tensor_tensor(out=s3[:, :, 128:192], in0=s3[:, :, 128:192],
                                                            in1=ssq[:, 8:16].unsqueeze(2).to_broadcast([128, 8, 64]), op=ALU.mult),
                          reads=[("st", i), "ssq"], writes=[("st", i)])
                    S.add("dve", lambda e: e.tensor_tensor(out=s3, in0=s3, in1=gq[:].unsqueeze(1).to_broadcast([128, 8, 192]), op=ALU.mult),
                          reads=[("st", i), "gq"], writes=[("st", i)])
                    S.add("act", lambda e: e.copy(out=b3[:, :, 0:128], in_=s3[:, :, 0:128]), reads=[("st", i)], writes=[("qb", i)])
                    x1 = s3[:, :, 128:160]
                    x2 = s3[:, :, 160:192]
                    cb_ = rml[:, t, 0:32].unsqueeze(1).to_broadcast([128, 8, 32])
                    sb_ = rml[:, t, 32:64].unsqueeze(1).to_broadcast([128, 8, 32])
                    ta = tmpa[:].rearrange("p (g h) -> p g h", g=8)
                    tb_ = tmpb[:].rearrange("p (g h) -> p g h", g=8)
                    S.add("pool", lambda e: e.tensor_tensor(out=ta, in0=x1, in1=cb_, op=ALU.mult), reads=[("st", i), "rml"], writes=["tmpa"])
                    S.add("pool", lambda e: e.tensor_tensor(out=tb_, in0=x2, in1=sb_, op=ALU.mult), reads=[("st", i), "rml"], writes=["tmpb"])
                    S.add("dve", lambda e: e.tensor_tensor(out=b3[:, :, 128:160], in0=ta, in1=tb_, op=ALU.subtract), reads=["tmpa", "tmpb"], writes=[("qb", i)])
                    S.add("pool", lambda e: e.tensor_tensor(out=ta, in0=x1, in1=sb_, op=ALU.mult), reads=[("st", i), "rml"], writes=["tmpa"])
                    S.add("pool", lambda e: e.tensor_tensor(out=tb_, in0=x2, in1=cb_, op=ALU.mult), reads=[("st", i), "rml"], writes=["tmpb"])
                    S.add("dve", lambda e: e.tensor_tensor(out=b3[:, :, 160:192], in0=ta, in1=tb_, op=ALU.add), reads=["tmpa", "tmpb"], writes=[("qb", i)])

            def s1():
                    for h in range(8):
                        S.add("pe", lambda e, h=h: e.transpose(out=pt[0][:, h, :], in_=qb[i][:, h * 192:h * 192 + 128], identity=self.ident_b[:]),
                              reads=[("qb", i)], writes=[("pt", 0)])
                    for h in range(8):
                        S.add("pe", lambda e, h=h: e.transpose(out=pt[1][0:64, h, :], in_=qb[i][:, h * 192 + 128:(h + 1) * 192], identity=self.ident_b[:]),
                              reads=[("qb", i)], writes=[("pt", 1)])
                    S.add("act", lambda e: e.copy(out=trn[i][:], in_=pt[0][:]), reads=[("pt", 0)], writes=[("trn", i)])
                    S.add("dve", lambda e: e.tensor_copy(out=trr[i][:], in_=pt[1][0:64, :, :]), reads=[("pt", 1)], writes=[("trr", i)])
                    S.add("sp", lambda e: e.dma_start(out=self.ap("aqTn")[:, :, tok].rearrange("g p n -> p g n"), in_=trn[i][:]), reads=[("trn", i)], dma=True)
                    S.add("sp", lambda e: e.dma_start(out=self.ap("aqTr")[:, :, tok].rearrange("g p n -> p g n"), in_=trr[i][:]), reads=[("trr", i)], dma=True)

            return [s0, s1]

        def make_kv(t):
            i = 1
            tok = slice(t * 128, (t + 1) * 128)
            k3 = st[i][:].rearrange("p (h d) -> p h d", h=8)
            kq3 = sq[:].rearrange("p (h d) -> p h d", h=8)
            kb3 = qb[i][:].rearrange("p (h d) -> p h d", h=8)

            def s0():
                    for cb in range(4):
                        for k in range(2):
                            S.add("pe", lambda e, cb=cb, k=k: e.matmul(pq[cb][:], ckvT[:, k, tok], wukv[:, k, cb * 512:(cb + 1) * 512],
                                                                       start=(k == 0), stop=(k == 1)),
                                  reads=["ckvT", "wukv"], writes=[("pq", cb)])
                        if cb % 2 == 0:
                            S.add("act", lambda e, cb=cb: e.copy(out=st[i][:, cb * 512:(cb + 1) * 512], in_=pq[cb][:]),
                                  reads=[("pq", cb)], writes=[("st", i)])
                        else:
                            S.add("dve", lambda e, cb=cb: e.tensor_copy(out=st[i][:, cb * 512:(cb + 1) * 512], in_=pq[cb][:]),
                                  reads=[("pq", cb)], writes=[("st", i)])
                    k3 = st[i][:].rearrange("p (h d) -> p h d", h=8)
                    kq3 = sq[:].rearrange("p (h d) -> p h d", h=8)
                    kb3 = qb[i][:].rearrange("p (h d) -> p h d", h=8)
                    S.add("pool", lambda e: e.tensor_tensor(out=kq3[:, :, 0:128], in0=k3[:, :, 0:128], in1=k3[:, :, 0:128], op=ALU.mult),
                          reads=[("st", i)], writes=["sq"])
                    S.add("dve", lambda e: e.tensor_reduce(out=ssq[:, 0:8], in_=kq3[:, :, 0:128], axis=AX.X, op=ALU.add), reads=["sq"], writes=["ssq"])
                    self.rstd(S, ssq[:, 0:8], "ssq", 128)
                    S.add("pool", lambda e: e.tensor_tensor(out=k3[:, :, 0:128], in0=k3[:, :, 0:128],
                                                            in1=ssq[:, 0:8].unsqueeze(2).to_broadcast([128, 8, 128]), op=ALU.mult),
                          reads=[("st", i), "ssq"], writes=[("st", i)])
                    S.add("dve", lambda e: e.tensor_tensor(out=kb3[:, :, 0:128], in0=k3[:, :, 0:128],
                                                           in1=gk[:].unsqueeze(1).to_broadcast([128, 8, 128]), op=ALU.mult),
                          reads=[("st", i), "gk"], writes=[("qb", i)])
                    S.add("act", lambda e: e.copy(out=kb3[:, :, 128:256], in_=k3[:, :, 128:256]), reads=[("st", i)], writes=[("qb", i)])

            def s1():
                    for h in range(8):
                        S.add("pe", lambda e, h=h: e.transpose(out=pt[2][:, h, :], in_=qb[i][:, h * 256:h * 256 + 128], identity=self.ident_b[:]),
                              reads=[("qb", i)], writes=[("pt", 2)])
                    S.add("act", lambda e: e.copy(out=trn[i][:], in_=pt[2][:]), reads=[("pt", 2)], writes=[("trn", i)])
                    S.add("sp", lambda e: e.dma_start(out=self.ap("akT")[:, :, tok].rearrange("g p n -> p g n"), in_=trn[i][:]), reads=[("trn", i)], dma=True)
                    S.add("sp", lambda e: e.dma_start(out=self.ap("av")[tok, :].rearrange("p (h d) -> p h d", h=8), in_=kb3[:, :, 128:256]),
                          reads=[("qb", i)], dma=True)

            return [s0, s1]

        for t in range(NT):
            pipeu.push(make_q(t))
            pipeu.push(make_kv(t))
        pipeu.drain()
        S.emit()
        S.close()

    def phase_attn(self, l, kind, with_ctx, heads=None):
        nc = self.nc
        S = Sched(nc)
        lam_init = 0.8 - 0.6 * math.exp(-0.3 * l)
        if kind == 0:
            nh, dv, nset = 4, 256, 2
        else:
            nh, dv, nset = 8, 128, 1
        qT = [[S.sb("aq%d_%d" % (i, s), [128, NTOK], BF16) for s in range(nset)] for i in range(2)]
        kT = [[S.sb("ak%d_%d" % (i, s), [128, NTOK], BF16) for s in range(nset)] for i in range(2)]
        if kind == 1:
            qTr = [S.sb("aqr%d" % i, [64, NTOK], BF16) for i in range(2)]
            kTr = S.sb("akr", [64, NTOK], BF16)
            S.add("sp", lambda e: e.dma_start(out=kTr[:], in_=self.ap("akrT")), writes=["kTr"], dma=True)
        va = [S.sb("va%d" % i, [128, NT, dv + 1], BF16) for i in range(2)]
        psS = [S.ps("psS%d" % i, [128, 512], F32) for i in range(3)]
        psO = [S.ps("psO%d" % i, [128, 512], F32) for i in range(4)]
        ptr = S.ps("ptr", [128, 8, 128], BF16)
        Pb = [S.sb("Pb%d" % i, [128, 512], BF16) for i in range(3)]
        rr = S.sb("rr", [128, 4], F32)
        t1 = S.sb("t1", [128, 256], F32)
        ob = S.sb("ob", [128, 256], F32)
        obb = [S.sb("obb%d" % i, [128, 256], BF16) for i in range(2)]
        ssn = S.sb("ssn", [128, 1], F32)
        junk = S.sb("junka", [128, 256], BF16)
        yst = [S.sb("yst%d" % i, [128, 2, 128], BF16) for i in range(2)]
        for i in range(2):
            S.add("pool", lambda e, i=i: e.memset(va[i][:, :, dv:dv + 1], 1.0), writes=[("va", i)])
        S.add("pool", lambda e: e.memset(ssn[:], 0.0), writes=["ssn"])
        if kind == 0:
            lamb = S.sb("lamb", [128, 512], F32)
            lamt = S.sb("lamt", [128, 256], F32)
            lamc = S.sb("lamc", [128, 2], F32)
            gsub = S.sb("gsub", [128, 256], F32)
            S.add("sp", lambda e: e.dma_start(out=lamb[:], in_=self.bcast_rows("da_lambda", l * 512, 512)), writes=["lamb"], dma=True)
            S.add("sp", lambda e: e.dma_start(out=gsub[:], in_=self.bcast_rows("da_subln_g", l * 256, 256)), writes=["gsub"], dma=True)
            S.add("pool", lambda e: e.tensor_scalar_mul(gsub[:], gsub[:], float(1.0 - lam_init)), reads=["gsub"], writes=["gsub"])
            l4 = lamb[:].rearrange("p (a b d) -> p a b d", a=2, b=2)
            S.add("dve", lambda e: e.tensor_tensor(out=lamt[:].rearrange("p (a d) -> p a d", a=2), in0=l4[:, :, 0, :], in1=l4[:, :, 1, :], op=ALU.mult),
                  reads=["lamb"], writes=["lamt"])
            S.add("dve", lambda e: e.tensor_reduce(out=lamc[:], in_=lamt[:].rearrange("p (a d) -> p a d", a=2), axis=AX.X, op=ALU.add),
                  reads=["lamt"], writes=["lamc"])
            S.add("act", lambda e: e.activation(out=lamc[:], in_=lamc[:], func=AF.Exp), reads=["lamc"], writes=["lamc"])
            S.add("dve", lambda e: e.tensor_tensor(out=lamc[:, 0:1], in0=lamc[:, 1:2], in1=lamc[:, 0:1], op=ALU.subtract), reads=["lamc"], writes=["lamc"])
            S.add("dve", lambda e: e.tensor_scalar_add(lamc[:, 0:1], lamc[:, 0:1], float(-lam_init)), reads=["lamc"], writes=["lamc"])
        st = {"P": 0, "O": 0, "y": 0}
        QBL = 256 if kind == 0 else 512
        qblocks = [(256 + j * QBL, QBL, [t for t in range(NT)]) for j in range(2048 // QBL)]
        if with_ctx:
            qblocks.append((0, 256, [0, 1]))
        for h in (range(nh) if heads is None else heads):
            i = h % 2
            for s in range(nset):
                if kind == 0:
                    qsrc = self.ap("dqT")[2 * h + s]
                    ksrc = self.ap("dkT")[2 * h + s]
                else:
                    qsrc = self.ap("aqTn")[h]
                    ksrc = self.ap("akT")[h]
                S.add("sp", lambda e, s=s, qsrc=qsrc: e.dma_start(out=qT[i][s][:], in_=qsrc), writes=[("qT", i, s)], dma=True)
                S.add("sp", lambda e, s=s, ksrc=ksrc: e.dma_start(out=kT[i][s][:], in_=ksrc), writes=[("kT", i, s)], dma=True)
            if kind == 1:
                S.add("sp", lambda e: e.dma_start(out=qTr[i][:], in_=self.ap("aqTr")[h]), writes=[("qTr", i)], dma=True)
            vsrc = self.ap("dv" if kind == 0 else "av")[:, h * dv:(h + 1) * dv].rearrange("(t p) d -> p t d", p=128)
            S.add("sp", lambda e, vsrc=vsrc: e.dma_start(out=va[i][:, :, 0:dv], in_=vsrc), writes=[("va", i)], dma=True)
            for (q0, QB, ktiles) in qblocks:
                nsub = QB // 128
                if kind == 0:
                    Oi = lambda s, sub: s * 2 + sub
                else:
                    Oi = lambda s, sub: sub
                pidx = []
                for n in range(len(ktiles)):
                    pidx.append((st["P"] % 3, st["P"] % 3))
                    st["P"] += 1

                def scores(n):
                    ps_i, pb = pidx[n]
                    kt = ktiles[n]
                    ks = slice(kt * 128, (kt + 1) * 128)
                    for s in range(nset):
                        if kind == 0:
                            S.add("pe", lambda e, s=s: e.matmul(psS[ps_i][:, s * QB:(s + 1) * QB], kT[i][s][:, ks], qT[i][s][:, q0:q0 + QB],
                                                                start=True, stop=True),
                                  reads=[("kT", i, s), ("qT", i, s)], writes=[("psS", ps_i, s)])
                        else:
                            S.add("pe", lambda e: e.matmul(psS[ps_i][:, 0:QB], kT[i][0][:, ks], qT[i][0][:, q0:q0 + QB], start=True, stop=False),
                                  reads=[("kT", i, 0), ("qT", i, 0)], writes=[("psS", ps_i, 0)])
                            S.add("pe", lambda e: e.matmul(psS[ps_i][:, 0:QB], kTr[:, ks], qTr[i][:, q0:q0 + QB], start=False, stop=True),
                                  reads=["kTr", ("qTr", i)], writes=[("psS", ps_i, 0)])

                scores(0)
                if len(ktiles) > 1:
                    scores(1)
                for n, kt in enumerate(ktiles):
                    ps_i, pb = pidx[n]
                    if n + 2 < len(ktiles):
                        scores(n + 2)
                    S.add("act", lambda e: e.activation(out=Pb[pb][:, 0:nset * QB], in_=psS[ps_i][:, 0:nset * QB], func=AF.Exp),
                          reads=[("psS", ps_i, s) for s in range(nset)], writes=[("Pb", pb)])
                    for s in range(nset):
                        for sub in range(nsub):
                            o = Oi(s, sub)
                            S.add("pe", lambda e, s=s, sub=sub, o=o: e.matmul(psO[o][:, 0:dv + 1], Pb[pb][:, s * QB + sub * 128:s * QB + (sub + 1) * 128],
                                                                              va[i][:, kt, :], start=(n == 0), stop=(n == len(ktiles) - 1)),
                                  reads=[("Pb", pb), ("va", i)], writes=[("psO", o)])
                for sub in range(nsub):
                    tq = q0 + sub * 128
                    y = st["y"] % 2
                    st["y"] += 1
                    if kind == 0:
                        o1, o2 = Oi(0, sub), Oi(1, sub)
                        S.add("dve", lambda e: e.reciprocal(out=rr[:, 0:1], in_=psO[o1][:, dv:dv + 1]), reads=[("psO", o1)], writes=["rr"])
                        S.add("dve", lambda e: e.reciprocal(out=rr[:, 1:2], in_=psO[o2][:, dv:dv + 1]), reads=[("psO", o2)], writes=["rr"])
                        S.add("dve", lambda e: e.tensor_tensor(out=rr[:, 1:2], in0=rr[:, 1:2], in1=lamc[:, 0:1], op=ALU.mult),
                              reads=["rr", "lamc"], writes=["rr"])
                        S.add("act", lambda e: e.activation(out=t1[:], in_=psO[o1][:, 0:dv], func=AF.Copy, scale=rr[:, 0:1]),
                              reads=[("psO", o1), "rr"], writes=["t1"])
                        S.add("dve", lambda e: e.scalar_tensor_tensor(out=ob[:], in0=psO[o2][:, 0:dv], scalar=rr[:, 1:2], in1=t1[:],
                                                                      op0=ALU.mult, op1=ALU.add),
                              reads=[("psO", o2), "rr", "t1"], writes=["ob"])
                        S.add("act", lambda e: e.activation(out=junk[:], in_=ob[:], func=AF.Square, accum_out=ssn[:, 0:1]),
                              reads=["ob", "ssn"], writes=["junk", "ssn"])
                        self.rstd(S, ssn[:, 0:1], "ssn", 256)
                        S.add("dve", lambda e: e.scalar_tensor_tensor(out=obb[y][:], in0=ob[:], scalar=ssn[:, 0:1], in1=gsub[:],
                                                                      op0=ALU.mult, op1=ALU.mult),
                              reads=["ob", "ssn", "gsub"], writes=[("obb", y)])
                        S.add("pool", lambda e: e.memset(ssn[:], 0.0), reads=["ssn"], writes=["ssn"])
                        nch = 2
                    else:
                        o1 = Oi(0, sub)
                        S.add("dve", lambda e: e.reciprocal(out=rr[:, 0:1], in_=psO[o1][:, dv:dv + 1]), reads=[("psO", o1)], writes=["rr"])
                        S.add("act", lambda e: e.activation(out=obb[y][:, 0:128], in_=psO[o1][:, 0:dv], func=AF.Copy, scale=rr[:, 0:1]),
                              reads=[("psO", o1), "rr"], writes=[("obb", y)])
                        nch = 1
                    for c in range(nch):
                        S.add("pe", lambda e, c=c: e.transpose(out=ptr[:, c, :], in_=obb[y][:, c * 128:(c + 1) * 128], identity=self.ident_b[:]),
                              reads=[("obb", y)], writes=["ptr"])
                    S.add("dve", lambda e: e.tensor_copy(out=yst[y][:, 0:nch, :], in_=ptr[:, 0:nch, :]), reads=["ptr"], writes=[("yst", y)])
                    br = 1 if kind == 0 else 2
                    S.add("sp", lambda e: e.dma_start(out=self.ap("ysT")[br, h * nch:(h + 1) * nch, :, tq:tq + 128].rearrange("c p n -> p c n"),
                                                      in_=yst[y][:, 0:nch, :]), reads=[("yst", y)], dma=True)
        S.emit()
        S.close()

    def phase_mlstm(self, l, heads=None):
        nc = self.nc
        NEG = {}
        pos_of = {0: lambda t: t, 1: lambda t: (1 - t) if t < 2 else 2 + (NT - 1 - t)}
        tile_at = {0: lambda p: p, 1: lambda p: (1 - p) if p < 2 else NT - 1 - (p - 2)}
        g2 = contextlib.ExitStack()
        UT = [g2.enter_context(nc.sbuf_tensor("UT%d_%d" % (l, d), [128, NT, 12], F32)) for d in range(2)]
        KB = [g2.enter_context(nc.sbuf_tensor("KB%d_%d" % (l, d), [128, 4, NT], F32)) for d in range(2)]
        S = Sched(nc)
        cj = S.sb("cj", [128, 128], F32)
        sel = S.sb("sel", [4, 512], F32)
        G = S.sb("G", [128, NT, 16], F32)
        IG = S.sb("IG", [128, NT, 8], F32)
        LF = S.sb("LF", [128, NT, 8], F32)
        ones = S.sb("ones", [128, 1], F32)
        zr = S.sb("zr", [4, NTOK], F32)
        S.add("sp", lambda e: e.dma_start(out=cj[:], in_=self.ap("consts")[:, 128:256]), writes=["cj"], dma=True)
        S.add("sp", lambda e: e.dma_start(out=sel[:], in_=self.ap("consts2")), writes=["sel"], dma=True)
        S.add("sp", lambda e: e.dma_start(out=G[:], in_=self.ap("mg").rearrange("(t p) c -> p t c", p=128)), writes=["G"], dma=True)
        S.add("pool", lambda e: e.memset(ones[:], 1.0), writes=["ones"])
        S.add("pool", lambda e: e.memset(zr[:], 0.0), writes=["zr"])
        G4 = G[:].rearrange("p t (d x c) -> p t d x c", d=2, x=2)
        IG3 = IG[:].rearrange("p t (d c) -> p t d c", d=2)
        LF3 = LF[:].rearrange("p t (d c) -> p t d c", d=2)
        for d in range(2):
            S.add("dve", lambda e, d=d: e.tensor_copy(out=IG3[:, :, d, :], in_=G4[:, :, d, 0, :]), reads=["G"], writes=["IG"])
            S.add("act", lambda e, d=d: e.activation(out=LF3[:, :, d, :], in_=G4[:, :, d, 1, :], func=AF.Exp, scale=-1.0), reads=["G"], writes=["LF"])
        S.add("act", lambda e: e.activation(out=LF[:], in_=LF[:], func=AF.Ln, bias=ones[:, 0:1]), reads=["LF", "ones"], writes=["LF"])
        S.add("dve", lambda e: e.tensor_scalar_mul(LF[:], LF[:], -1.0), reads=["LF"], writes=["LF"])
        prow = [S.ps("prow%d" % i, [128, 512], F32) for i in range(2)]
        rows = {}
        for d in range(2):
            for nm in ("I", "L", "F", "A", "M", "T", "U", "U2", "E"):
                rows[(nm, d)] = S.sb("row%s%d" % (nm, d), [4, NTOK], F32)
        R = [S.sb("R%d" % d, [4, NT + 1], F32) for d in range(2)]
        KP = [S.sb("KP%d" % d, [4, NT], F32) for d in range(2)]
        cnt = 0
        for d in range(2):
            rm = self.ident_f if d == 0 else cj
            for src, nm in ((IG, "I"), (LF, "L")):
                for p0 in range(0, NT, 4):
                    pr = prow[cnt % 2]
                    cnt += 1
                    n = min(4, NT - p0)
                    for pp in range(n):
                        t = tile_at[d](p0 + pp)
                        S.add("pe", lambda e, pr=pr, pp=pp, t=t, src=src, rm=rm, d=d: e.matmul(
                            pr[0:4, pp * 128:(pp + 1) * 128], src[:, t, 4 * d:4 * d + 4], rm[:], start=True, stop=True),
                            reads=["IG", "LF", "cj"], writes=[("prow", id(pr))])
                    S.add("dve", lambda e, pr=pr, p0=p0, n=n, nm=nm, d=d: e.tensor_copy(out=rows[(nm, d)][:, p0 * 128:(p0 + n) * 128], in_=pr[0:4, 0:n * 128]),
                          reads=[("prow", id(pr))], writes=[("row", nm, d)])
            r = lambda nm: rows[(nm, d)]
            k = lambda nm: ("row", nm, d)
            v3 = lambda nm: rows[(nm, d)][:].rearrange("c (t n) -> c t n", n=128)
            S.add("dve", lambda e, d=d: e.tensor_tensor_scan(out=r("F")[:], data0=r("L")[:], data1=zr[:], initial=0.0, op0=ALU.add, op1=ALU.add),
                  reads=[k("L"), "zr"], writes=[k("F")])
            S.add("dve", lambda e, d=d: e.tensor_tensor(out=r("A")[:], in0=r("I")[:], in1=r("F")[:], op=ALU.subtract), reads=[k("I"), k("F")], writes=[k("A")])
            S.add("dve", lambda e, d=d: e.tensor_tensor_scan(out=r("M")[:], data0=r("A")[:], data1=r("A")[:], initial=0.0, op0=ALU.max, op1=ALU.max),
                  reads=[k("A")], writes=[k("M")])
            S.add("dve", lambda e, d=d: e.memset(R[d][:, 0:1], 0.0), writes=[("R", d)])
            S.add("dve", lambda e, d=d: e.tensor_copy(out=R[d][:, 1:NT + 1], in_=v3("M")[:, :, 127]), reads=[k("M")], writes=[("R", d)])
            Rc = R[d][:, 0:NT].unsqueeze(2).to_broadcast([4, NT, 128])
            Rn = R[d][:, 1:NT + 1].unsqueeze(2).to_broadcast([4, NT, 128])
            S.add("dve", lambda e, d=d: e.tensor_tensor(out=v3("T"), in0=v3("A"), in1=Rc, op=ALU.subtract), reads=[k("A"), ("R", d)], writes=[k("T")])
            S.add("act", lambda e, d=d: e.activation(out=r("U")[:], in_=r("T")[:], func=AF.Exp), reads=[k("T")], writes=[k("U")])
            S.add("dve", lambda e, d=d: e.tensor_tensor(out=v3("T"), in0=v3("A"), in1=Rn, op=ALU.subtract), reads=[k("A"), ("R", d), k("U")], writes=[k("T")])
            S.add("act", lambda e, d=d: e.activation(out=r("U2")[:], in_=r("T")[:], func=AF.Exp), reads=[k("T")], writes=[k("U2")])
            S.add("dve", lambda e, d=d: e.tensor_tensor(out=v3("T"), in0=v3("F"), in1=Rc, op=ALU.add), reads=[k("F"), ("R", d), k("U2")], writes=[k("T")])
            S.add("act", lambda e, d=d: e.activation(out=r("E")[:], in_=r("T")[:], func=AF.Exp, scale=-1.0), reads=[k("T")], writes=[k("E")])
            S.add("dve", lambda e, d=d: e.tensor_tensor(out=KP[d][:], in0=R[d][:, 0:NT], in1=R[d][:, 1:NT + 1], op=ALU.subtract), reads=[("R", d)], writes=[("KP", d)])
            S.add("act", lambda e, d=d: e.activation(out=KP[d][:], in_=KP[d][:], func=AF.Exp), reads=[("KP", d)], writes=[("KP", d)])
            pk = prow[cnt % 2]
            cnt += 1
            for c in range(4):
                S.add("pe", lambda e, c=c, pk=pk, d=d: e.matmul(pk[:, c * NT:(c + 1) * NT], sel[:, c * 128:(c + 1) * 128], KP[d][:], start=True, stop=True),
                      reads=["sel", ("KP", d)], writes=[("prow", id(pk))])
            S.add("dve", lambda e, pk=pk, d=d: e.tensor_copy(out=KB[d][:].rearrange("p c t -> p (c t)"), in_=pk[:, 0:4 * NT]),
                  reads=[("prow", id(pk))], writes=[("KB", d)])
            xs = [S.sb("xs%d_%d" % (d, i), [128, 12], F32) for i in range(2)]
            for p in range(NT):
                t = tile_at[d](p)
                pr = prow[cnt % 2]
                cnt += 1
                for qi, nm in enumerate(("U", "U2", "E")):
                    S.add("pe", lambda e, pr=pr, qi=qi, nm=nm, p=p, d=d: e.matmul(pr[:, qi * 4:(qi + 1) * 4], rows[(nm, d)][:, p * 128:(p + 1) * 128],
                                                                               self.ident_f[0:4, 0:4], start=True, stop=True),
                          reads=[("row", nm, d)], writes=[("prow", id(pr))])
                if d == 0:
                    S.add("dve", lambda e, pr=pr, t=t: e.tensor_copy(out=UT[0][:, t, :], in_=pr[:, 0:12]), reads=[("prow", id(pr))], writes=[("UT", 0)])
                else:
                    x = xs[p % 2]
                    S.add("dve", lambda e, pr=pr, x=x: e.tensor_copy(out=x[:], in_=pr[:, 0:12]), reads=[("prow", id(pr))], writes=[("xs", p % 2)])
                    pr2 = prow[cnt % 2]
                    cnt += 1
                    S.add("pe", lambda e, pr2=pr2, x=x: e.matmul(pr2[:, 0:12], cj[:], x[:], start=True, stop=True),
                          reads=[("xs", p % 2), "cj"], writes=[("prow", id(pr2))])
                    S.add("dve", lambda e, pr2=pr2, t=t: e.tensor_copy(out=UT[1][:, t, :], in_=pr2[:, 0:12]), reads=[("prow", id(pr2))], writes=[("UT", 1)])
        S.emit()
        S.close()

        S = Sched(nc)
        mask = [S.sb("mask%d" % d, [128, 128], F32) for d in range(2)]
        S.add("sp", lambda e: e.dma_start(out=mask[0][:], in_=self.ap("consts")[:, 256:384]), writes=["mask"], dma=True)
        S.add("sp", lambda e: e.dma_start(out=mask[1][:], in_=self.ap("consts")[:, 384:512]), writes=["mask"], dma=True)
        qT = [S.sb("mq%d" % i, [128, 2, NTOK], BF16) for i in range(2)]
        kT = [S.sb("mk%d" % i, [128, 2, NTOK], BF16) for i in range(2)]
        ktm = [S.sb("mkt%d" % i, [128, NT, 256], BF16) for i in range(2)]
        va = [S.sb("mva%d" % i, [128, NT, 257], BF16) for i in range(2)]
        for i in range(2):
            S.add("pool", lambda e, i=i: e.memset(va[i][:, :, 256:257], 1.0), writes=[("va", i)])
        psKQ = [S.ps("psKQ%d" % d, [128, 512], F32) for d in range(2)]
        psND = [S.ps("psND%d" % d, [128, 512], F32) for d in range(2)]
        psD = [[S.ps("psD%d_%d" % (d, c), [128, 512], F32) for c in range(2)] for d in range(2)]
        Wm = [[S.sb("Wm%d_%d" % (d, i), [128, 128], BF16) for i in range(2)] for d in range(2)]
        gv = [[S.sb("gv%d_%d" % (d, i), [128, 257], BF16) for i in range(2)] for d in range(2)]
        gv2 = [[S.sb("gw%d_%d" % (d, i), [128, 257], BF16) for i in range(2)] for d in range(2)]
        S32 = [[S.sb("S32_%d_%d" % (d, c), [128, 257], F32) for c in range(2)] for d in range(2)]
        Sbf = [[S.sb("Sbf_%d_%d" % (d, c), [128, 257], BF16) for c in range(2)] for d in range(2)]
        dn = [S.sb("dn%d" % d, [128, 1], F32) for d in range(2)]
        ho = [[S.sb("ho%d_%d" % (d, i), [128, 256], F32) for i in range(2)] for d in range(2)]
        hdst = ["hmf", "hmb"]
        for h in (range(4) if heads is None else heads):
            i = h % 2
            S.add("sp", lambda e: e.dma_start(out=qT[i][:], in_=self.ap("mqkT")[2 * h:2 * h + 2].rearrange("c p n -> p c n")), writes=[("qT", i)], dma=True)
            S.add("sp", lambda e: e.dma_start(out=kT[i][:], in_=self.ap("mqkT")[8 + 2 * h:10 + 2 * h].rearrange("c p n -> p c n")), writes=[("kT", i)], dma=True)
            S.add("sp", lambda e: e.dma_start(out=ktm[i][:], in_=self.ap("mk_tm")[:, h * 256:(h + 1) * 256].rearrange("(t p) c -> p t c", p=128)),
                  writes=[("ktm", i)], dma=True)
            S.add("sp", lambda e: e.dma_start(out=va[i][:, :, 0:256], in_=self.ap("mv")[:, h * 256:(h + 1) * 256].rearrange("(t p) c -> p t c", p=128)),
                  writes=[("va", i)], dma=True)
            for p in range(NT):
                for d in range(2):
                    t = tile_at[d](p)
                    tok = slice(t * 128, (t + 1) * 128)
                    j = p % 2
                    for c in range(2):
                        S.add("pe", lambda e, c=c: e.matmul(psKQ[d][:, 0:128], kT[i][:, c, tok], qT[i][:, c, tok], start=(c == 0), stop=(c == 1)),
                              reads=[("kT", i), ("qT", i)], writes=[("psKQ", d)])
                    S.add("dve", lambda e: e.tensor_tensor(out=Wm[d][j][:], in0=psKQ[d][:, 0:128], in1=mask[d][:], op=ALU.mult),
                          reads=[("psKQ", d), "mask"], writes=[("Wm", d, j)])
                    S.add("act", lambda e: e.activation(out=gv[d][j][:], in_=va[i][:, t, :], func=AF.Copy, scale=UT[d][:, t, h:h + 1]),
                          reads=[("va", i)], writes=[("gv", d, j)])
                    if p < NT - 1:
                        S.add("act", lambda e: e.activation(out=gv2[d][j][:], in_=va[i][:, t, :], func=AF.Copy, scale=UT[d][:, t, 4 + h:5 + h]),
                              reads=[("va", i)], writes=[("gv2", d, j)])
                    first = True
                    if p > 0:
                        for c in range(2):
                            S.add("pe", lambda e, c=c, first=first: e.matmul(psND[d][:, 0:257], qT[i][:, c, tok], Sbf[d][c][:], start=first, stop=False),
                                  reads=[("qT", i), ("Sbf", d, c)], writes=[("psND", d)])
                            first = False
                    S.add("pe", lambda e, first=first: e.matmul(psND[d][:, 0:257], Wm[d][j][:], gv[d][j][:], start=first, stop=True),
                          reads=[("Wm", d, j), ("gv", d, j)], writes=[("psND", d)])
                    S.add("act", lambda e: e.activation(out=dn[d][:], in_=psND[d][:, 256:257], func=AF.Abs), reads=[("psND", d)], writes=[("dn", d)])
                    S.add("dve", lambda e: e.tensor_tensor(out=dn[d][:], in0=dn[d][:], in1=UT[d][:, t, 8 + h:9 + h], op=ALU.max),
                          reads=[("dn", d)], writes=[("dn", d)])
                    S.add("dve", lambda e: e.reciprocal(out=dn[d][:], in_=dn[d][:]), reads=[("dn", d)], writes=[("dn", d)])
                    S.add("act", lambda e: e.activation(out=ho[d][j][:], in_=psND[d][:, 0:256], func=AF.Copy, scale=dn[d][:, 0:1]),
                          reads=[("psND", d), ("dn", d)], writes=[("ho", d, j)])
                    S.add("sp", lambda e: e.dma_start(out=self.ap(hdst[d])[tok, h * 256:(h + 1) * 256], in_=ho[d][j][:]), reads=[("ho", d, j)], dma=True)
                    if p < NT - 1:
                        for c in range(2):
                            S.add("pe", lambda e, c=c: e.matmul(psD[d][c][:, 0:257], ktm[i][:, t, c * 128:(c + 1) * 128], gv2[d][j][:], start=True, stop=True),
                                  reads=[("ktm", i), ("gv2", d, j)], writes=[("psD", d, c)])
                            if p == 0:
                                S.add("dve", lambda e, c=c: e.tensor_copy(out=S32[d][c][:], in_=psD[d][c][:, 0:257]),
                                      reads=[("psD", d, c)], writes=[("S32", d, c)])
                            else:
                                S.add("dve", lambda e, c=c: e.scalar_tensor_tensor(out=S32[d][c][:], in0=S32[d][c][:], scalar=KB[d][:, h, p:p + 1],
                                                                                   in1=psD[d][c][:, 0:257], op0=ALU.mult, op1=ALU.add),
                                      reads=[("psD", d, c), ("S32", d, c)], writes=[("S32", d, c)])
                            S.add("pool", lambda e, c=c: e.tensor_copy(out=Sbf[d][c][:], in_=S32[d][c][:]), reads=[("S32", d, c)], writes=[("Sbf", d, c)])
        S.emit()
        S.close()

        S = Sched(nc)
        gn = S.sb("gn", [128, 1024], F32)
        S.add("sp", lambda e: e.dma_start(out=gn[:], in_=self.bcast_rows("m_norm_g", l * 1024, 1024)), writes=["gn"], dma=True)
        hf = [S.sb("hf%d" % i, [128, 1024], F32) for i in range(2)]
        hb = [S.sb("hb%d" % i, [128, 1024], F32) for i in range(2)]
        ot = [S.sb("ot%d" % i, [128, 1024], BF16) for i in range(2)]
        sq = S.sb("sqm", [128, 1024], F32)
        s4 = S.sb("s4", [128, 4], F32)
        yb = [S.sb("ybm%d" % i, [128, 1024], BF16) for i in range(2)]
        ptm = S.ps("ptm3", [128, 8, 128], BF16)
        ys = [S.sb("ysm%d" % i, [128, 8, 128], BF16) for i in range(2)]
        pipe3 = Pipe(2)

        def make_item3(t):
            i = t % 2
            tok = slice(t * 128, (t + 1) * 128)
            h3 = hf[i][:].rearrange("p (h d) -> p h d", h=4)

            def s0():
                    S.add("sp", lambda e: e.dma_start(out=hf[i][:], in_=self.ap("hmf")[tok, :]), writes=[("hf", i)], dma=True)
                    S.add("sp", lambda e: e.dma_start(out=hb[i][:], in_=self.ap("hmb")[tok, :]), writes=[("hb", i)], dma=True)
                    S.add("sp", lambda e: e.dma_start(out=ot[i][:], in_=self.ap("mo")[tok, :]), writes=[("ot", i)], dma=True)
                    S.add("dve", lambda e: e.tensor_tensor(out=hf[i][:], in0=hf[i][:], in1=hb[i][:], op=ALU.add), reads=[("hf", i), ("hb", i)], writes=[("hf", i)])
                    S.add("pool", lambda e: e.tensor_tensor(out=sq[:], in0=hf[i][:], in1=hf[i][:], op=ALU.mult), reads=[("hf", i)], writes=["sq"])
                    S.add("dve", lambda e: e.tensor_reduce(out=s4[:], in_=sq[:].rearrange("p (h d) -> p h d", h=4), axis=AX.X, op=ALU.add), reads=["sq"], writes=["s4"])
                    self.rstd(S, s4[:], "s4", 256)
                    h3 = hf[i][:].rearrange("p (h d) -> p h d", h=4)
                    S.add("pool", lambda e: e.tensor_tensor(out=h3, in0=h3, in1=s4[:].unsqueeze(2).to_broadcast([128, 4, 256]), op=ALU.mult),
                          reads=[("hf", i), "s4"], writes=[("hf", i)])
                    S.add("dve", lambda e: e.tensor_tensor(out=hf[i][:], in0=hf[i][:], in1=gn[:], op=ALU.mult), reads=[("hf", i), "gn"], writes=[("hf", i)])
                    S.add("pool", lambda e: e.tensor_tensor(out=yb[i][:], in0=hf[i][:], in1=ot[i][:], op=ALU.mult), reads=[("hf", i), ("ot", i)], writes=[("yb", i)])

            def s1():
                    for c in range(8):
                        S.add("pe", lambda e, c=c: e.transpose(out=ptm[:, c, :], in_=yb[i][:, c * 128:(c + 1) * 128], identity=self.ident_b[:]),
                              reads=[("yb", i)], writes=["ptm"])
                    S.add("act", lambda e: e.copy(out=ys[i][:], in_=ptm[:]), reads=["ptm"], writes=[("ys", i)])
                    S.add("sp", lambda e: e.dma_start(out=self.ap("ysT")[0, :, :, tok].rearrange("c p n -> p c n"), in_=ys[i][:]), reads=[("ys", i)], dma=True)

            return [s0, s1]

        for t in range(NT):
            pipe3.push(make_item3(t))
        pipe3.drain()
        S.emit()
        S.close()
        g2.close()

    def phase_merge_a(self, l, tiles):
        nc = self.nc
        S = Sched(nc)
        ysall = S.sb("ysall", [128, 24, NTOK], BF16)
        for b in range(3):
            S.add("sp", lambda e, b=b: e.dma_start(out=ysall[:, b * 8:(b + 1) * 8, :], in_=self.ap("ysT")[b].rearrange("c p n -> p c n")),
                  writes=[("ys", b)], dma=True)
        wbr = [S.sb("wbr%d" % i, [128, 24, 128], BF16) for i in range(2)]
        gj = [S.sb("gj%d" % i, [128, 3, NTOK], BF16) for i in range(2)]
        mTs = [S.sb("mTs%d" % i, [128, NTOK], BF16) for i in range(2)]
        tmpm = [[S.sb("tmpm%d_%d" % (i, r_), [128, 512], F32) for i in range(3)] for r_ in range(2)]
        accm = [S.sb("accm%d" % r_, [128, 512], F32) for r_ in range(2)]
        py = [S.ps("py%d" % i, [128, 512], F32) for i in range(6)]
        tstart = tiles[0] * 128
        tend = (tiles[-1] + 1) * 128
        blocks = [(t0, min(512, tend - t0)) for t0 in range(tstart, tend, 512)]
        cnt = {"py": 0, "r": 0}
        def load_j(j):
            wi = j % 2
            S.add("pool", lambda e: e.dma_start(out=wbr[wi][:], in_=self.ap("w_branch")[l][:, :, j * 128:(j + 1) * 128].rearrange("b (c p) w -> p (b c) w", p=128)),
                  writes=[("wbr", wi)], dma=True)
            S.add("sp", lambda e: e.dma_start(out=gj[wi][:, :, tstart:tend], in_=self.ap("gT")[:, :, tstart:tend].rearrange("(b j) p n -> j p b n", b=3)[j]),
                  writes=[("gj", wi)], dma=True)

        load_j(0)
        for j in range(NKC):
            wi = j % 2
            if j + 1 < NKC:
                load_j(j + 1)
            for (t0, n) in blocks:
                rr_ = cnt["r"] % 2
                cnt["r"] += 1
                for b in range(3):
                    pi = cnt["py"] % 6
                    cnt["py"] += 1
                    for c in range(8):
                        S.add("pe", lambda e, c=c: e.matmul(py[pi][:, 0:n], wbr[wi][:, b * 8 + c, :], ysall[:, b * 8 + c, t0:t0 + n], start=(c == 0), stop=(c == 7)),
                              reads=[("wbr", wi), ("ys", b)], writes=[("py", pi)])
                    S.add("dve", lambda e: e.tensor_tensor(out=tmpm[rr_][b][:, 0:n], in0=py[pi][:, 0:n], in1=gj[wi][:, b, t0:t0 + n], op=ALU.mult),
                          reads=[("py", pi), ("gj", wi)], writes=[("tmpm", rr_, b)])
                S.add("pool", lambda e: e.tensor_tensor(out=accm[rr_][:, 0:n], in0=tmpm[rr_][0][:, 0:n], in1=tmpm[rr_][1][:, 0:n], op=ALU.add),
                      reads=[("tmpm", rr_, 0), ("tmpm", rr_, 1)], writes=[("accm", rr_)])
                S.add("pool", lambda e: e.tensor_tensor(out=mTs[wi][:, t0:t0 + n], in0=accm[rr_][:, 0:n], in1=tmpm[rr_][2][:, 0:n], op=ALU.add),
                      reads=[("accm", rr_), ("tmpm", rr_, 2)], writes=[("mTs", wi)])
            S.add("sp", lambda e: e.dma_start(out=self.ap("mTd")[j][:, tstart:tend], in_=mTs[wi][:, tstart:tend]), reads=[("mTs", wi)], dma=True)
        S.emit()
        S.close()

    def phase_merge_b(self, l, tiles):
        nc = self.nc
        S = Sched(nc)
        wout = S.sb("wout", [128, NKC, D], BF16)
        for h in range(2):
            S.add("pool", lambda e, h=h: e.dma_start(out=wout[:, h * 8:(h + 1) * 8, :],
                                                     in_=self.ap("w_out")[l][h * 1024:(h + 1) * 1024, :].rearrange("(k p) w -> p k w", p=128)),
                  writes=[("wout", h)], dma=True)
        mods = {}
        for r in (0, 1):
            for seg, add1 in ((2, False), (4, True), (3, False)):
                if r == 1 and tiles[0] >= NT_C:
                    continue
                tl = S.sb("modm_%d_%d" % (r, seg), [128, D], F32)
                S.add("sp", lambda e, tl=tl, r=r, seg=seg: e.dma_start(out=tl[:], in_=self.bcast_rows("modrow", modoff(l, r, seg), D)),
                      writes=[("mod", r, seg)], dma=True)
                if add1:
                    S.add("pool", lambda e, tl=tl: e.tensor_scalar_add(tl[:], tl[:], 1.0), reads=[("mod", r, seg)], writes=[("mod", r, seg)])
                mods[(r, seg)] = tl
        mT = [S.sb("mTb%d" % i, [128, NKC, 128], BF16) for i in range(2)]
        pyo = [S.ps("pyo%d" % i, [128, 512], F32) for i in range(4)]
        pth = [S.ps("pth%d" % i, [128, 8, 128], BF16) for i in range(2)]
        xts = [S.sb("xtm%d" % i, [128, D], F32) for i in range(2)]
        junk = S.sb("junkm", [128, D], BF16)
        xm32 = S.sb("xm32m", [128, D], F32)
        xmb = S.sb("xmbm", [128, D], BF16)
        ssb = S.sb("ssbm", [128, NT], F32)
        hst = [S.sb("hstm%d" % i, [128, NKC, 128], BF16) for i in range(2)]
        S.add("dve", lambda e: e.memset(ssb[:], 0.0), writes=[("ss", t) for t in range(NT)])
        cnt = {"po": 0}
        rw = S.sb("rw", [128, NKC, 16], BF16)
        rb = S.sb("rb", [128, 16], F32)
        S.add("pool", lambda e: e.dma_start(out=rw[:], in_=self.ap("router_w").rearrange("(k p) e -> p k e", p=128)), writes=["rw"], dma=True)
        S.add("sp", lambda e: e.dma_start(out=rb[:], in_=self.bcast_rows("router_bias", 0, 16)), writes=["rb"], dma=True)
        prt = S.ps("prt", [128, 512], F32)
        sc = S.sb("r_sc", [128, 16], F32)
        sel = S.sb("r_sel", [128, 16], F32)
        eq = S.sb("r_eq", [128, 16], F32)
        sm = S.sb("r_sm", [128, 16], F32)
        m1 = S.sb("r_m1", [128, 4], F32)
        m2 = S.sb("r_m2", [128, 4], F32)
        gm = S.sb("r_gm", [128, 2], F32)
        cmb = [S.sb("cmb%d" % i_, [128, 16], F32) for i_ in range(2)]
        V4 = lambda a_: a_[:].rearrange("p (g k) -> p g k", g=4)
        dv = lambda fn, rd, wr: S.add("dve", fn, reads=rd, writes=wr)
        xmbs = [xmb, S.sb("xmbm1", [128, D], BF16)]
        pipe = Pipe(2)

        def make_item(ti, t):
            i = ti % 2
            xt = xts[i]
            r = 1 if t < NT_C else 0
            tok = slice(t * 128, (t + 1) * 128)
            pos = []
            for dc in range(4):
                pos.append(cnt["po"] % 4)
                cnt["po"] += 1

            def s0():
                S.add("sp", lambda e: e.dma_start(out=mT[i][:], in_=self.ap("mTd")[:, :, tok].rearrange("j p n -> p j n")), writes=[("mT", i)], dma=True)
                S.add("sp", lambda e: e.dma_start(out=xt[:], in_=self.ap("xres")[tok, :]), writes=[("xt", i)], dma=True)
                for dc in range(4):
                    po = pos[dc]
                    for j in range(NKC):
                        S.add("pe", lambda e, j=j: e.matmul(pyo[po][:], mT[i][:, j, :], wout[:, j, dc * 512:(dc + 1) * 512],
                                                            start=(j == 0), stop=(j == NKC - 1)),
                              reads=[("mT", i), ("wout", j // 8)], writes=[("pyo", po)])
                    S.add("dve", lambda e: e.tensor_tensor(out=xm32[:, dc * 512:(dc + 1) * 512], in0=pyo[po][:], in1=mods[(r, 2)][:, dc * 512:(dc + 1) * 512], op=ALU.mult),
                          reads=[("pyo", po), ("mod", r, 2)], writes=["xm32"])
                S.add("pool", lambda e: e.tensor_tensor(out=xt[:], in0=xt[:], in1=xm32[:], op=ALU.add), reads=[("xt", i), "xm32"], writes=[("xt", i)])
                S.add("sp", lambda e: e.dma_start(out=self.ap("xres")[tok, :], in_=xt[:]), reads=[("xt", i)], dma=True)
                self.rms_modulate(S, xt, ("xt", i), ssb[:, t:t + 1], ("ss", t), junk, mods[(r, 4)], ("mod", r, 4), mods[(r, 3)], ("mod", r, 3),
                                  xm32, xmbs[i], ("xmb", i))

            def s1():
                for j in range(NKC):
                    S.add("pe", lambda e, j=j: e.transpose(out=pth[j // 8][:, j % 8, :], in_=xmbs[i][:, j * 128:(j + 1) * 128], identity=self.ident_b[:]),
                          reads=[("xmb", i)], writes=[("pth", j // 8)])
                S.add("act", lambda e: e.copy(out=hst[i][:, 0:8, :], in_=pth[0][:]), reads=[("pth", 0)], writes=[("hst", i, 0)])
                S.add("dve", lambda e: e.tensor_copy(out=hst[i][:, 8:16, :], in_=pth[1][:]), reads=[("pth", 1)], writes=[("hst", i, 1)])
                S.add("sp", lambda e: e.dma_start(out=self.ap("h2Td")[:, :, tok], in_=hst[i][:]), reads=[("hst", i, 0), ("hst", i, 1)], dma=True)
                for k in range(NKC):
                    S.add("pe", lambda e, k=k: e.matmul(prt[:, 0:16], hst[i][:, k, :], rw[:, k, :], start=(k == 0), stop=(k == NKC - 1)),
                          reads=[("hst", i, 0), ("hst", i, 1), "rw"], writes=["prt"])
                S.add("act", lambda e: e.activation(out=sc[:], in_=prt[:, 0:16], func=AF.Sigmoid), reads=["prt"], writes=["sc"])
                dv(lambda e: e.tensor_tensor(out=sel[:], in0=sc[:], in1=rb[:], op=ALU.add), ["sc", "rb"], ["sel"])
                dv(lambda e: e.tensor_reduce(out=m1[:], in_=V4(sel), axis=AX.X, op=ALU.max), ["sel"], ["m1"])
                dv(lambda e: e.tensor_tensor(out=V4(eq), in0=V4(sel), in1=m1[:].unsqueeze(2).to_broadcast([128, 4, 4]), op=ALU.is_equal), ["sel", "m1"], ["eq"])
                dv(lambda e: e.scalar_tensor_tensor(out=sm[:], in0=eq[:], scalar=-1e30, in1=sel[:], op0=ALU.mult, op1=ALU.add), ["eq", "sel"], ["sm"])
                dv(lambda e: e.tensor_reduce(out=m2[:], in_=V4(sm), axis=AX.X, op=ALU.max), ["sm"], ["m2"])
                dv(lambda e: e.tensor_tensor(out=m1[:], in0=m1[:], in1=m2[:], op=ALU.add), ["m1", "m2"], ["m1"])
                dv(lambda e: e.tensor_reduce(out=gm[:, 0:1], in_=m1[:], axis=AX.X, op=ALU.max), ["m1"], ["gm"])
                dv(lambda e: e.tensor_tensor(out=m2[:], in0=m1[:], in1=gm[:, 0:1].to_broadcast([128, 4]), op=ALU.is_equal), ["m1", "gm"], ["m2"])
                dv(lambda e: e.tensor_scalar(out=m2[:], in0=m2[:], scalar1=-1.0, scalar2=1e30, op0=ALU.add, op1=ALU.mult), ["m2"], ["m2"])
                dv(lambda e: e.tensor_tensor(out=V4(sm), in0=V4(sel), in1=m2[:].unsqueeze(2).to_broadcast([128, 4, 4]), op=ALU.add), ["sel", "m2"], ["sm"])
                dv(lambda e: e.tensor_reduce(out=gm[:, 0:1], in_=sm[:], axis=AX.X, op=ALU.max), ["sm"], ["gm"])
                dv(lambda e: e.tensor_tensor(out=eq[:], in0=sm[:], in1=gm[:, 0:1].to_broadcast([128, 16]), op=ALU.is_equal), ["sm", "gm"], ["eq"])
                dv(lambda e: e.scalar_tensor_tensor(out=sm[:], in0=eq[:], scalar=-1e30, in1=sm[:], op0=ALU.mult, op1=ALU.add), ["eq", "sm"], ["sm"])
                dv(lambda e: e.tensor_reduce(out=gm[:, 1:2], in_=sm[:], axis=AX.X, op=ALU.max), ["sm"], ["gm"])
                dv(lambda e: e.tensor_tensor(out=sel[:], in0=sm[:], in1=gm[:, 1:2].to_broadcast([128, 16]), op=ALU.is_equal), ["sm", "gm"], ["sel"])
                dv(lambda e: e.tensor_tensor(out=eq[:], in0=eq[:], in1=sel[:], op=ALU.add), ["eq", "sel"], ["eq"])
                dv(lambda e: e.tensor_tensor(out=sc[:], in0=sc[:], in1=eq[:], op=ALU.mult), ["sc", "eq"], ["sc"])
                dv(lambda e: e.tensor_reduce(out=gm[:, 0:1], in_=sc[:], axis=AX.X, op=ALU.add), ["sc"], ["gm"])
                dv(lambda e: e.reciprocal(out=gm[:, 0:1], in_=gm[:, 0:1]), ["gm"], ["gm"])
                dv(lambda e: e.tensor_scalar(out=cmb[i][:], in0=sc[:], scalar1=gm[:, 0:1], scalar2=None, op0=ALU.mult), ["sc", "gm"], [("cmb", i)])
                S.add("sp", lambda e: e.dma_start(out=self.ap("combd")[tok, :], in_=cmb[i][:]), reads=[("cmb", i)], dma=True)

            return [s0, s1]

        for ti, t in enumerate(tiles):
            pipe.push(make_item(ti, t))
        pipe.drain()
        S.emit()
        S.close()

    def phase_moe(self, l, tiles, out_name=None):
        nc = self.nc
        GSZ = 6
        ngr = (len(tiles) + GSZ - 1) // GSZ
        base, rem = divmod(len(tiles), ngr)
        groups = []
        pos_ = 0
        for gi_ in range(ngr):
            sz = base + (1 if gi_ < rem else 0)
            groups.append(tiles[pos_:pos_ + sz])
            pos_ += sz
        S = Sched(nc)
        g2b = {}
        for r in (0, 1):
            if r == 1 and tiles[0] >= NT_C:
                continue
            g2b[r] = S.sb("g2b%d" % r, [128, D], F32)
            S.add("sp", lambda e, r=r: e.dma_start(out=g2b[r][:], in_=self.bcast_rows("modrow", modoff(l, r, 5), D)), writes=[("g2b", r)], dma=True)
        h2T = S.sb("h2T", [128, NKC, GSZ * 128], BF16)
        acc = [S.sb("acc%d" % i, [128, D], F32) for i in range(GSZ)]
        comb = S.sb("comb", [128, GSZ, 16], F32)
        w1 = [S.sb("w1_%d" % i, [128, NKC, 512], BF16) for i in range(2)]
        w3 = [S.sb("w3_%d" % i, [128, NKC, 512], BF16) for i in range(2)]
        w2 = [S.sb("w2_%d" % i, [128, 4, D], BF16) for i in range(2)]
        psA = [S.ps("psA%d" % i, [128, 512], F32) for i in range(2)]
        psG = [S.ps("psG%d" % i, [128, 512], F32) for i in range(2)]
        ptr = [S.ps("ptrm%d" % i, [128, 4, 128], BF16) for i in range(2)]
        psO = [S.ps("psOm%d" % i, [128, 512], F32) for i in range(2)]
        sa = [S.sb("sa%d" % i, [128, 512], F32) for i in range(2)]
        hid = [S.sb("hid%d" % i, [128, 512], BF16) for i in range(2)]
        hidT = [S.sb("hidT%d" % i, [128, 4, 128], BF16) for i in range(2)]
        sc = S.sb("r_sc", [128, 16], F32)
        sel = S.sb("r_sel", [128, 16], F32)
        eq = S.sb("r_eq", [128, 16], F32)
        sm = S.sb("r_sm", [128, 16], F32)
        m1 = S.sb("r_m1", [128, 4], F32)
        m2 = S.sb("r_m2", [128, 4], F32)
        gm = S.sb("r_gm", [128, 2], F32)
        xtz = S.sb("xtz", [128, D], F32)
        xts2 = [xtz, xtz]
        cnt = {"w": 0, "a": 0, "h": 0, "o": 0}
        V4 = lambda a: a[:].rearrange("p (g k) -> p g k", g=4)
        dv = lambda fn, rd, wr: S.add("dve", fn, reads=rd, writes=wr)
        for grp in groups:
            ng = len(grp)
            t0 = grp[0] * 128
            S.add("sp", lambda e: e.dma_start(out=h2T[:, :, 0:ng * 128], in_=self.ap("h2Td")[:, :, t0:t0 + ng * 128]), writes=["h2T"], dma=True)
            S.add("sp", lambda e: e.dma_start(out=comb[:, 0:ng, :], in_=self.ap("combd")[t0:t0 + ng * 128, :].rearrange("(g p) e -> p g e", p=128)),
                  writes=[("comb", gi) for gi in range(GSZ)], dma=True)
            for ex in range(N_EXP):
                wi = cnt["w"] % 2
                cnt["w"] += 1
                S.add("pool", lambda e: e.dma_start(out=w1[wi][:], in_=self.ap("moe_w1")[l, ex].rearrange("(k p) f -> p k f", p=128)), writes=[("w1", wi)], dma=True)
                S.add("pool", lambda e: e.dma_start(out=w3[wi][:], in_=self.ap("moe_w3")[l, ex].rearrange("(k p) f -> p k f", p=128)), writes=[("w3", wi)], dma=True)
                S.add("pool", lambda e: e.dma_start(out=w2[wi][:], in_=self.ap("moe_w2")[l, ex].rearrange("(k p) f -> p k f", p=128)), writes=[("w2", wi)], dma=True)
                ais = []
                for gi in range(ng):
                    ais.append(cnt["a"] % 2)
                    cnt["a"] += 1

                def up(gi):
                    tk = slice(gi * 128, (gi + 1) * 128)
                    ai = ais[gi]
                    for k in range(NKC):
                        S.add("pe", lambda e, k=k: e.matmul(psA[ai][:], h2T[:, k, tk], w1[wi][:, k, :], start=(k == 0), stop=(k == NKC - 1)),
                              reads=["h2T", ("w1", wi)], writes=[("psA", ai)])
                    for k in range(NKC):
                        S.add("pe", lambda e, k=k: e.matmul(psG[ai][:], h2T[:, k, tk], w3[wi][:, k, :], start=(k == 0), stop=(k == NKC - 1)),
                              reads=["h2T", ("w3", wi)], writes=[("psG", ai)])
                    S.add("act", lambda e: e.activation(out=sa[ai][:], in_=psA[ai][:], func=AF.Silu), reads=[("psA", ai)], writes=[("sa", ai)])
                    S.add("dve", lambda e: e.scalar_tensor_tensor(out=hid[ai][:], in0=psG[ai][:], scalar=comb[:, gi, ex:ex + 1], in1=sa[ai][:],
                                                                  op0=ALU.mult, op1=ALU.mult),
                          reads=[("psG", ai), ("comb", gi), ("sa", ai)], writes=[("hid", ai)])

                def down(gi):
                    ai = ais[gi]
                    for fc in range(4):
                        S.add("pe", lambda e, fc=fc: e.transpose(out=ptr[ai][:, fc, :], in_=hid[ai][:, fc * 128:(fc + 1) * 128], identity=self.ident_b[:]),
                              reads=[("hid", ai)], writes=[("ptr", ai)])
                    S.add("act", lambda e: e.copy(out=hidT[ai][:], in_=ptr[ai][:]), reads=[("ptr", ai)], writes=[("hidT", ai)])
                    for dc in range(4):
                        oi = cnt["o"] % 2
                        cnt["o"] += 1
                        for fc in range(4):
                            S.add("pe", lambda e, fc=fc: e.matmul(psO[oi][:], hidT[ai][:, fc, :], w2[wi][:, fc, dc * 512:(dc + 1) * 512],
                                                                  start=(fc == 0), stop=(fc == 3)),
                                  reads=[("hidT", ai), ("w2", wi)], writes=[("psO", oi)])
                        if ex == 0:
                            S.add("dve", lambda e: e.tensor_copy(out=acc[gi][:, dc * 512:(dc + 1) * 512], in_=psO[oi][:]),
                                  reads=[("psO", oi)], writes=[("acc", gi, dc)])
                        else:
                            S.add("dve", lambda e: e.tensor_tensor(out=acc[gi][:, dc * 512:(dc + 1) * 512], in0=acc[gi][:, dc * 512:(dc + 1) * 512],
                                                                   in1=psO[oi][:], op=ALU.add),
                                  reads=[("psO", oi), ("acc", gi, dc)], writes=[("acc", gi, dc)])

                up(0)
                for gi in range(ng):
                    if gi + 1 < ng:
                        up(gi + 1)
                    down(gi)
            for gi, t in enumerate(grp):
                r = 1 if t < NT_C else 0
                tok = slice(t * 128, (t + 1) * 128)
                xi = 0
                xt = xts2[xi]
                S.add("sp", lambda e: e.dma_start(out=xt[:], in_=self.ap("xres")[tok, :]), writes=[("xt", xi)], dma=True)
                S.add("dve", lambda e: e.tensor_tensor(out=acc[gi][:], in0=acc[gi][:], in1=g2b[r][:], op=ALU.mult),
                      reads=[("acc", gi, dc) for dc in range(4)] + [("g2b", r)], writes=[("acc", gi, dc) for dc in range(4)])
                S.add("pool", lambda e: e.tensor_tensor(out=xt[:], in0=xt[:], in1=acc[gi][:], op=ALU.add),
                      reads=[("xt", xi)] + [("acc", gi, dc) for dc in range(4)], writes=[("xt", xi)])
                if out_name is not None and t >= NT_C:
                    S.add("sp", lambda e: e.dma_start(out=self.T[out_name].ap()[(t - NT_C) * 128:(t - NT_C + 1) * 128, :], in_=xt[:]), reads=[("xt", xi)], dma=True)
                else:
                    S.add("sp", lambda e: e.dma_start(out=self.ap("xres")[tok, :], in_=xt[:]), reads=[("xt", xi)], dma=True)
        S.emit()
        S.close()


def build(layers=(0, 1), dbg=(), phases=None, groups=None, aheads=None, final_out=False):
    nc = bass.Bass("TRN2", target_bir_lowering=False)
    Sched.GLOBAL = None
    mk = MK(nc, dbg)
    if final_out:
        mk.T["out"] = nc.dram_tensor("out", [NT_L * 128, D], F32, kind="ExternalOutput")
    mk.load_consts()
    ph = lambda p: phases is None or p in phases
    for l in layers:
        if ph("ada"):
            mk.phase_ada(l)
    for l in layers:
        if ph("mod1"):
            mk.phase_mod1(l)
        if ph("inproj"):
            mk.phase_inproj(l, groups)
        if ph("mlstm"):
            mk.phase_mlstm(l, heads=aheads)
        if ph("mla_up"):
            mk.phase_mla_up(l)
        if ph("da"):
            mk.phase_attn(l, 0, l < DEPTH - 1, heads=aheads)
        if ph("mla"):
            mk.phase_attn(l, 1, l < DEPTH - 1, heads=aheads)
        tiles = list(range(NT)) if l < DEPTH - 1 else list(range(NT_C, NT))
        if ph("merge"):
            mk.phase_merge_a(l, tiles)
            mk.phase_merge_b(l, tiles)
        if ph("moe"):
            mk.phase_moe(l, tiles, out_name=("out" if (l == DEPTH - 1 and final_out) else None))
    mk.g.close()
    if Sched.GLOBAL is not None:
        Sched.GLOBAL["stack"].close()
    return nc, mk


def _axial(n, rot):
    rows = n // 64
    r = np.repeat(np.arange(rows, dtype=np.float32), 64)
    col = np.tile(np.arange(64, dtype=np.float32), rows)
    nf = rot // 4
    inv = (10000.0 ** (-np.arange(nf, dtype=np.float32) / nf)).astype(np.float32)
    return np.concatenate([r[:, None] * inv, col[:, None] * inv], -1)


def _rope_tab(rot):
    a = _axial(NT_L * 128, rot)
    t = np.zeros((NTOK, rot), np.float32)
    t[:NT_C * 128, :rot // 2] = 1.0
    t[NT_C * 128:, :rot // 2] = np.cos(a)
    t[NT_C * 128:, rot // 2:] = np.sin(a)
    return t


def _consts():
    c = np.zeros((128, 512), np.float32)
    c[:, 0:128] = np.eye(128)
    c[:, 128:256] = np.eye(128)[::-1]
    c[:, 256:384] = np.triu(np.ones((128, 128)))
    c[:, 384:512] = np.tril(np.ones((128, 128)))
    c2 = np.zeros((4, 512), np.float32)
    for k in range(4):
        c2[k, k * 128:(k + 1) * 128] = 1.0
    return c, c2


N_CORES = 4
_CACHE = {}


def kernel(**inputs):
    if "nc" not in _CACHE:
        _CACHE["nc"] = build(final_out=True)
    nc, mk = _CACHE["nc"]
    names = [n for n in mk.T if n in INPUT_SHAPES]
    c1, c2 = _consts()
    rd = _rope_tab(128)
    rope_da2 = np.ascontiguousarray(np.concatenate([rd[:, :64], rd[:, :64], -rd[:, 64:], rd[:, 64:]], 1))
    shared = {"consts": c1, "consts2": c2, "rope_da2": rope_da2, "rope_mla": _rope_tab(64)}
    B = inputs["x"].shape[0]
    in_maps = []
    for core in range(N_CORES):
        b = core % B
        d = {}
        for n in names:
            if n == "xs":
                d[n] = np.ascontiguousarray(np.concatenate([inputs["ctx"][b], inputs["x"][b]], 0), dtype=np.float32)
            elif n == "cvec":
                d[n] = np.ascontiguousarray(np.stack([inputs["c"][b], inputs["c_ctx"]], 0), dtype=np.float32)
            elif n in shared:
                d[n] = shared[n]
            else:
                d[n] = np.ascontiguousarray(inputs[n], dtype=np.float32)
        in_maps.append(d)
    res = run_bass_kernel_spmd(nc, in_maps, core_ids=list(range(N_CORES)))
    out = np.stack([np.asarray(res.results[b]["out"], dtype=np.float32) for b in range(B)], 0)
    return out
```

```python
import contextlib
import math
import numpy as np
import concourse.bass as bass
import concourse.mybir as mybir
from concourse.bass_utils import run_bass_kernel_spmd

F32 = mybir.dt.float32
BF16 = mybir.dt.bfloat16
AF = mybir.ActivationFunctionType
ALU = mybir.AluOpType
AX = mybir.AxisListType

ENGS = ("pe", "act", "dve", "pool", "sp")
N_DMA_SEMS = 16

D = 2048
NKC = D // 128
DEPTH = 2
NT_C = 2
NT_L = 16
NT = NT_C + NT_L
NTOK = NT * 128
D_IN = 14160
OFF_MQK, OFF_MV, OFF_MO, OFF_MG = 0, 2048, 3072, 4096
OFF_DQ, OFF_DK, OFF_DV = 4112, 5136, 6160
OFF_CQ, OFF_CKV, OFF_KR, OFF_G = 7184, 7696, 7952, 8016
EPS = 1e-6
N_EXP = 16
INPUT_SHAPES = {
    "xs": [NTOK, D], "cvec": [2, D], "consts": [128, 512], "consts2": [4, 512],
    "rope_da2": [NTOK, 256], "rope_mla": [NTOK, 64],
    "w_ada": [DEPTH, D, 6 * D], "b_ada": [DEPTH, 6 * D],
    "w_in": [DEPTH, D, D_IN], "b_in": [DEPTH, D_IN],
    "m_conv_w": [DEPTH, 5, 2048], "m_conv_b": [DEPTH, 2048], "m_norm_g": [DEPTH, 1024],
    "da_q_norm_g": [DEPTH, 128], "da_k_norm_g": [DEPTH, 128], "da_lambda": [DEPTH, 4, 128],
    "da_subln_g": [DEPTH, 256], "mla_cq_norm_g": [DEPTH, 512], "mla_ckv_norm_g": [DEPTH, 256],
    "mla_w_uq": [DEPTH, 512, 1536], "mla_w_ukv": [DEPTH, 256, 2048],
    "mla_q_norm_g": [DEPTH, 192], "mla_k_norm_g": [DEPTH, 192],
    "w_branch": [DEPTH, 3, 1024, 2048], "w_out": [DEPTH, 2048, 2048],
    "moe_w1": [DEPTH, 16, 2048, 512], "moe_w3": [DEPTH, 16, 2048, 512], "moe_w2": [DEPTH, 16, 512, 2048],
    "router_w": [2048, 16], "router_bias": [16],
}


class Pipe:
    def __init__(self, nstages):
        self.n = nstages
        self.items = []

    def push(self, stages):
        self.items.append(stages)
        i = len(self.items) - 1
        for s in range(self.n):
            j = i - s
            if j >= 0 and s < len(self.items[j]):
                self.items[j][s]()

    def drain(self):
        last = len(self.items) - 1
        for extra in range(1, self.n):
            for s in range(extra, self.n):
                j = last + extra - s
                if 0 <= j <= last and s < len(self.items[j]):
                    self.items[j][s]()
        self.items = []


class Op:
    def __init__(self, eng, fn, dma):
        self.eng = eng
        self.fn = fn
        self.deps = []
        self.needed = False
        self.idx = -1
        self.dma = dma
        self.prev_dma = None
        self.cnt = 0


class Rec:
    def __init__(self):
        self.call = None

    def __getattr__(self, name):
        def f(*a, **k):
            self.call = (name, a, k)
            return self
        return f


class Sched:
    PID = 0
    GLOBAL = None

    def __init__(self, nc, same_engine_sync=True):
        self.nc = nc
        self.ops = {e: [] for e in ENGS}
        self.res = {}
        self.stack = contextlib.ExitStack()
        self.same_engine_sync = same_engine_sync
        self.dma_count = {e: 0 for e in ENGS}
        self.dma_last = {}
        self.seen = {e: {e2: -1 for e2 in ENGS} for e in ENGS}
        self.seen_dma = {e: {} for e in ENGS}
        Sched.PID += 1
        self.pid = Sched.PID

    def sb(self, name, shape, dt=F32):
        return self.stack.enter_context(self.nc.sbuf_tensor("p%d_%s" % (self.pid, name), list(shape), dt))

    def ps(self, name, shape, dt=F32):
        return self.stack.enter_context(self.nc.psum_tensor("p%d_%s" % (self.pid, name), list(shape), dt))

    def add(self, eng, fn, reads=(), writes=(), dma=False):
        rec = Rec()
        fn(rec)
        op = Op(eng, rec.call, dma)
        lst = self.ops[eng]
        op.idx = len(lst)
        deps = []
        for k in reads:
            r = self.res.get(k)
            if r is not None and r[0] is not None:
                deps.append(r[0])
        for k in writes:
            r = self.res.get(k)
            if r is not None:
                if r[0] is not None:
                    deps.append(r[0])
                deps.extend(r[1])
        seen = self.seen[eng]
        sd = self.seen_dma[eng]
        out = []
        for d in deps:
            if d is op:
                continue
            if d.eng == eng and not d.dma:
                if eng == "pe" or (not self.same_engine_sync and not dma):
                    continue
            if d.dma:
                key = (d.eng, d.dma_slot)
                if sd.get(key, -1) >= d.dma_seq:
                    continue
                sd[key] = d.dma_seq
                out.append(d)
            else:
                if seen[d.eng] >= d.idx:
                    continue
                seen[d.eng] = d.idx
                d.needed = True
                out.append(d)
        op.deps = out
        if dma:
            c = self.dma_count[eng]
            self.dma_count[eng] = c + 1
            op.dma_slot = c % N_DMA_SEMS
            op.dma_seq = c // N_DMA_SEMS
            prev = self.dma_last.get((eng, op.dma_slot))
            op.prev_dma = prev
            self.dma_last[(eng, op.dma_slot)] = op
            if prev is not None:
                sd[(eng, op.dma_slot)] = max(sd.get((eng, op.dma_slot), -1), prev.dma_seq)
        lst.append(op)
        for k in reads:
            r = self.res.setdefault(k, [None, []])
            r[1].append(op)
        for k in writes:
            self.res[k] = [op, []]
        return op

    def emit(self):
        nc = self.nc
        G = Sched.GLOBAL
        if G is None:
            G = Sched.GLOBAL = {"stack": contextlib.ExitStack(), "sems": {}, "dsems": {}, "base": {e: 0 for e in ENGS}, "dbase": {}}
        gs = G["stack"]
        for e in ENGS:
            if e not in G["sems"]:
                G["sems"][e] = gs.enter_context(nc.semaphore("gs_" + e))
        for e in ENGS:
            if self.dma_count[e] > 0:
                for j in range(N_DMA_SEMS):
                    if (e, j) not in G["dsems"]:
                        G["dsems"][(e, j)] = gs.enter_context(nc.semaphore("gd_%s_%d" % (e, j)))
                        G["dbase"][(e, j)] = 0
        sems, dsems, base, dbase = G["sems"], G["dsems"], G["base"], G["dbase"]
        final_cnt = {}
        for e in ENGS:
            c = base[e]
            last = None
            for op in self.ops[e]:
                if not op.dma:
                    last = op
            if last is not None:
                last.needed = True
            for op in self.ops[e]:
                if op.dma:
                    continue
                if op.needed:
                    c += 1
                op.cnt = c
            final_cnt[e] = c
        dval = lambda d: 16 * (dbase[(d.eng, d.dma_slot)] + d.dma_seq + 1)
        block = self.stack.enter_context(nc.Block())
        engmap = {"pe": block.tensor, "act": block.scalar, "dve": block.vector,
                  "pool": block.gpsimd, "sp": block.sync}

        def make(e):
            def body(eng):
                for op in self.ops[e]:
                    for d in op.deps:
                        if d.dma:
                            eng.wait_ge(dsems[(d.eng, d.dma_slot)], dval(d))
                        else:
                            eng.wait_ge(sems[d.eng], d.cnt)
                    if op.dma:
                        if op.prev_dma is not None:
                            eng.wait_ge(dsems[(e, op.prev_dma.dma_slot)], dval(op.prev_dma))
                        ins = getattr(eng, op.fn[0])(*op.fn[1], **op.fn[2])
                        ins.then_inc(dsems[(e, op.dma_slot)], 16)
                    else:
                        ins = getattr(eng, op.fn[0])(*op.fn[1], **op.fn[2])
                        if op.needed:
                            ins.then_inc(sems[e], 1)
                for e2 in ENGS:
                    if final_cnt[e2] > base[e2]:
                        eng.wait_ge(sems[e2], final_cnt[e2])
                for (e2, j), last in self.dma_last.items():
                    eng.wait_ge(dsems[(e2, j)], dval(last))
            return body

        for e in ENGS:
            engmap[e](make(e))
        for e in ENGS:
            base[e] = final_cnt[e]
        for (e2, j), last in self.dma_last.items():
            dbase[(e2, j)] += last.dma_seq + 1

    def close(self):
        self.stack.close()


def modoff(l, r, seg):
    return ((l * 2 + r) * 6 + seg) * D


class MK:
    def __init__(self, nc, dbg=()):
        self.nc = nc
        self.dbg = set(dbg)
        self.g = contextlib.ExitStack()
        self.T = {}
        Sx = self.scr
        Sx("modrow", [DEPTH, 2, 6 * D], F32)
        Sx("xres", [NTOK, D], F32)
        Sx("hTd", [128, NKC, NTOK], BF16)
        Sx("mqkT", [16, 128, NTOK], BF16)
        Sx("mk_tm", [NTOK, 1024], BF16)
        Sx("mv", [NTOK, 1024], BF16)
        Sx("mo", [NTOK, 1024], BF16)
        Sx("mg", [NTOK, 16], F32)
        Sx("dqT", [8, 128, NTOK], BF16)
        Sx("dkT", [8, 128, NTOK], BF16)
        Sx("dv", [NTOK, 1024], BF16)
        Sx("cqT", [4, 128, NTOK], BF16)
        Sx("ckvT", [2, 128, NTOK], BF16)
        Sx("akrT", [64, NTOK], BF16)
        Sx("gT", [48, 128, NTOK], BF16)
        Sx("aqTn", [8, 128, NTOK], BF16)
        Sx("aqTr", [8, 64, NTOK], BF16)
        Sx("akT", [8, 128, NTOK], BF16)
        Sx("av", [NTOK, 1024], BF16)
        Sx("ysT", [3, 8, 128, NTOK], BF16)
        Sx("hmf", [NTOK, 1024], F32)
        Sx("h2Td", [128, NKC, NTOK], BF16)
        Sx("mTd", [NKC, 128, NTOK], BF16)
        Sx("combd", [NTOK, 16], F32)
        Sx("hmb", [NTOK, 1024], F32)

    def tens(self, name):
        if name not in self.T:
            self.T[name] = self.nc.dram_tensor(name, list(INPUT_SHAPES[name]), F32, kind="ExternalInput")
        return self.T[name]

    def scr(self, name, shape, dt):
        kind = "ExternalOutput" if name in self.dbg else "Internal"
        self.T[name] = self.nc.dram_tensor(name, list(shape), dt, kind=kind)

    def ap(self, name):
        return self.tens(name).ap()

    def bcast_rows(self, name, offset, width, nparts=128):
        return bass.AP(self.tens(name), offset, [[0, nparts], [1, width]])

    def load_consts(self):
        nc = self.nc
        self.ident_f = self.g.enter_context(nc.sbuf_tensor("ident_f", [128, 128], F32))
        self.ident_b = self.g.enter_context(nc.sbuf_tensor("ident_b", [128, 128], BF16))
        self.eps_col = self.g.enter_context(nc.sbuf_tensor("eps_col", [128, 1], F32))
        S = Sched(nc)
        S.add("pool", lambda e: e.memset(self.eps_col[:], EPS), writes=["epsc"])
        S.add("sp", lambda e: e.dma_start(out=self.ident_f[:], in_=self.ap("consts")[:, 0:128]),
              writes=["idf"], dma=True)
        S.add("dve", lambda e: e.tensor_copy(out=self.ident_b[:], in_=self.ident_f[:]), reads=["idf"], writes=["idb"])
        for t in range(0, NT, 6):
            S.add("sp", lambda e, t=t: e.dma_start(out=self.ap("xres")[t * 128:(t + 6) * 128, :],
                                                   in_=self.ap("xs")[t * 128:(t + 6) * 128, :]), dma=True)
        S.emit()
        S.close()

    def phase_ada(self, l):
        nc = self.nc
        S = Sched(nc)
        cv = S.sb("cv", [2, D], F32)
        sv = S.sb("sv", [2, D], F32)
        sT = S.sb("sT", [128, NKC, 2], BF16)
        pT = S.ps("pT", [128, NKC, 2], F32)
        wb = [S.sb("wada%d" % i, [128, NKC, 512], BF16) for i in range(2)]
        bb = [S.sb("bada%d" % i, [2, 512], F32) for i in range(2)]
        ob = [S.sb("oada%d" % i, [2, 512], F32) for i in range(2)]
        pm = [S.ps("pm%d" % i, [128, 512], F32) for i in range(2)]
        S.add("sp", lambda e: e.dma_start(out=cv[:], in_=self.ap("cvec")), writes=["cv"], dma=True)
        S.add("act", lambda e: e.activation(out=sv[:], in_=cv[:], func=AF.Silu), reads=["cv"], writes=["sv"])
        for k in range(NKC):
            S.add("pe", lambda e, k=k: e.transpose(out=pT[:, k, :], in_=sv[:, k * 128:(k + 1) * 128],
                                                   identity=self.ident_f[0:2, 0:2]),
                  reads=["sv"], writes=["pT"])
        S.add("dve", lambda e: e.tensor_copy(out=sT[:], in_=pT[:]), reads=["pT"], writes=["sT"])
        wl = self.ap("w_ada")[l]
        nblk = 6 * D // 512
        for j in range(nblk):
            i = j % 2
            S.add("pool", lambda e, j=j, i=i: e.dma_start(
                out=wb[i][:], in_=wl[:, j * 512:(j + 1) * 512].rearrange("(k p) w -> p k w", p=128)),
                writes=[("wb", i)], dma=True)
            S.add("sp", lambda e, j=j, i=i: e.dma_start(
                out=bb[i][:], in_=self.bcast_rows("b_ada", l * 6 * D + j * 512, 512, 2)),
                writes=[("bb", i)], dma=True)
            for k in range(NKC):
                S.add("pe", lambda e, k=k, i=i: e.matmul(pm[i][0:2, :], sT[:, k, :], wb[i][:, k, :],
                                                         start=(k == 0), stop=(k == NKC - 1)),
                      reads=["sT", ("wb", i)], writes=[("pm", i)])
            S.add("dve", lambda e, i=i: e.tensor_tensor(out=ob[i][:], in0=pm[i][0:2, :], in1=bb[i][:], op=ALU.add),
                  reads=[("pm", i), ("bb", i)], writes=[("ob", i)])
            S.add("sp", lambda e, j=j, i=i: e.dma_start(out=self.ap("modrow")[l][:, j * 512:(j + 1) * 512], in_=ob[i][:]),
                  reads=[("ob", i)], dma=True)
        S.emit()
        S.close()

    def load_mod_tiles(self, S, l, segs, tag):
        out = {}
        for r in (0, 1):
            for seg, add1 in segs:
                tl = S.sb("mod%s_%d_%d" % (tag, r, seg), [128, D], F32)
                key = ("mod", r, seg)
                S.add("sp", lambda e, tl=tl, r=r, seg=seg: e.dma_start(
                    out=tl[:], in_=self.bcast_rows("modrow", modoff(l, r, seg), D)), writes=[key], dma=True)
                if add1:
                    S.add("pool", lambda e, tl=tl: e.tensor_scalar_add(tl[:], tl[:], 1.0), reads=[key], writes=[key])
                out[(r, seg)] = tl
        return out

    def rstd(self, S, ap, key, n):
        S.add("act", lambda e: e.activation(out=ap, in_=ap, func=AF.Sqrt, scale=1.0 / n, bias=self.eps_col[:, 0:1]),
              reads=[key], writes=[key])
        S.add("dve", lambda e: e.reciprocal(out=ap, in_=ap), reads=[key], writes=[key])

    def rms_modulate(self, S, xt, xkey, ss, sskey, junk, sc, sckey, sh, shkey, xm32, xmb, xmkey):
        S.add("act", lambda e: e.activation(out=junk[:], in_=xt[:], func=AF.Square, accum_out=ss),
              reads=[xkey, sskey], writes=["junk", sskey])
        self.rstd(S, ss, sskey, D)
        S.add("dve", lambda e: e.scalar_tensor_tensor(out=xm32[:], in0=xt[:], scalar=ss, in1=sc[:],
                                                      op0=ALU.mult, op1=ALU.mult),
              reads=[xkey, sskey, sckey], writes=["xm32"])
        S.add("pool", lambda e: e.tensor_tensor(out=xmb[:], in0=xm32[:], in1=sh[:], op=ALU.add),
              reads=["xm32", shkey], writes=[xmkey])

    def phase_mod1(self, l):
        nc = self.nc
        S = Sched(nc)
        mods = self.load_mod_tiles(S, l, [(1, True), (0, False)], "a")
        xts = [S.sb("xt%d" % i, [128, D], F32) for i in range(2)]
        junk = S.sb("junk", [128, D], BF16)
        xm32 = S.sb("xm32", [128, D], F32)
        xmbs = [S.sb("xmb%d" % i, [128, D], BF16) for i in range(2)]
        ssb = S.sb("ssb", [128, NT], F32)
        hst = [S.sb("hst%d" % i, [128, NKC, 128], BF16) for i in range(2)]
        pts = [S.ps("pt%d" % i, [128, 8, 128], BF16) for i in range(4)]
        S.add("dve", lambda e: e.memset(ssb[:], 0.0), writes=[("ss", t) for t in range(NT)])
        pipe = Pipe(2)

        def make_item(t):
            r = 1 if t < NT_C else 0
            i = t % 2
            xt = xts[i]

            def s0():
                S.add("sp", lambda e: e.dma_start(out=xt[:], in_=self.ap("xres")[t * 128:(t + 1) * 128, :]),
                      writes=[("xt", i)], dma=True)
                self.rms_modulate(S, xt, ("xt", i), ssb[:, t:t + 1], ("ss", t), junk,
                                  mods[(r, 1)], ("mod", r, 1), mods[(r, 0)], ("mod", r, 0), xm32, xmbs[i], ("xmb", i))

            def s1():
                for j in range(NKC):
                    pt = pts[2 * i + j // 8]
                    S.add("pe", lambda e, pt=pt, j=j: e.transpose(out=pt[:, j % 8, :], in_=xmbs[i][:, j * 128:(j + 1) * 128],
                                                                  identity=self.ident_b[:]),
                          reads=[("xmb", i)], writes=[("pt", 2 * i + j // 8)])
                S.add("act", lambda e: e.copy(out=hst[i][:, 0:8, :], in_=pts[2 * i][:]),
                      reads=[("pt", 2 * i)], writes=[("hst", i, 0)])
                S.add("dve", lambda e: e.tensor_copy(out=hst[i][:, 8:16, :], in_=pts[2 * i + 1][:]),
                      reads=[("pt", 2 * i + 1)], writes=[("hst", i, 1)])
                S.add("sp", lambda e: e.dma_start(out=self.ap("hTd")[:, :, t * 128:(t + 1) * 128], in_=hst[i][:]),
                      reads=[("hst", i, 0), ("hst", i, 1)], dma=True)

            return [s0, s1]

        for t in range(NT):
            pipe.push(make_item(t))
        pipe.drain()
        S.emit()
        S.close()

    def rows_to_cols(self, S, name, src_ap_rows, nrows, nchunks, pst, pstkey, eng="dve"):
        rt = S.sb(name + "_r", [nrows, nchunks * 128], F32)
        ct = S.sb(name + "_c", [128, nchunks, nrows], F32)
        S.add("sp", lambda e: e.dma_start(out=rt[:], in_=src_ap_rows), writes=[name + "_r"], dma=True)
        for c in range(nchunks):
            S.add("pe", lambda e, c=c: e.transpose(out=pst[:, c * nrows:(c + 1) * nrows], in_=rt[:, c * 128:(c + 1) * 128],
                                                   identity=self.ident_f[0:nrows, 0:nrows]),
                  reads=[name + "_r"], writes=[pstkey])
        S.add(eng, lambda e: e.tensor_copy(out=ct[:].rearrange("p c r -> p (c r)"), in_=pst[:, 0:nchunks * nrows]),
              reads=[pstkey], writes=[name + "_c"])
        return ct

    def phase_inproj(self, l, groups=None):
        nc = self.nc
        S = Sched(nc)
        hT = S.sb("hT", [128, NKC, NTOK], BF16)
        wbs = [S.sb("wblk%d" % i, [128, NKC, 512], BF16) for i in range(2)]
        bts = [S.sb("bt%d" % i, [128, 512], F32) for i in range(2)]
        pa = [S.ps("pa%d" % i, [128, 512], F32) for i in range(5)]
        pt = [S.ps("ptb%d" % i, [128, 8, 128], BF16) for i in range(2)]
        pmisc = S.ps("pmisc", [128, 512], F32)
        st32 = [S.sb("st32_%d" % i, [128, 512], F32) for i in range(2)]
        stb = [S.sb("stb%d" % i, [128, 512], BF16) for i in range(3)]
        NROT = 2
        tmpR = [[S.sb("tmp%d_%d" % (i, r_), [128, 512], F32) for i in range(3)] for r_ in range(NROT)]
        ss4R = [S.sb("ss4_%d" % r_, [128, 8], F32) for r_ in range(NROT)]
        rot = {"i": 0}
        tmp = tmpR[0]
        ss4 = ss4R[0]
        trs = [S.sb("trs%d" % i, [128, 4, 128], BF16) for i in range(4)]
        w_l = self.ap("w_in")[l]
        st = {"wcnt": 0, "pa": 0, "stb": 0, "st32": 0, "pt": 0, "trs": 0}
        for h in range(0, NT, 6):
            S.add("sp", lambda e, h=h: e.dma_start(out=hT[:, :, h * 128:(h + 6) * 128],
                                                   in_=self.ap("hTd")[:, :, h * 128:(h + 6) * 128]),
                  writes=[("hT", h // 6)], dma=True)
        hkeys = [("hT", i) for i in range(3)]

        plan = []
        _do = lambda g: groups is None or g in groups
        for gname, off in (("mv", OFF_MV), ("dv", OFF_DV), ("mo", OFF_MO)):
            if _do(gname):
                plan += [(off, 512, True), (off + 512, 512, True)]
        if _do("mg"):
            plan.append((OFF_MG, 16, True))
        for gname, off in (("dq", OFF_DQ), ("dk", OFF_DK)):
            if _do(gname):
                plan += [(off, 512, True), (off + 512, 512, True)]
        if _do("cq"):
            plan.append((OFF_CQ, 512, True))
        if _do("kr"):
            plan.append((OFF_CKV, 320, True))
        if _do("g"):
            plan += [(OFF_G + c0, 512, False) for c0 in range(0, 6144, 512)]
        if _do("mqk"):
            plan += [(OFF_MQK + c0, 512, False) for c0 in range(0, 2048, 512)]
        issued = {"n": 0}

        def issue_w(k):
            c0, W, bias = plan[k]
            i = k % 2
            S.add("pool", lambda e: e.dma_start(out=wbs[i][:, :, 0:W],
                                                in_=w_l[:, c0:c0 + W].rearrange("(k p) w -> p k w", p=128)),
                  writes=[("wb", i)], dma=True)
            if bias:
                S.add("sp", lambda e: e.dma_start(out=bts[i][:, 0:W], in_=self.bcast_rows("b_in", l * D_IN + c0, W)),
                      writes=[("bt", i)], dma=True)

        def load_w(c0, W, bias=True):
            k = st["wcnt"]
            st["wcnt"] += 1
            assert plan[k] == (c0, W, bias), (plan[k], c0, W, bias)
            while issued["n"] <= min(k + 1, len(plan) - 1):
                issue_w(issued["n"])
                issued["n"] += 1
            return k % 2

        def nxt(k, n):
            i = st[k] % n
            st[k] += 1
            return i

        def mm_tm(t, wi, W):
            p = nxt("pa", 5)
            for k in range(NKC):
                S.add("pe", lambda e, k=k: e.matmul(pa[p][:, 0:W], hT[:, k, t * 128:(t + 1) * 128], wbs[wi][:, k, 0:W],
                                                    start=(k == 0), stop=(k == NKC - 1)),
                      reads=[("hT", t // 6), ("wb", wi)], writes=[("pa", p)])
            return p

        def store(eng, dst_ap, src_ap, rkeys):
            S.add(eng, lambda e: e.dma_start(out=dst_ap, in_=src_ap), reads=rkeys, dma=True)

        do = lambda g: groups is None or g in groups

        for gname, off, width, dst in (("mv", OFF_MV, 1024, "mv"), ("dv", OFF_DV, 1024, "dv"), ("mo", OFF_MO, 1024, "mo")):
            if not do(gname):
                continue
            for c0 in range(0, width, 512):
                wi = load_w(off + c0, 512)
                for t in range(NT):
                    p = mm_tm(t, wi, 512)
                    b = nxt("stb", 3)
                    if gname == "mo":
                        a = nxt("st32", 2)
                        S.add("dve", lambda e, p=p, a=a: e.tensor_tensor(out=st32[a][:], in0=pa[p][:], in1=bts[wi][:], op=ALU.add),
                              reads=[("pa", p), ("bt", wi)], writes=[("st32", a)])
                        S.add("act", lambda e, a=a, b=b: e.activation(out=stb[b][:], in_=st32[a][:], func=AF.Sigmoid),
                              reads=[("st32", a)], writes=[("stb", b)])
                    else:
                        S.add("dve", lambda e, p=p, b=b: e.tensor_tensor(out=stb[b][:], in0=pa[p][:], in1=bts[wi][:], op=ALU.add),
                              reads=[("pa", p), ("bt", wi)], writes=[("stb", b)])
                    store("sp", self.ap(dst)[t * 128:(t + 1) * 128, c0:c0 + 512], stb[b][:], [("stb", b)])
        if do("mg"):
            wi = load_w(OFF_MG, 16)
            for t in range(NT):
                p = mm_tm(t, wi, 16)
                a = nxt("st32", 2)
                S.add("dve", lambda e, p=p, a=a: e.tensor_tensor(out=st32[a][:, 0:16], in0=pa[p][:, 0:16], in1=bts[wi][:, 0:16], op=ALU.add),
                      reads=[("pa", p), ("bt", wi)], writes=[("st32", a)])
                store("sp", self.ap("mg")[t * 128:(t + 1) * 128, :], st32[a][:, 0:16], [("st32", a)])

        def transposes_out(src_b, skey, ngrp, gw, dst_ap_fn):
            q = nxt("pt", 2)
            for g_ in range(ngrp):
                S.add("pe", lambda e, g_=g_: e.transpose(out=pt[q][0:gw, g_, :], in_=src_b[:, g_ * gw:(g_ + 1) * gw],
                                                         identity=self.ident_b[:]),
                      reads=[skey], writes=[("pt", q)])
            r = nxt("trs", 4)
            S.add("act", lambda e: e.copy(out=trs[r][0:gw, 0:ngrp, :], in_=pt[q][0:gw, 0:ngrp, :]),
                  reads=[("pt", q)], writes=[("trs", r)])
            store("sp", dst_ap_fn(), trs[r][0:gw, 0:ngrp, :], [("trs", r)])

        if do("kr"):
            rml = S.sb("rml", [128, NT, 64], F32)
            S.add("sp", lambda e: e.dma_start(out=rml[:], in_=self.ap("rope_mla").rearrange("(t p) c -> p t c", p=128)),
                  writes=["rml"], dma=True)

        def rope(src, skey, ngrp, half, cos_ap, sin_ap, dstb, dkey):
            x1 = src[:, :, 0:half]
            x2 = src[:, :, half:2 * half]
            cb = cos_ap.unsqueeze(1).to_broadcast([128, ngrp, half])
            sb_ = sin_ap.unsqueeze(1).to_broadcast([128, ngrp, half])
            ta = tmp[0][:, 0:ngrp * half].rearrange("p (g h) -> p g h", g=ngrp)
            tb = tmp[1][:, 0:ngrp * half].rearrange("p (g h) -> p g h", g=ngrp)
            S.add("pool", lambda e: e.tensor_tensor(out=ta, in0=x1, in1=cb, op=ALU.mult), reads=[skey, "rml"], writes=[("tmp0", rot["i"])])
            S.add("pool", lambda e: e.tensor_tensor(out=tb, in0=x2, in1=sb_, op=ALU.mult), reads=[skey, "rml"], writes=[("tmp1", rot["i"])])
            S.add("dve", lambda e: e.tensor_tensor(out=dstb[:, :, 0:half], in0=ta, in1=tb, op=ALU.subtract),
                  reads=[("tmp0", rot["i"]), ("tmp1", rot["i"])], writes=[dkey])
            S.add("pool", lambda e: e.tensor_tensor(out=ta, in0=x1, in1=sb_, op=ALU.mult), reads=[skey, "rml"], writes=[("tmp0", rot["i"])])
            S.add("dve", lambda e: e.tensor_tensor(out=tb, in0=x2, in1=cb, op=ALU.mult), reads=[skey, "rml"], writes=[("tmp1", rot["i"])])
            S.add("dve", lambda e: e.tensor_tensor(out=dstb[:, :, half:2 * half], in0=ta, in1=tb, op=ALU.add),
                  reads=[("tmp0", rot["i"]), ("tmp1", rot["i"])], writes=[dkey])

        if do("dq") or do("dk"):
            rdt = [S.sb("rdt%d" % i, [128, 256], F32) for i in range(2)]
            ones_b = S.sb("ones_b", [1, 128], BF16)
            S.add("pool", lambda e: e.memset(ones_b[:], 1.0), writes=["ones_b"])
            brow = [S.sb("brow%d" % i, [1, 512], BF16) for i in range(2)]
            junkq = S.sb("junkq", [128, 4, 128], BF16)
            stq = S.sb("stq18", [128, NT, 512], BF16)
            ssq4 = [S.sb("ssq4_%d" % i, [128, 4], F32) for i in range(4)]
            xg4 = [S.sb("xg4_%d" % i, [128, 512], F32) for i in range(2)]
            tq1 = [S.sb("tq1_%d" % i, [128, 512], F32) for i in range(2)]
            tq2 = [S.sb("tq2_%d" % i, [128, 512], F32) for i in range(1)]
            qc = {"n": 0}
        for gname, off, gain, scale, dst in (("dq", OFF_DQ, "da_q_norm_g", 128 ** -0.5, "dqT"), ("dk", OFF_DK, "da_k_norm_g", 1.0, "dkT")):
            if not do(gname):
                continue
            gb = S.sb("gb_" + gname, [128, 128], F32)
            S.add("sp", lambda e, gb=gb, gain=gain: e.dma_start(out=gb[:], in_=self.bcast_rows(gain, l * 128, 128)),
                  writes=["gb_" + gname], dma=True)
            S.add("pool", lambda e, gb=gb, scale=scale: e.tensor_scalar_mul(gb[:], gb[:], float(scale)),
                  reads=["gb_" + gname], writes=["gb_" + gname])
            for c0 in range(0, 1024, 512):
                wi = load_w(off + c0, 512)
                S.add("pool", lambda e: e.dma_start(out=brow[wi][:], in_=bass.AP(self.tens("b_in"), l * D_IN + off + c0, [[0, 1], [1, 512]])),
                      writes=[("brow", wi)], dma=True)
                for t in range(NT):
                    n_ = qc["n"]
                    qc["n"] += 1
                    r4, r3, r2 = n_ % 4, n_ % 2, 0
                    r1 = n_ % 2
                    ss = ssq4[r4]
                    xg = xg4[r3]
                    rd = rdt[r3]
                    p = nxt("pa", 5)
                    S.add("sp", lambda e: e.dma_start(out=rd[:], in_=self.ap("rope_da2")[t * 128:(t + 1) * 128, :]), writes=[("rdt", r3)], dma=True)
                    for k in range(NKC):
                        S.add("pe", lambda e, k=k: e.matmul(pa[p][:], hT[:, k, t * 128:(t + 1) * 128], wbs[wi][:, k, :],
                                                            start=(k == 0), stop=False),
                              reads=[("hT", t // 6), ("wb", wi)], writes=[("pa", p)])
                    S.add("pe", lambda e: e.matmul(pa[p][:], ones_b[:], brow[wi][:], start=False, stop=True),
                          reads=["ones_b", ("brow", wi)], writes=[("pa", p)])
                    S.add("dve", lambda e: e.memset(ss[:], 0.0), reads=[("ssq", r4)], writes=[("ssq", r4)])
                    for g_ in range(4):
                        S.add("act", lambda e, g_=g_: e.activation(out=junkq[:, g_, :], in_=pa[p][:, g_ * 128:(g_ + 1) * 128], func=AF.Square,
                                                                   accum_out=ss[:, g_:g_ + 1]),
                              reads=[("pa", p), ("ssq", r4)], writes=[("junkq", g_), ("ssqc", r4, g_)])
                    S.add("act", lambda e: e.activation(out=ss[:], in_=ss[:], func=AF.Sqrt, scale=1.0 / 128, bias=self.eps_col[:, 0:1]),
                          reads=[("ssqc", r4, g_) for g_ in range(4)] + [("ssq", r4)], writes=[("ssq", r4)] + [("ssqc", r4, g_) for g_ in range(4)])
                    x3 = xg[:].rearrange("p (g h) -> p g h", g=4)
                    S.add("dve", lambda e, x3=x3, gb=gb: e.tensor_tensor(out=x3, in0=pa[p][:].rearrange("p (g h) -> p g h", g=4),
                                                                         in1=gb[:].unsqueeze(1).to_broadcast([128, 4, 128]), op=ALU.mult),
                          reads=[("pa", p), "gb_" + gname] + [("ssqc", r4, g_) for g_ in range(4)], writes=[("xg", r3)])
                    S.add("dve", lambda e: e.reciprocal(out=ss[:], in_=ss[:]), reads=[("ssq", r4)], writes=[("ssq", r4)])
                    t1 = tq1[r1][:].rearrange("p (g h) -> p g h", g=4)
                    t2 = tq2[r2][:].rearrange("p (g h) -> p g h", g=4)
                    S.add("dve", lambda e: e.tensor_tensor(out=t1, in0=x3, in1=rd[:, 0:128].unsqueeze(1).to_broadcast([128, 4, 128]), op=ALU.mult),
                          reads=[("xg", r3), ("rdt", r3)], writes=[("tq1", r1)])
                    S.add("pool", lambda e: e.tensor_tensor(out=t2[:, :, 0:64], in0=x3[:, :, 64:128],
                                                            in1=rd[:, 128:192].unsqueeze(1).to_broadcast([128, 4, 64]), op=ALU.mult),
                          reads=[("xg", r3), ("rdt", r3)], writes=[("tq2", r2)])
                    S.add("pool", lambda e: e.tensor_tensor(out=t2[:, :, 64:128], in0=x3[:, :, 0:64],
                                                            in1=rd[:, 192:256].unsqueeze(1).to_broadcast([128, 4, 64]), op=ALU.mult),
                          reads=[("xg", r3), ("rdt", r3)], writes=[("tq2", r2)])
                    S.add("pool", lambda e: e.tensor_tensor(out=t1, in0=t1, in1=t2, op=ALU.add),
                          reads=[("tq1", r1), ("tq2", r2)], writes=[("tq1", r1)])
                    S.add("pool", lambda e: e.tensor_tensor(out=stq[:, t, :].rearrange("p (g h) -> p g h", g=4), in0=t1,
                                                            in1=ss[:].unsqueeze(2).to_broadcast([128, 4, 128]), op=ALU.mult),
                          reads=[("tq1", r1), ("ssq", r4)], writes=[("stq", t)])
                g0 = c0 // 128
                for t in range(NT):
                    transposes_out(stq[:, t, :], ("stq", t), 4, 128,
                                   lambda g0=g0, t=t, dst=dst: self.ap(dst)[g0:g0 + 4, :, t * 128:(t + 1) * 128].rearrange("g p n -> p g n"))

        def bias_mm_tile(t, wi, W, p):
            for k in range(NKC):
                S.add("pe", lambda e, k=k: e.matmul(pa[p][:, 0:W], hT[:, k, t * 128:(t + 1) * 128], wbs[wi][:, k, 0:W], start=(k == 0), stop=False),
                      reads=[("hT", t // 6), ("wb", wi)], writes=[("pa", p)])
            S.add("pe", lambda e: e.matmul(pa[p][:, 0:W], ones_b[:], brow[wi][:, 0:W], start=False, stop=True),
                  reads=["ones_b", ("brow", wi)], writes=[("pa", p)])

        if do("cq"):
            gcq = S.sb("gcq", [128, 512], F32)
            S.add("sp", lambda e: e.dma_start(out=gcq[:], in_=self.bcast_rows("mla_cq_norm_g", l * 512, 512)), writes=["gcq"], dma=True)
            wi = load_w(OFF_CQ, 512)
            S.add("pool", lambda e: e.dma_start(out=brow[wi][:], in_=bass.AP(self.tens("b_in"), l * D_IN + OFF_CQ, [[0, 1], [1, 512]])),
                  writes=[("brow", wi)], dma=True)
            for t in range(NT):
                n_ = qc["n"]
                qc["n"] += 1
                r4 = n_ % 4
                ss = ssq4[r4]
                p = nxt("pa", 5)
                bias_mm_tile(t, wi, 512, p)
                S.add("dve", lambda e: e.memset(ss[:], 0.0), reads=[("ssq", r4)], writes=[("ssq", r4)])
                S.add("act", lambda e: e.activation(out=junkq[:].rearrange("p g h -> p (g h)"), in_=pa[p][:], func=AF.Square, accum_out=ss[:, 0:1]),
                      reads=[("pa", p), ("ssq", r4)], writes=[("junkq", g_) for g_ in range(4)] + [("ssq", r4)])
                S.add("act", lambda e: e.activation(out=ss[:, 0:1], in_=ss[:, 0:1], func=AF.Sqrt, scale=1.0 / 512, bias=self.eps_col[:, 0:1]),
                      reads=[("ssq", r4)], writes=[("ssq", r4)])
                S.add("dve", lambda e: e.reciprocal(out=ss[:, 0:1], in_=ss[:, 0:1]), reads=[("ssq", r4)], writes=[("ssq", r4)])
                S.add("dve", lambda e: e.scalar_tensor_tensor(out=stq[:, t, :], in0=pa[p][:], scalar=ss[:, 0:1], in1=gcq[:], op0=ALU.mult, op1=ALU.mult),
                      reads=[("pa", p), ("ssq", r4), "gcq"], writes=[("stq", t)])
            for t in range(NT):
                transposes_out(stq[:, t, :], ("stq", t), 4, 128,
                               lambda t=t: self.ap("cqT")[:, :, t * 128:(t + 1) * 128].rearrange("g p n -> p g n"))

        if do("kr"):
            gkv = S.sb("gkv", [128, 320], F32)
            S.add("sp", lambda e: e.dma_start(out=gkv[:, 0:256], in_=self.bcast_rows("mla_ckv_norm_g", l * 256, 256)), writes=["gkv"], dma=True)
            S.add("sp", lambda e: e.dma_start(out=gkv[:, 256:320], in_=self.bcast_rows("mla_k_norm_g", l * 192 + 128, 64)), writes=["gkv"], dma=True)
            wi = load_w(OFF_CKV, 320)
            S.add("pool", lambda e: e.dma_start(out=brow[wi][:, 0:320], in_=bass.AP(self.tens("b_in"), l * D_IN + OFF_CKV, [[0, 1], [1, 320]])),
                  writes=[("brow", wi)], dma=True)
            for t in range(NT):
                n_ = qc["n"]
                qc["n"] += 1
                r4 = n_ % 4
                r2_ = n_ % 2
                ss = ssq4[r4]
                kx = xg4[r2_]
                p = nxt("pa", 5)
                bias_mm_tile(t, wi, 320, p)
                S.add("dve", lambda e: e.memset(ss[:], 0.0), reads=[("ssq", r4)], writes=[("ssq", r4)])
                S.add("act", lambda e: e.activation(out=junkq[:, 0:2, :].rearrange("p g h -> p (g h)"), in_=pa[p][:, 0:256], func=AF.Square, accum_out=ss[:, 0:1]),
                      reads=[("pa", p), ("ssq", r4)], writes=[("junkq", 0), ("junkq", 1), ("ssqa", r4)])
                S.add("act", lambda e: e.activation(out=junkq[:, 2, 0:64], in_=pa[p][:, 256:320], func=AF.Square, accum_out=ss[:, 1:2]),
                      reads=[("pa", p), ("ssq", r4)], writes=[("junkq", 2), ("ssqb", r4)])
                S.add("act", lambda e: e.activation(out=ss[:, 0:1], in_=ss[:, 0:1], func=AF.Sqrt, scale=1.0 / 256, bias=self.eps_col[:, 0:1]),
                      reads=[("ssqa", r4), ("ssqb", r4), ("ssq", r4)], writes=[("ssq", r4), ("ssqa", r4)])
                S.add("act", lambda e: e.activation(out=ss[:, 1:2], in_=ss[:, 1:2], func=AF.Sqrt, scale=1.0 / 64, bias=self.eps_col[:, 0:1]),
                      reads=[("ssq", r4), ("ssqb", r4)], writes=[("ssq", r4), ("ssqb", r4)])
                S.add("dve", lambda e: e.reciprocal(out=ss[:, 0:2], in_=ss[:, 0:2]), reads=[("ssq", r4)], writes=[("ssq", r4)])
                S.add("dve", lambda e: e.scalar_tensor_tensor(out=stq[:, t, 0:256], in0=pa[p][:, 0:256], scalar=ss[:, 0:1], in1=gkv[:, 0:256],
                                                              op0=ALU.mult, op1=ALU.mult),
                      reads=[("pa", p), ("ssq", r4), "gkv"], writes=[("stq", t)])
                S.add("dve", lambda e: e.scalar_tensor_tensor(out=kx[:, 0:64], in0=pa[p][:, 256:320], scalar=ss[:, 1:2], in1=gkv[:, 256:320],
                                                              op0=ALU.mult, op1=ALU.mult),
                      reads=[("pa", p), ("ssq", r4), "gkv"], writes=[("xg", r2_)])
                x1, x2 = kx[:, 0:32], kx[:, 32:64]
                cs, sn = rml[:, t, 0:32], rml[:, t, 32:64]
                ta, tb_ = tq1[0][:, 0:32], tq2[0][:, 0:32]
                pk = lambda fn, rd_, wr_: S.add("pool", fn, reads=rd_, writes=wr_)
                pk(lambda e: e.tensor_tensor(out=ta, in0=x1, in1=cs, op=ALU.mult), [("xg", r2_), "rml"], [("tq1", 0)])
                pk(lambda e: e.tensor_tensor(out=tb_, in0=x2, in1=sn, op=ALU.mult), [("xg", r2_), "rml"], [("tq2", 0)])
                pk(lambda e: e.tensor_tensor(out=stq[:, t, 256:288], in0=ta, in1=tb_, op=ALU.subtract), [("tq1", 0), ("tq2", 0)], [("stq", t)])
                pk(lambda e: e.tensor_tensor(out=ta, in0=x1, in1=sn, op=ALU.mult), [("xg", r2_), "rml"], [("tq1", 0)])
                pk(lambda e: e.tensor_tensor(out=tb_, in0=x2, in1=cs, op=ALU.mult), [("xg", r2_), "rml"], [("tq2", 0)])
                pk(lambda e: e.tensor_tensor(out=stq[:, t, 288:320], in0=ta, in1=tb_, op=ALU.add), [("tq1", 0), ("tq2", 0)], [("stq", t)])
            for t in range(NT):
                transposes_out(stq[:, t, :], ("stq", t), 2, 128,
                               lambda t=t: self.ap("ckvT")[:, :, t * 128:(t + 1) * 128].rearrange("g p n -> p g n"))
                q = nxt("pt", 2)
                S.add("pe", lambda e, q=q: e.transpose(out=pt[q][0:64, 0, :], in_=stq[:, t, 256:320], identity=self.ident_b[:]),
                      reads=[("stq", t)], writes=[("pt", q)])
                r = nxt("trs", 4)
                S.add("act", lambda e, r=r, q=q: e.copy(out=trs[r][0:64, 0, :], in_=pt[q][0:64, 0, :]), reads=[("pt", q)], writes=[("trs", r)])
                store("sp", self.ap("akrT")[:, t * 128:(t + 1) * 128], trs[r][0:64, 0, :], [("trs", r)])

        tb = [(0, 512), (512, 512), (1024, 512), (1536, 512), (2048, 256)]

        def mm_fm(wi, j, t0, n):
            p = nxt("pa", 5)
            for k in range(NKC):
                S.add("pe", lambda e, k=k: e.matmul(pa[p][:, 0:n], wbs[wi][:, k, j * 128:(j + 1) * 128], hT[:, k, t0:t0 + n],
                                                    start=(k == 0), stop=(k == NKC - 1)),
                      reads=hkeys + [("wb", wi)], writes=[("pa", p)])
            return p

        if do("g"):
            bcg = self.rows_to_cols(S, "bcg", bass.AP(self.tens("b_in"), l * D_IN + OFF_G, [[128, 48], [1, 128]]), 48, 1, pmisc, "pmisc")
            for c0 in range(0, 6144, 512):
                wi = load_w(OFF_G + c0, 512, bias=False)
                for j in range(4):
                    ch = c0 // 128 + j
                    for (t0, n) in tb:
                        p = mm_fm(wi, j, t0, n)
                        b = nxt("stb", 3)
                        S.add("act", lambda e, p=p, b=b, n=n, ch=ch: e.activation(out=stb[b][:, 0:n], in_=pa[p][:, 0:n], func=AF.Sigmoid,
                                                                                  bias=bcg[:, 0, ch:ch + 1]),
                              reads=[("pa", p), "bcg_c"], writes=[("stb", b)])
                        store("sp", self.ap("gT")[ch, :, t0:t0 + n], stb[b][:, 0:n], [("stb", b)])

        if do("mqk"):
            bcq = self.rows_to_cols(S, "bcq", bass.AP(self.tens("b_in"), l * D_IN + OFF_MQK, [[128, 16], [1, 128]]), 16, 1, pmisc, "pmisc")
            cvb = self.rows_to_cols(S, "cvb", bass.AP(self.tens("m_conv_b"), l * 2048, [[128, 16], [1, 128]]), 16, 1, pmisc, "pmisc")
            cvw = self.rows_to_cols(S, "cvw", self.ap("m_conv_w")[l], 5, 16, pmisc, "pmisc")
            pre = [S.sb("pre%d" % i, [128, 2304 + 8], BF16) for i in range(2)]
            sl = [S.sb("sil%d" % i, [128, 2304], BF16) for i in range(2)]
            mks = [S.sb("mks%d" % i, [128, 6, 128], BF16) for i in range(2)]
            dg = [S.sb("dg%d" % i, [128, 5, 128], BF16) for i in range(2)]
            for i in range(2):
                S.add("pool", lambda e, i=i: e.memset(pre[i][:], 0.0), writes=[("pre", i)])
            oblocks = [(0, 256, 2)] + [(256 + 512 * j, 512, 262 + 512 * j) for j in range(4)]
            for c0 in range(0, 2048, 512):
                wi = load_w(OFF_MQK + c0, 512, bias=False)
                for j in range(4):
                    ch = c0 // 128 + j
                    i = ch % 2
                    for jj in range(5):
                        S.add("dve", lambda e, jj=jj: e.tensor_scalar(out=dg[i][:, jj, :], in0=self.ident_b[:], scalar1=cvw[:, ch, jj:jj + 1],
                                                                       scalar2=None, op0=ALU.mult),
                              reads=["cvw_c"], writes=[("dg", i)])
                    for (t0, n) in tb:
                        p = mm_fm(wi, j, t0, n)
                        po = (2 + t0) if t0 < 256 else (262 + t0 - 256)
                        if t0 == 0:
                            S.add("act", lambda e, p=p: e.activation(out=pre[i][:, 2:258], in_=pa[p][:, 0:256], func=AF.Identity,
                                                                     bias=bcq[:, 0, ch:ch + 1]),
                                  reads=[("pa", p), "bcq_c"], writes=[("pre", i)])
                            S.add("act", lambda e, p=p: e.activation(out=pre[i][:, 262:518], in_=pa[p][:, 256:512], func=AF.Identity,
                                                                     bias=bcq[:, 0, ch:ch + 1]),
                                  reads=[("pa", p), "bcq_c"], writes=[("pre", i)])
                        elif (t0 // 512) % 2 == 1:
                            S.add("act", lambda e, p=p, po=po, n=n: e.activation(out=pre[i][:, po:po + n], in_=pa[p][:, 0:n],
                                                                                 func=AF.Identity, bias=bcq[:, 0, ch:ch + 1]),
                                  reads=[("pa", p), "bcq_c"], writes=[("pre", i)])
                        else:
                            S.add("dve", lambda e, p=p, po=po, n=n: e.tensor_scalar(out=pre[i][:, po:po + n], in0=pa[p][:, 0:n],
                                                                                    scalar1=bcq[:, 0, ch:ch + 1], scalar2=None, op0=ALU.add),
                                  reads=[("pa", p), "bcq_c"], writes=[("pre", i)])
                    for (ts, n, po) in oblocks:
                        p = nxt("pa", 5)
                        for jj in range(5):
                            S.add("pe", lambda e, jj=jj: e.matmul(pa[p][:, 0:n], dg[i][:, jj, :], pre[i][:, po - 2 + jj:po - 2 + jj + n],
                                                                  start=(jj == 0), stop=(jj == 4)),
                                  reads=[("dg", i), ("pre", i)], writes=[("pa", p)])
                        S.add("act", lambda e, p=p, ts=ts, n=n: e.activation(out=sl[i][:, ts:ts + n], in_=pa[p][:, 0:n], func=AF.Silu,
                                                                             bias=cvb[:, 0, ch:ch + 1]),
                              reads=[("pa", p), "cvb_c"], writes=[("sl", i)])
                    if ch < 8:
                        S.add("pool", lambda e, i=i: e.tensor_scalar_mul(sl[i][:], sl[i][:], 1.0 / 16.0), reads=[("sl", i)], writes=[("sl", i)])
                    store("sp", self.ap("mqkT")[ch], sl[i][:], [("sl", i)])
                    if ch >= 8:
                        for t3 in range(0, NT, 6):
                            q = nxt("pt", 2)
                            for tt in range(6):
                                t = t3 + tt
                                S.add("pe", lambda e, q=q, tt=tt, t=t, i=i: e.transpose(out=pt[q][:, tt, :], in_=sl[i][:, t * 128:(t + 1) * 128],
                                                                                        identity=self.ident_b[:]),
                                      reads=[("sl", i)], writes=[("pt", q)])
                            m = nxt("trs", 2)
                            S.add("dve", lambda e, q=q, m=m: e.tensor_copy(out=mks[m][:], in_=pt[q][:, 0:6, :]), reads=[("pt", q)], writes=[("mks", m)])
                            store("sp", self.ap("mk_tm")[t3 * 128:(t3 + 6) * 128, (ch - 8) * 128:(ch - 7) * 128].rearrange("(t p) c -> p t c", p=128),
                                  mks[m][:], [("mks", m)])
        S.emit()
        S.close()


    def phase_mla_up(self, l):
        nc = self.nc
        S = Sched(nc)
        cqT = S.sb("cqTs", [128, 4, NTOK], BF16)
        ckvT = S.sb("ckvTs", [128, 2, NTOK], BF16)
        wuq = S.sb("wuq", [128, 4, 1536], BF16)
        wukv = S.sb("wukv", [128, 2, 2048], BF16)
        gq = S.sb("gq", [128, 192], F32)
        gk = S.sb("gk", [128, 128], F32)
        rml = S.sb("rml2", [128, NT, 64], F32)
        pq = [S.ps("pq%d" % i, [128, 512], F32) for i in range(4)]
        pt = [S.ps("ptm%d" % i, [128, 8, 128], BF16) for i in range(3)]
        st = [S.sb("stq%d" % i, [128, 2048], F32) for i in range(2)]
        sq = S.sb("sqq", [128, 2048], F32)
        qb = [S.sb("qb%d" % i, [128, 2048], BF16) for i in range(2)]
        ssq = S.sb("ssq", [128, 16], F32)
        tmpa = S.sb("tmpa", [128, 256], F32)
        tmpb = S.sb("tmpb", [128, 256], F32)
        trn = [S.sb("trn%d" % i, [128, 8, 128], BF16) for i in range(2)]
        trr = [S.sb("trr%d" % i, [64, 8, 128], BF16) for i in range(2)]
        S.add("sp", lambda e: e.dma_start(out=cqT[:], in_=self.ap("cqT").rearrange("g p n -> p g n")), writes=["cqT"], dma=True)
        S.add("sp", lambda e: e.dma_start(out=ckvT[:], in_=self.ap("ckvT").rearrange("g p n -> p g n")), writes=["ckvT"], dma=True)
        S.add("pool", lambda e: e.dma_start(out=wuq[:], in_=self.ap("mla_w_uq")[l].rearrange("(k p) w -> p k w", p=128)), writes=["wuq"], dma=True)
        S.add("pool", lambda e: e.dma_start(out=wukv[:], in_=self.ap("mla_w_ukv")[l].rearrange("(k p) w -> p k w", p=128)), writes=["wukv"], dma=True)
        S.add("sp", lambda e: e.dma_start(out=gq[:], in_=self.bcast_rows("mla_q_norm_g", l * 192, 192)), writes=["gq"], dma=True)
        S.add("sp", lambda e: e.dma_start(out=gk[:], in_=self.bcast_rows("mla_k_norm_g", l * 192, 128)), writes=["gk"], dma=True)
        S.add("pool", lambda e: e.tensor_scalar_mul(gq[:], gq[:], 192.0 ** -0.5), reads=["gq"], writes=["gq"])
        S.add("sp", lambda e: e.dma_start(out=rml[:], in_=self.ap("rope_mla").rearrange("(t p) c -> p t c", p=128)), writes=["rml"], dma=True)
        pipeu = Pipe(2)

        def make_q(t):
            i = 0
            tok = slice(t * 128, (t + 1) * 128)
            s3 = st[i][:, 0:1536].rearrange("p (h d) -> p h d", h=8)
            q3 = sq[:, 0:1536].rearrange("p (h d) -> p h d", h=8)
            b3 = qb[i][:, 0:1536].rearrange("p (h d) -> p h d", h=8)

            def s0():
                    for cb in range(3):
                        for k in range(4):
                            S.add("pe", lambda e, cb=cb, k=k: e.matmul(pq[cb][:], cqT[:, k, tok], wuq[:, k, cb * 512:(cb + 1) * 512],
                                                                       start=(k == 0), stop=(k == 3)),
                                  reads=["cqT", "wuq"], writes=[("pq", cb)])
                        eng = ("act", "dve", "act")[cb]
                        if eng == "act":
                            S.add("act", lambda e, cb=cb: e.copy(out=st[i][:, cb * 512:(cb + 1) * 512], in_=pq[cb][:]),
                                  reads=[("pq", cb)], writes=[("st", i)])
                        else:
                            S.add("dve", lambda e, cb=cb: e.tensor_copy(out=st[i][:, cb * 512:(cb + 1) * 512], in_=pq[cb][:]),
                                  reads=[("pq", cb)], writes=[("st", i)])
                    s3 = st[i][:, 0:1536].rearrange("p (h d) -> p h d", h=8)
                    q3 = sq[:, 0:1536].rearrange("p (h d) -> p h d", h=8)
                    b3 = qb[i][:, 0:1536].rearrange("p (h d) -> p h d", h=8)
                    S.add("pool", lambda e: e.tensor_tensor(out=sq[:, 0:1536], in0=st[i][:, 0:1536], in1=st[i][:, 0:1536], op=ALU.mult),
                          reads=[("st", i)], writes=["sq"])
                    S.add("dve", lambda e: e.tensor_reduce(out=ssq[:, 0:8], in_=q3[:, :, 0:128], axis=AX.X, op=ALU.add), reads=["sq"], writes=["ssq"])
                    S.add("dve", lambda e: e.tensor_reduce(out=ssq[:, 8:16], in_=q3[:, :, 128:192], axis=AX.X, op=ALU.add), reads=["sq"], writes=["ssq"])
                    self.rstd(S, ssq[:, 0:8], "ssq", 128)
                    self.rstd(S, ssq[:, 8:16], "ssq", 64)
                    S.add("pool", lambda e: e.tensor_tensor(out=s3[:, :, 0:128], in0=s3[:, :, 0:128],
                                                            in1=ssq[:, 0:8].unsqueeze(2).to_broadcast([128, 8, 128]), op=ALU.mult),
                          reads=[("st", i), "ssq"], writes=[("st", i)])
                    S.add("pool", lambda e: e.tensor_tensor(out=s3[:, :, 128:192], in0=s3[:, :, 128:192],
                                                            in1=ssq[:, 8:16].unsqueeze(2).to_broadcast([128, 8, 64]), op=ALU.mult),
                          reads=[("st", i), "ssq"], writes=[("st", i)])
                    S.add("dve", lambda e: e.tensor_tensor(out=s3, in0=s3, in1=gq[:].unsqueeze(1).to_broadcast([128, 8, 192]), op=ALU.mult),
                          reads=[("st", i), "gq"], writes=[("st", i)])
                    S.add("act", lambda e: e.copy(out=b3[:, :, 0:128], in_=s3[:, :, 0:128]), reads=[("st", i)], writes=[("qb", i)])
                    x1 = s3[:, :, 128:160]
                    x2 = s3[:, :, 160:192]
                    cb_ = rml[:, t, 0:32].unsqueeze(1).to_broadcast([128, 8, 32])
                    sb_ = rml[:, t, 32:64].unsqueeze(1).to_broadcast([128, 8, 32])
                    ta = tmpa[:].rearrange("p (g h) -> p g h", g=8)
                    tb_ = tmpb[:].rearrange("p (g h) -> p g h", g=8)
                    S.add("pool", lambda e: e.tensor_tensor(out=ta, in0=x1, in1=cb_, op=ALU.mult), reads=[("st", i), "rml"], writes=["tmpa"])
                    S.add("pool", lambda e: e.tensor_tensor(out=tb_, in0=x2, in1=sb_, op=ALU.mult), reads=[("st", i), "rml"], writes=["tmpb"])
                    S.add("dve", lambda e: e.tensor_tensor(out=b3[:, :, 128:160], in0=ta, in1=tb_, op=ALU.subtract), reads=["tmpa", "tmpb"], writes=[("qb", i)])
                    S.add("pool", lambda e: e.tensor_tensor(out=ta, in0=x1, in1=sb_, op=ALU.mult), reads=[("st", i), "rml"], writes=["tmpa"])
                    S.add("pool", lambda e: e.tensor_tensor(out=tb_, in0=x2, in1=cb_, op=ALU.mult), reads=[("st", i), "rml"], writes=["tmpb"])
                    S.add("dve", lambda e: e.tensor_tensor(out=b3[:, :, 160:192], in0=ta, in1=tb_, op=ALU.add), reads=["tmpa", "tmpb"], writes=[("qb", i)])

            def s1():
                    for h in range(8):
                        S.add("pe", lambda e, h=h: e.transpose(out=pt[0][:, h, :], in_=qb[i][:, h * 192:h * 192 + 128], identity=self.ident_b[:]),
                              reads=[("qb", i)], writes=[("pt", 0)])
                    for h in range(8):
                        S.add("pe", lambda e, h=h: e.transpose(out=pt[1][0:64, h, :], in_=qb[i][:, h * 192 + 128:(h + 1) * 192], identity=self.ident_b[:]),
                              reads=[("qb", i)], writes=[("pt", 1)])
                    S.add("act", lambda e: e.copy(out=trn[i][:], in_=pt[0][:]), reads=[("pt", 0)], writes=[("trn", i)])
                    S.add("dve", lambda e: e.tensor_copy(out=trr[i][:], in_=pt[1][0:64, :, :]), reads=[("pt", 1)], writes=[("trr", i)])
                    S.add("sp", lambda e: e.dma_start(out=self.ap("aqTn")[:, :, tok].rearrange("g p n -> p g n"), in_=trn[i][:]), reads=[("trn", i)], dma=True)
                    S.add("sp", lambda e: e.dma_start(out=self.ap("aqTr")[:, :, tok].rearrange("g p n -> p g n"), in_=trr[i][:]), reads=[("trr", i)], dma=True)

            return [s0, s1]

        def make_kv(t):
            i = 1
            tok = slice(t * 128, (t + 1) * 128)
            k3 = st[i][:].rearrange("p (h d) -> p h d", h=8)
            kq3 = sq[:].rearrange("p (h d) -> p h d", h=8)
            kb3 = qb[i][:].rearrange("p (h d) -> p h d", h=8)

            def s0():
                    for cb in range(4):
                        for k in range(2):
                            S.add("pe", lambda e, cb=cb, k=k: e.matmul(pq[cb][:], ckvT[:, k, tok], wukv[:, k, cb * 512:(cb + 1) * 512],
                                                                       start=(k == 0), stop=(k == 1)),
                                  reads=["ckvT", "wukv"], writes=[("pq", cb)])
                        if cb % 2 == 0:
                            S.add("act", lambda e, cb=cb: e.copy(out=st[i][:, cb * 512:(cb + 1) * 512], in_=pq[cb][:]),
                                  reads=[("pq", cb)], writes=[("st", i)])
                        else:
                            S.add("dve", lambda e, cb=cb: e.tensor_copy(out=st[i][:, cb * 512:(cb + 1) * 512], in_=pq[cb][:]),
                                  reads=[("pq", cb)], writes=[("st", i)])
                    k3 = st[i][:].rearrange("p (h d) -> p h d", h=8)
                    kq3 = sq[:].rearrange("p (h d) -> p h d", h=8)
                    kb3 = qb[i][:].rearrange("p (h d) -> p h d", h=8)
                    S.add("pool", lambda e: e.tensor_tensor(out=kq3[:, :, 0:128], in0=k3[:, :, 0:128], in1=k3[:, :, 0:128], op=ALU.mult),
                          reads=[("st", i)], writes=["sq"])
                    S.add("dve", lambda e: e.tensor_reduce(out=ssq[:, 0:8], in_=kq3[:, :, 0:128], axis=AX.X, op=ALU.add), reads=["sq"], writes=["ssq"])
                    self.rstd(S, ssq[:, 0:8], "ssq", 128)
                    S.add("pool", lambda e: e.tensor_tensor(out=k3[:, :, 0:128], in0=k3[:, :, 0:128],
                                                            in1=ssq[:, 0:8].unsqueeze(2).to_broadcast([128, 8, 128]), op=ALU.mult),
                          reads=[("st", i), "ssq"], writes=[("st", i)])
                    S.add("dve", lambda e: e.tensor_tensor(out=kb3[:, :, 0:128], in0=k3[:, :, 0:128],
                                                           in1=gk[:].unsqueeze(1).to_broadcast([128, 8, 128]), op=ALU.mult),
                          reads=[("st", i), "gk"], writes=[("qb", i)])
                    S.add("act", lambda e: e.copy(out=kb3[:, :, 128:256], in_=k3[:, :, 128:256]), reads=[("st", i)], writes=[("qb", i)])

            def s1():
                    for h in range(8):
                        S.add("pe", lambda e, h=h: e.transpose(out=pt[2][:, h, :], in_=qb[i][:, h * 256:h * 256 + 128], identity=self.ident_b[:]),
                              reads=[("qb", i)], writes=[("pt", 2)])
                    S.add("act", lambda e: e.copy(out=trn[i][:], in_=pt[2][:]), reads=[("pt", 2)], writes=[("trn", i)])
                    S.add("sp", lambda e: e.dma_start(out=self.ap("akT")[:, :, tok].rearrange("g p n -> p g n"), in_=trn[i][:]), reads=[("trn", i)], dma=True)
                    S.add("sp", lambda e: e.dma_start(out=self.ap("av")[tok, :].rearrange("p (h d) -> p h d", h=8), in_=kb3[:, :, 128:256]),
                          reads=[("qb", i)], dma=True)

            return [s0, s1]

        for t in range(NT):
            pipeu.push(make_q(t))
            pipeu.push(make_kv(t))
        pipeu.drain()
        S.emit()
        S.close()

    def phase_attn(self, l, kind, with_ctx, heads=None):
        nc = self.nc
        S = Sched(nc)
        lam_init = 0.8 - 0.6 * math.exp(-0.3 * l)
        if kind == 0:
            nh, dv, nset = 4, 256, 2
        else:
            nh, dv, nset = 8, 128, 1
        qT = [[S.sb("aq%d_%d" % (i, s), [128, NTOK], BF16) for s in range(nset)] for i in range(2)]
        kT = [[S.sb("ak%d_%d" % (i, s), [128, NTOK], BF16) for s in range(nset)] for i in range(2)]
        if kind == 1:
            qTr = [S.sb("aqr%d" % i, [64, NTOK], BF16) for i in range(2)]
            kTr = S.sb("akr", [64, NTOK], BF16)
            S.add("sp", lambda e: e.dma_start(out=kTr[:], in_=self.ap("akrT")), writes=["kTr"], dma=True)
        va = [S.sb("va%d" % i, [128, NT, dv + 1], BF16) for i in range(2)]
        psS = [S.ps("psS%d" % i, [128, 512], F32) for i in range(3)]
        psO = [S.ps("psO%d" % i, [128, 512], F32) for i in range(4)]
        ptr = S.ps("ptr", [128, 8, 128], BF16)
        Pb = [S.sb("Pb%d" % i, [128, 512], BF16) for i in range(3)]
        rr = S.sb("rr", [128, 4], F32)
        t1 = S.sb("t1", [128, 256], F32)
        ob = S.sb("ob", [128, 256], F32)
        obb = [S.sb("obb%d" % i, [128, 256], BF16) for i in range(2)]
        ssn = S.sb("ssn", [128, 1], F32)
        junk = S.sb("junka", [128, 256], BF16)
        yst = [S.sb("yst%d" % i, [128, 2, 128], BF16) for i in range(2)]
        for i in range(2):
            S.add("pool", lambda e, i=i: e.memset(va[i][:, :, dv:dv + 1], 1.0), writes=[("va", i)])
        S.add("pool", lambda e: e.memset(ssn[:], 0.0), writes=["ssn"])
        if kind == 0:
            lamb = S.sb("lamb", [128, 512], F32)
            lamt = S.sb("lamt", [128, 256], F32)
            lamc = S.sb("lamc", [128, 2], F32)
            gsub = S.sb("gsub", [128, 256], F32)
            S.add("sp", lambda e: e.dma_start(out=lamb[:], in_=self.bcast_rows("da_lambda", l * 512, 512)), writes=["lamb"], dma=True)
            S.add("sp", lambda e: e.dma_start(out=gsub[:], in_=self.bcast_rows("da_subln_g", l * 256, 256)), writes=["gsub"], dma=True)
            S.add("pool", lambda e: e.tensor_scalar_mul(gsub[:], gsub[:], float(1.0 - lam_init)), reads=["gsub"], writes=["gsub"])
            l4 = lamb[:].rearrange("p (a b d) -> p a b d", a=2, b=2)
            S.add("dve", lambda e: e.tensor_tensor(out=lamt[:].rearrange("p (a d) -> p a d", a=2), in0=l4[:, :, 0, :], in1=l4[:, :, 1, :], op=ALU.mult),
                  reads=["lamb"], writes=["lamt"])
            S.add("dve", lambda e: e.tensor_reduce(out=lamc[:], in_=lamt[:].rearrange("p (a d) -> p a d", a=2), axis=AX.X, op=ALU.add),
                  reads=["lamt"], writes=["lamc"])
            S.add("act", lambda e: e.activation(out=lamc[:], in_=lamc[:], func=AF.Exp), reads=["lamc"], writes=["lamc"])
            S.add("dve", lambda e: e.tensor_tensor(out=lamc[:, 0:1], in0=lamc[:, 1:2], in1=lamc[:, 0:1], op=ALU.subtract), reads=["lamc"], writes=["lamc"])
            S.add("dve", lambda e: e.tensor_scalar_add(lamc[:, 0:1], lamc[:, 0:1], float(-lam_init)), reads=["lamc"], writes=["lamc"])
        st = {"P": 0, "O": 0, "y": 0}
        QBL = 256 if kind == 0 else 512
        qblocks = [(256 + j * QBL, QBL, [t for t in range(NT)]) for j in range(2048 // QBL)]
        if with_ctx:
            qblocks.append((0, 256, [0, 1]))
        for h in (range(nh) if heads is None else heads):
            i = h % 2
            for s in range(nset):
                if kind == 0:
                    qsrc = self.ap("dqT")[2 * h + s]
                    ksrc = self.ap("dkT")[2 * h + s]
                else:
                    qsrc = self.ap("aqTn")[h]
                    ksrc = self.ap("akT")[h]
                S.add("sp", lambda e, s=s, qsrc=qsrc: e.dma_start(out=qT[i][s][:], in_=qsrc), writes=[("qT", i, s)], dma=True)
                S.add("sp", lambda e, s=s, ksrc=ksrc: e.dma_start(out=kT[i][s][:], in_=ksrc), writes=[("kT", i, s)], dma=True)
            if kind == 1:
                S.add("sp", lambda e: e.dma_start(out=qTr[i][:], in_=self.ap("aqTr")[h]), writes=[("qTr", i)], dma=True)
            vsrc = self.ap("dv" if kind == 0 else "av")[:, h * dv:(h + 1) * dv].rearrange("(t p) d -> p t d", p=128)
            S.add("sp", lambda e, vsrc=vsrc: e.dma_start(out=va[i][:, :, 0:dv], in_=vsrc), writes=[("va", i)], dma=True)
            for (q0, QB, ktiles) in qblocks:
                nsub = QB // 128
                if kind == 0:
                    Oi = lambda s, sub: s * 2 + sub
                else:
                    Oi = lambda s, sub: sub
                pidx = []
                for n in range(len(ktiles)):
                    pidx.append((st["P"] % 3, st["P"] % 3))
                    st["P"] += 1

                def scores(n):
                    ps_i, pb = pidx[n]
                    kt = ktiles[n]
                    ks = slice(kt * 128, (kt + 1) * 128)
                    for s in range(nset):
                        if kind == 0:
                            S.add("pe", lambda e, s=s: e.matmul(psS[ps_i][:, s * QB:(s + 1) * QB], kT[i][s][:, ks], qT[i][s][:, q0:q0 + QB],
                                                                start=True, stop=True),
                                  reads=[("kT", i, s), ("qT", i, s)], writes=[("psS", ps_i, s)])
                        else:
                            S.add("pe", lambda e: e.matmul(psS[ps_i][:, 0:QB], kT[i][0][:, ks], qT[i][0][:, q0:q0 + QB], start=True, stop=False),
                                  reads=[("kT", i, 0), ("qT", i, 0)], writes=[("psS", ps_i, 0)])
                            S.add("pe", lambda e: e.matmul(psS[ps_i][:, 0:QB], kTr[:, ks], qTr[i][:, q0:q0 + QB], start=False, stop=True),
                                  reads=["kTr", ("qTr", i)], writes=[("psS", ps_i, 0)])

                scores(0)
                if len(ktiles) > 1:
                    scores(1)
                for n, kt in enumerate(ktiles):
                    ps_i, pb = pidx[n]
                    if n + 2 < len(ktiles):
                        scores(n + 2)
                    S.add("act", lambda e: e.activation(out=Pb[pb][:, 0:nset * QB], in_=psS[ps_i][:, 0:nset * QB], func=AF.Exp),
                          reads=[("psS", ps_i, s) for s in range(nset)], writes=[("Pb", pb)])
                    for s in range(nset):
                        for sub in range(nsub):
                            o = Oi(s, sub)
                            S.add("pe", lambda e, s=s, sub=sub, o=o: e.matmul(psO[o][:, 0:dv + 1], Pb[pb][:, s * QB + sub * 128:s * QB + (sub + 1) * 128],
                                                                              va[i][:, kt, :], start=(n == 0), stop=(n == len(ktiles) - 1)),
                                  reads=[("Pb", pb), ("va", i)], writes=[("psO", o)])
                for sub in range(nsub):
                    tq = q0 + sub * 128
                    y = st["y"] % 2
                    st["y"] += 1
                    if kind == 0:
                        o1, o2 = Oi(0, sub), Oi(1, sub)
                        S.add("dve", lambda e: e.reciprocal(out=rr[:, 0:1], in_=psO[o1][:, dv:dv + 1]), reads=[("psO", o1)], writes=["rr"])
                        S.add("dve", lambda e: e.reciprocal(out=rr[:, 1:2], in_=psO[o2][:, dv:dv + 1]), reads=[("psO", o2)], writes=["rr"])
                        S.add("dve", lambda e: e.tensor_tensor(out=rr[:, 1:2], in0=rr[:, 1:2], in1=lamc[:, 0:1], op=ALU.mult),
                              reads=["rr", "lamc"], writes=["rr"])
                        S.add("act", lambda e: e.activation(out=t1[:], in_=psO[o1][:, 0:dv], func=AF.Copy, scale=rr[:, 0:1]),
                              reads=[("psO", o1), "rr"], writes=["t1"])
                        S.add("dve", lambda e: e.scalar_tensor_tensor(out=ob[:], in0=psO[o2][:, 0:dv], scalar=rr[:, 1:2], in1=t1[:],
                                                                      op0=ALU.mult, op1=ALU.add),
                              reads=[("psO", o2), "rr", "t1"], writes=["ob"])
                        S.add("act", lambda e: e.activation(out=junk[:], in_=ob[:], func=AF.Square, accum_out=ssn[:, 0:1]),
                              reads=["ob", "ssn"], writes=["junk", "ssn"])
                        self.rstd(S, ssn[:, 0:1], "ssn", 256)
                        S.add("dve", lambda e: e.scalar_tensor_tensor(out=obb[y][:], in0=ob[:], scalar=ssn[:, 0:1], in1=gsub[:],
                                                                      op0=ALU.mult, op1=ALU.mult),
                              reads=["ob", "ssn", "gsub"], writes=[("obb", y)])
                        S.add("pool", lambda e: e.memset(ssn[:], 0.0), reads=["ssn"], writes=["ssn"])
                        nch = 2
                    else:
                        o1 = Oi(0, sub)
                        S.add("dve", lambda e: e.reciprocal(out=rr[:, 0:1], in_=psO[o1][:, dv:dv + 1]), reads=[("psO", o1)], writes=["rr"])
                        S.add("act", lambda e: e.activation(out=obb[y][:, 0:128], in_=psO[o1][:, 0:dv], func=AF.Copy, scale=rr[:, 0:1]),
                              reads=[("psO", o1), "rr"], writes=[("obb", y)])
                        nch = 1
                    for c in range(nch):
                        S.add("pe", lambda e, c=c: e.transpose(out=ptr[:, c, :], in_=obb[y][:, c * 128:(c + 1) * 128], identity=self.ident_b[:]),
                              reads=[("obb", y)], writes=["ptr"])
                    S.add("dve", lambda e: e.tensor_copy(out=yst[y][:, 0:nch, :], in_=ptr[:, 0:nch, :]), reads=["ptr"], writes=[("yst", y)])
                    br = 1 if kind == 0 else 2
                    S.add("sp", lambda e: e.dma_start(out=self.ap("ysT")[br, h * nch:(h + 1) * nch, :, tq:tq + 128].rearrange("c p n -> p c n"),
                                                      in_=yst[y][:, 0:nch, :]), reads=[("yst", y)], dma=True)
        S.emit()
        S.close()

    def phase_mlstm(self, l, heads=None):
        nc = self.nc
        NEG = {}
        pos_of = {0: lambda t: t, 1: lambda t: (1 - t) if t < 2 else 2 + (NT - 1 - t)}
        tile_at = {0: lambda p: p, 1: lambda p: (1 - p) if p < 2 else NT - 1 - (p - 2)}
        g2 = contextlib.ExitStack()
        UT = [g2.enter_context(nc.sbuf_tensor("UT%d_%d" % (l, d), [128, NT, 12], F32)) for d in range(2)]
        KB = [g2.enter_context(nc.sbuf_tensor("KB%d_%d" % (l, d), [128, 4, NT], F32)) for d in range(2)]
        S = Sched(nc)
        cj = S.sb("cj", [128, 128], F32)
        sel = S.sb("sel", [4, 512], F32)
        G = S.sb("G", [128, NT, 16], F32)
        IG = S.sb("IG", [128, NT, 8], F32)
        LF = S.sb("LF", [128, NT, 8], F32)
        ones = S.sb("ones", [128, 1], F32)
        zr = S.sb("zr", [4, NTOK], F32)
        S.add("sp", lambda e: e.dma_start(out=cj[:], in_=self.ap("consts")[:, 128:256]), writes=["cj"], dma=True)
        S.add("sp", lambda e: e.dma_start(out=sel[:], in_=self.ap("consts2")), writes=["sel"], dma=True)
        S.add("sp", lambda e: e.dma_start(out=G[:], in_=self.ap("mg").rearrange("(t p) c -> p t c", p=128)), writes=["G"], dma=True)
        S.add("pool", lambda e: e.memset(ones[:], 1.0), writes=["ones"])
        S.add("pool", lambda e: e.memset(zr[:], 0.0), writes=["zr"])
        G4 = G[:].rearrange("p t (d x c) -> p t d x c", d=2, x=2)
        IG3 = IG[:].rearrange("p t (d c) -> p t d c", d=2)
        LF3 = LF[:].rearrange("p t (d c) -> p t d c", d=2)
        for d in range(2):
            S.add("dve", lambda e, d=d: e.tensor_copy(out=IG3[:, :, d, :], in_=G4[:, :, d, 0, :]), reads=["G"], writes=["IG"])
            S.add("act", lambda e, d=d: e.activation(out=LF3[:, :, d, :], in_=G4[:, :, d, 1, :], func=AF.Exp, scale=-1.0), reads=["G"], writes=["LF"])
        S.add("act", lambda e: e.activation(out=LF[:], in_=LF[:], func=AF.Ln, bias=ones[:, 0:1]), reads=["LF", "ones"], writes=["LF"])
        S.add("dve", lambda e: e.tensor_scalar_mul(LF[:], LF[:], -1.0), reads=["LF"], writes=["LF"])
        prow = [S.ps("prow%d" % i, [128, 512], F32) for i in range(2)]
        rows = {}
        for d in range(2):
            for nm in ("I", "L", "F", "A", "M", "T", "U", "U2", "E"):
                rows[(nm, d)] = S.sb("row%s%d" % (nm, d), [4, NTOK], F32)
        R = [S.sb("R%d" % d, [4, NT + 1], F32) for d in range(2)]
        KP = [S.sb("KP%d" % d, [4, NT], F32) for d in range(2)]
        cnt = 0
        for d in range(2):
            rm = self.ident_f if d == 0 else cj
            for src, nm in ((IG, "I"), (LF, "L")):
                for p0 in range(0, NT, 4):
                    pr = prow[cnt % 2]
                    cnt += 1
                    n = min(4, NT - p0)
                    for pp in range(n):
                        t = tile_at[d](p0 + pp)
                        S.add("pe", lambda e, pr=pr, pp=pp, t=t, src=src, rm=rm, d=d: e.matmul(
                            pr[0:4, pp * 128:(pp + 1) * 128], src[:, t, 4 * d:4 * d + 4], rm[:], start=True, stop=True),
                            reads=["IG", "LF", "cj"], writes=[("prow", id(pr))])
                    S.add("dve", lambda e, pr=pr, p0=p0, n=n, nm=nm, d=d: e.tensor_copy(out=rows[(nm, d)][:, p0 * 128:(p0 + n) * 128], in_=pr[0:4, 0:n * 128]),
                          reads=[("prow", id(pr))], writes=[("row", nm, d)])
            r = lambda nm: rows[(nm, d)]
            k = lambda nm: ("row", nm, d)
            v3 = lambda nm: rows[(nm, d)][:].rearrange("c (t n) -> c t n", n=128)
            S.add("dve", lambda e, d=d: e.tensor_tensor_scan(out=r("F")[:], data0=r("L")[:], data1=zr[:], initial=0.0, op0=ALU.add, op1=ALU.add),
                  reads=[k("L"), "zr"], writes=[k("F")])
            S.add("dve", lambda e, d=d: e.tensor_tensor(out=r("A")[:], in0=r("I")[:], in1=r("F")[:], op=ALU.subtract), reads=[k("I"), k("F")], writes=[k("A")])
            S.add("dve", lambda e, d=d: e.tensor_tensor_scan(out=r("M")[:], data0=r("A")[:], data1=r("A")[:], initial=0.0, op0=ALU.max, op1=ALU.max),
                  reads=[k("A")], writes=[k("M")])
            S.add("dve", lambda e, d=d: e.memset(R[d][:, 0:1], 0.0), writes=[("R", d)])
            S.add("dve", lambda e, d=d: e.tensor_copy(out=R[d][:, 1:NT + 1], in_=v3("M")[:, :, 127]), reads=[k("M")], writes=[("R", d)])
            Rc = R[d][:, 0:NT].unsqueeze(2).to_broadcast([4, NT, 128])
            Rn = R[d][:, 1:NT + 1].unsqueeze(2).to_broadcast([4, NT, 128])
            S.add("dve", lambda e, d=d: e.tensor_tensor(out=v3("T"), in0=v3("A"), in1=Rc, op=ALU.subtract), reads=[k("A"), ("R", d)], writes=[k("T")])
            S.add("act", lambda e, d=d: e.activation(out=r("U")[:], in_=r("T")[:], func=AF.Exp), reads=[k("T")], writes=[k("U")])
            S.add("dve", lambda e, d=d: e.tensor_tensor(out=v3("T"), in0=v3("A"), in1=Rn, op=ALU.subtract), reads=[k("A"), ("R", d), k("U")], writes=[k("T")])
            S.add("act", lambda e, d=d: e.activation(out=r("U2")[:], in_=r("T")[:], func=AF.Exp), reads=[k("T")], writes=[k("U2")])
            S.add("dve", lambda e, d=d: e.tensor_tensor(out=v3("T"), in0=v3("F"), in1=Rc, op=ALU.add), reads=[k("F"), ("R", d), k("U2")], writes=[k("T")])
            S.add("act", lambda e, d=d: e.activation(out=r("E")[:], in_=r("T")[:], func=AF.Exp, scale=-1.0), reads=[k("T")], writes=[k("E")])
            S.add("dve", lambda e, d=d: e.tensor_tensor(out=KP[d][:], in0=R[d][:, 0:NT], in1=R[d][:, 1:NT + 1], op=ALU.subtract), reads=[("R", d)], writes=[("KP", d)])
            S.add("act", lambda e, d=d: e.activation(out=KP[d][:], in_=KP[d][:], func=AF.Exp), reads=[("KP", d)], writes=[("KP", d)])
            pk = prow[cnt % 2]
            cnt += 1
            for c in range(4):
                S.add("pe", lambda e, c=c, pk=pk, d=d: e.matmul(pk[:, c * NT:(c + 1) * NT], sel[:, c * 128:(c + 1) * 128], KP[d][:], start=True, stop=True),
                      reads=["sel", ("KP", d)], writes=[("prow", id(pk))])
            S.add("dve", lambda e, pk=pk, d=d: e.tensor_copy(out=KB[d][:].rearrange("p c t -> p (c t)"), in_=pk[:, 0:4 * NT]),
                  reads=[("prow", id(pk))], writes=[("KB", d)])
            xs = [S.sb("xs%d_%d" % (d, i), [128, 12], F32) for i in range(2)]
            for p in range(NT):
                t = tile_at[d](p)
                pr = prow[cnt % 2]
                cnt += 1
                for qi, nm in enumerate(("U", "U2", "E")):
                    S.add("pe", lambda e, pr=pr, qi=qi, nm=nm, p=p, d=d: e.matmul(pr[:, qi * 4:(qi + 1) * 4], rows[(nm, d)][:, p * 128:(p + 1) * 128],
                                                                               self.ident_f[0:4, 0:4], start=True, stop=True),
                          reads=[("row", nm, d)], writes=[("prow", id(pr))])
                if d == 0:
                    S.add("dve", lambda e, pr=pr, t=t: e.tensor_copy(out=UT[0][:, t, :], in_=pr[:, 0:12]), reads=[("prow", id(pr))], writes=[("UT", 0)])
                else:
                    x = xs[p % 2]
                    S.add("dve", lambda e, pr=pr, x=x: e.tensor_copy(out=x[:], in_=pr[:, 0:12]), reads=[("prow", id(pr))], writes=[("xs", p % 2)])
                    pr2 = prow[cnt % 2]
                    cnt += 1
                    S.add("pe", lambda e, pr2=pr2, x=x: e.matmul(pr2[:, 0:12], cj[:], x[:], start=True, stop=True),
                          reads=[("xs", p % 2), "cj"], writes=[("prow", id(pr2))])
                    S.add("dve", lambda e, pr2=pr2, t=t: e.tensor_copy(out=UT[1][:, t, :], in_=pr2[:, 0:12]), reads=[("prow", id(pr2))], writes=[("UT", 1)])
        S.emit()
        S.close()

        S = Sched(nc)
        mask = [S.sb("mask%d" % d, [128, 128], F32) for d in range(2)]
        S.add("sp", lambda e: e.dma_start(out=mask[0][:], in_=self.ap("consts")[:, 256:384]), writes=["mask"], dma=True)
        S.add("sp", lambda e: e.dma_start(out=mask[1][:], in_=self.ap("consts")[:, 384:512]), writes=["mask"], dma=True)
        qT = [S.sb("mq%d" % i, [128, 2, NTOK], BF16) for i in range(2)]
        kT = [S.sb("mk%d" % i, [128, 2, NTOK], BF16) for i in range(2)]
        ktm = [S.sb("mkt%d" % i, [128, NT, 256], BF16) for i in range(2)]
        va = [S.sb("mva%d" % i, [128, NT, 257], BF16) for i in range(2)]
        for i in range(2):
            S.add("pool", lambda e, i=i: e.memset(va[i][:, :, 256:257], 1.0), writes=[("va", i)])
        psKQ = [S.ps("psKQ%d" % d, [128, 512], F32) for d in range(2)]
        psND = [S.ps("psND%d" % d, [128, 512], F32) for d in range(2)]
        psD = [[S.ps("psD%d_%d" % (d, c), [128, 512], F32) for c in range(2)] for d in range(2)]
        Wm = [[S.sb("Wm%d_%d" % (d, i), [128, 128], BF16) for i in range(2)] for d in range(2)]
        gv = [[S.sb("gv%d_%d" % (d, i), [128, 257], BF16) for i in range(2)] for d in range(2)]
        gv2 = [[S.sb("gw%d_%d" % (d, i), [128, 257], BF16) for i in range(2)] for d in range(2)]
        S32 = [[S.sb("S32_%d_%d" % (d, c), [128, 257], F32) for c in range(2)] for d in range(2)]
        Sbf = [[S.sb("Sbf_%d_%d" % (d, c), [128, 257], BF16) for c in range(2)] for d in range(2)]
        dn = [S.sb("dn%d" % d, [128, 1], F32) for d in range(2)]
        ho = [[S.sb("ho%d_%d" % (d, i), [128, 256], F32) for i in range(2)] for d in range(2)]
        hdst = ["hmf", "hmb"]
        for h in (range(4) if heads is None else heads):
            i = h % 2
            S.add("sp", lambda e: e.dma_start(out=qT[i][:], in_=self.ap("mqkT")[2 * h:2 * h + 2].rearrange("c p n -> p c n")), writes=[("qT", i)], dma=True)
            S.add("sp", lambda e: e.dma_start(out=kT[i][:], in_=self.ap("mqkT")[8 + 2 * h:10 + 2 * h].rearrange("c p n -> p c n")), writes=[("kT", i)], dma=True)
            S.add("sp", lambda e: e.dma_start(out=ktm[i][:], in_=self.ap("mk_tm")[:, h * 256:(h + 1) * 256].rearrange("(t p) c -> p t c", p=128)),
                  writes=[("ktm", i)], dma=True)
            S.add("sp", lambda e: e.dma_start(out=va[i][:, :, 0:256], in_=self.ap("mv")[:, h * 256:(h + 1) * 256].rearrange("(t p) c -> p t c", p=128)),
                  writes=[("va", i)], dma=True)
            for p in range(NT):
                for d in range(2):
                    t = tile_at[d](p)
                    tok = slice(t * 128, (t + 1) * 128)
                    j = p % 2
                    for c in range(2):
                        S.add("pe", lambda e, c=c: e.matmul(psKQ[d][:, 0:128], kT[i][:, c, tok], qT[i][:, c, tok], start=(c == 0), stop=(c == 1)),
                              reads=[("kT", i), ("qT", i)], writes=[("psKQ", d)])
                    S.add("dve", lambda e: e.tensor_tensor(out=Wm[d][j][:], in0=psKQ[d][:, 0:128], in1=mask[d][:], op=ALU.mult),
                          reads=[("psKQ", d), "mask"], writes=[("Wm", d, j)])
                    S.add("act", lambda e: e.activation(out=gv[d][j][:], in_=va[i][:, t, :], func=AF.Copy, scale=UT[d][:, t, h:h + 1]),
                          reads=[("va", i)], writes=[("gv", d, j)])
                    if p < NT - 1:
                        S.add("act", lambda e: e.activation(out=gv2[d][j][:], in_=va[i][:, t, :], func=AF.Copy, scale=UT[d][:, t, 4 + h:5 + h]),
                              reads=[("va", i)], writes=[("gv2", d, j)])
                    first = True
                    if p > 0:
                        for c in range(2):
                            S.add("pe", lambda e, c=c, first=first: e.matmul(psND[d][:, 0:257], qT[i][:, c, tok], Sbf[d][c][:], start=first, stop=False),
                                  reads=[("qT", i), ("Sbf", d, c)], writes=[("psND", d)])
                            first = False
                    S.add("pe", lambda e, first=first: e.matmul(psND[d][:, 0:257], Wm[d][j][:], gv[d][j][:], start=first, stop=True),
                          reads=[("Wm", d, j), ("gv", d, j)], writes=[("psND", d)])
                    S.add("act", lambda e: e.activation(out=dn[d][:], in_=psND[d][:, 256:257], func=AF.Abs), reads=[("psND", d)], writes=[("dn", d)])
                    S.add("dve", lambda e: e.tensor_tensor(out=dn[d][:], in0=dn[d][:], in1=UT[d][:, t, 8 + h:9 + h], op=ALU.max),
                          reads=[("dn", d)], writes=[("dn", d)])
                    S.add("dve", lambda e: e.reciprocal(out=dn[d][:], in_=dn[d][:]), reads=[("dn", d)], writes=[("dn", d)])
                    S.add("act", lambda e: e.activation(out=ho[d][j][:], in_=psND[d][:, 0:256], func=AF.Copy, scale=dn[d][:, 0:1]),
                          reads=[("psND", d), ("dn", d)], writes=[("ho", d, j)])
                    S.add("sp", lambda e: e.dma_start(out=self.ap(hdst[d])[tok, h * 256:(h + 1) * 256], in_=ho[d][j][:]), reads=[("ho", d, j)], dma=True)
                    if p < NT - 1:
                        for c in range(2):
                            S.add("pe", lambda e, c=c: e.matmul(psD[d][c][:, 0:257], ktm[i][:, t, c * 128:(c + 1) * 128], gv2[d][j][:], start=True, stop=True),
                                  reads=[("ktm", i), ("gv2", d, j)], writes=[("psD", d, c)])
                            if p == 0:
                                S.add("dve", lambda e, c=c: e.tensor_copy(out=S32[d][c][:], in_=psD[d][c][:, 0:257]),
                                      reads=[("psD", d, c)], writes=[("S32", d, c)])
                            else:
                                S.add("dve", lambda e, c=c: e.scalar_tensor_tensor(out=S32[d][c][:], in0=S32[d][c][:], scalar=KB[d][:, h, p:p + 1],
                                                                                   in1=psD[d][c][:, 0:257], op0=ALU.mult, op1=ALU.add),
                                      reads=[("psD", d, c), ("S32", d, c)], writes=[("S32", d, c)])
                            S.add("pool", lambda e, c=c: e.tensor_copy(out=Sbf[d][c][:], in_=S32[d][c][:]), reads=[("S32", d, c)], writes=[("Sbf", d, c)])
        S.emit()
        S.close()

        S = Sched(nc)
        gn = S.sb("gn", [128, 1024], F32)
        S.add("sp", lambda e: e.dma_start(out=gn[:], in_=self.bcast_rows("m_norm_g", l * 1024, 1024)), writes=["gn"], dma=True)
        hf = [S.sb("hf%d" % i, [128, 1024], F32) for i in range(2)]
        hb = [S.sb("hb%d" % i, [128, 1024], F32) for i in range(2)]
        ot = [S.sb("ot%d" % i, [128, 1024], BF16) for i in range(2)]
        sq = S.sb("sqm", [128, 1024], F32)
        s4 = S.sb("s4", [128, 4], F32)
        yb = [S.sb("ybm%d" % i, [128, 1024], BF16) for i in range(2)]
        ptm = S.ps("ptm3", [128, 8, 128], BF16)
        ys = [S.sb("ysm%d" % i, [128, 8, 128], BF16) for i in range(2)]
        pipe3 = Pipe(2)

        def make_item3(t):
            i = t % 2
            tok = slice(t * 128, (t + 1) * 128)
            h3 = hf[i][:].rearrange("p (h d) -> p h d", h=4)

            def s0():
                    S.add("sp", lambda e: e.dma_start(out=hf[i][:], in_=self.ap("hmf")[tok, :]), writes=[("hf", i)], dma=True)
                    S.add("sp", lambda e: e.dma_start(out=hb[i][:], in_=self.ap("hmb")[tok, :]), writes=[("hb", i)], dma=True)
                    S.add("sp", lambda e: e.dma_start(out=ot[i][:], in_=self.ap("mo")[tok, :]), writes=[("ot", i)], dma=True)
                    S.add("dve", lambda e: e.tensor_tensor(out=hf[i][:], in0=hf[i][:], in1=hb[i][:], op=ALU.add), reads=[("hf", i), ("hb", i)], writes=[("hf", i)])
                    S.add("pool", lambda e: e.tensor_tensor(out=sq[:], in0=hf[i][:], in1=hf[i][:], op=ALU.mult), reads=[("hf", i)], writes=["sq"])
                    S.add("dve", lambda e: e.tensor_reduce(out=s4[:], in_=sq[:].rearrange("p (h d) -> p h d", h=4), axis=AX.X, op=ALU.add), reads=["sq"], writes=["s4"])
                    self.rstd(S, s4[:], "s4", 256)
                    h3 = hf[i][:].rearrange("p (h d) -> p h d", h=4)
                    S.add("pool", lambda e: e.tensor_tensor(out=h3, in0=h3, in1=s4[:].unsqueeze(2).to_broadcast([128, 4, 256]), op=ALU.mult),
                          reads=[("hf", i), "s4"], writes=[("hf", i)])
                    S.add("dve", lambda e: e.tensor_tensor(out=hf[i][:], in0=hf[i][:], in1=gn[:], op=ALU.mult), reads=[("hf", i), "gn"], writes=[("hf", i)])
                    S.add("pool", lambda e: e.tensor_tensor(out=yb[i][:], in0=hf[i][:], in1=ot[i][:], op=ALU.mult), reads=[("hf", i), ("ot", i)], writes=[("yb", i)])

            def s1():
                    for c in range(8):
                        S.add("pe", lambda e, c=c: e.transpose(out=ptm[:, c, :], in_=yb[i][:, c * 128:(c + 1) * 128], identity=self.ident_b[:]),
                              reads=[("yb", i)], writes=["ptm"])
                    S.add("act", lambda e: e.copy(out=ys[i][:], in_=ptm[:]), reads=["ptm"], writes=[("ys", i)])
                    S.add("sp", lambda e: e.dma_start(out=self.ap("ysT")[0, :, :, tok].rearrange("c p n -> p c n"), in_=ys[i][:]), reads=[("ys", i)], dma=True)

            return [s0, s1]

        for t in range(NT):
            pipe3.push(make_item3(t))
        pipe3.drain()
        S.emit()
        S.close()
        g2.close()

    def phase_merge_a(self, l, tiles):
        nc = self.nc
        S = Sched(nc)
        ysall = S.sb("ysall", [128, 24, NTOK], BF16)
        for b in range(3):
            S.add("sp", lambda e, b=b: e.dma_start(out=ysall[:, b * 8:(b + 1) * 8, :], in_=self.ap("ysT")[b].rearrange("c p n -> p c n")),
                  writes=[("ys", b)], dma=True)
        wbr = [S.sb("wbr%d" % i, [128, 24, 128], BF16) for i in range(2)]
        gj = [S.sb("gj%d" % i, [128, 3, NTOK], BF16) for i in range(2)]
        mTs = [S.sb("mTs%d" % i, [128, NTOK], BF16) for i in range(2)]
        tmpm = [[S.sb("tmpm%d_%d" % (i, r_), [128, 512], F32) for i in range(3)] for r_ in range(2)]
        accm = [S.sb("accm%d" % r_, [128, 512], F32) for r_ in range(2)]
        py = [S.ps("py%d" % i, [128, 512], F32) for i in range(6)]
        tstart = tiles[0] * 128
        tend = (tiles[-1] + 1) * 128
        blocks = [(t0, min(512, tend - t0)) for t0 in range(tstart, tend, 512)]
        cnt = {"py": 0, "r": 0}
        def load_j(j):
            wi = j % 2
            S.add("pool", lambda e: e.dma_start(out=wbr[wi][:], in_=self.ap("w_branch")[l][:, :, j * 128:(j + 1) * 128].rearrange("b (c p) w -> p (b c) w", p=128)),
                  writes=[("wbr", wi)], dma=True)
            S.add("sp", lambda e: e.dma_start(out=gj[wi][:, :, tstart:tend], in_=self.ap("gT")[:, :, tstart:tend].rearrange("(b j) p n -> j p b n", b=3)[j]),
                  writes=[("gj", wi)], dma=True)

        load_j(0)
        for j in range(NKC):
            wi = j % 2
            if j + 1 < NKC:
                load_j(j + 1)
            for (t0, n) in blocks:
                rr_ = cnt["r"] % 2
                cnt["r"] += 1
                for b in range(3):
                    pi = cnt["py"] % 6
                    cnt["py"] += 1
                    for c in range(8):
                        S.add("pe", lambda e, c=c: e.matmul(py[pi][:, 0:n], wbr[wi][:, b * 8 + c, :], ysall[:, b * 8 + c, t0:t0 + n], start=(c == 0), stop=(c == 7)),
                              reads=[("wbr", wi), ("ys", b)], writes=[("py", pi)])
                    S.add("dve", lambda e: e.tensor_tensor(out=tmpm[rr_][b][:, 0:n], in0=py[pi][:, 0:n], in1=gj[wi][:, b, t0:t0 + n], op=ALU.mult),
                          reads=[("py", pi), ("gj", wi)], writes=[("tmpm", rr_, b)])
                S.add("pool", lambda e: e.tensor_tensor(out=accm[rr_][:, 0:n], in0=tmpm[rr_][0][:, 0:n], in1=tmpm[rr_][1][:, 0:n], op=ALU.add),
                      reads=[("tmpm", rr_, 0), ("tmpm", rr_, 1)], writes=[("accm", rr_)])
                S.add("pool", lambda e: e.tensor_tensor(out=mTs[wi][:, t0:t0 + n], in0=accm[rr_][:, 0:n], in1=tmpm[rr_][2][:, 0:n], op=ALU.add),
                      reads=[("accm", rr_), ("tmpm", rr_, 2)], writes=[("mTs", wi)])
            S.add("sp", lambda e: e.dma_start(out=self.ap("mTd")[j][:, tstart:tend], in_=mTs[wi][:, tstart:tend]), reads=[("mTs", wi)], dma=True)
        S.emit()
        S.close()

    def phase_merge_b(self, l, tiles):
        nc = self.nc
        S = Sched(nc)
        wout = S.sb("wout", [128, NKC, D], BF16)
        for h in range(2):
            S.add("pool", lambda e, h=h: e.dma_start(out=wout[:, h * 8:(h + 1) * 8, :],
                                                     in_=self.ap("w_out")[l][h * 1024:(h + 1) * 1024, :].rearrange("(k p) w -> p k w", p=128)),
                  writes=[("wout", h)], dma=True)
        mods = {}
        for r in (0, 1):
            for seg, add1 in ((2, False), (4, True), (3, False)):
                if r == 1 and tiles[0] >= NT_C:
                    continue
                tl = S.sb("modm_%d_%d" % (r, seg), [128, D], F32)
                S.add("sp", lambda e, tl=tl, r=r, seg=seg: e.dma_start(out=tl[:], in_=self.bcast_rows("modrow", modoff(l, r, seg), D)),
                      writes=[("mod", r, seg)], dma=True)
                if add1:
                    S.add("pool", lambda e, tl=tl: e.tensor_scalar_add(tl[:], tl[:], 1.0), reads=[("mod", r, seg)], writes=[("mod", r, seg)])
                mods[(r, seg)] = tl
        mT = [S.sb("mTb%d" % i, [128, NKC, 128], BF16) for i in range(2)]
        pyo = [S.ps("pyo%d" % i, [128, 512], F32) for i in range(4)]
        pth = [S.ps("pth%d" % i, [128, 8, 128], BF16) for i in range(2)]
        xts = [S.sb("xtm%d" % i, [128, D], F32) for i in range(2)]
        junk = S.sb("junkm", [128, D], BF16)
        xm32 = S.sb("xm32m", [128, D], F32)
        xmb = S.sb("xmbm", [128, D], BF16)
        ssb = S.sb("ssbm", [128, NT], F32)
        hst = [S.sb("hstm%d" % i, [128, NKC, 128], BF16) for i in range(2)]
        S.add("dve", lambda e: e.memset(ssb[:], 0.0), writes=[("ss", t) for t in range(NT)])
        cnt = {"po": 0}
        rw = S.sb("rw", [128, NKC, 16], BF16)
        rb = S.sb("rb", [128, 16], F32)
        S.add("pool", lambda e: e.dma_start(out=rw[:], in_=self.ap("router_w").rearrange("(k p) e -> p k e", p=128)), writes=["rw"], dma=True)
        S.add("sp", lambda e: e.dma_start(out=rb[:], in_=self.bcast_rows("router_bias", 0, 16)), writes=["rb"], dma=True)
        prt = S.ps("prt", [128, 512], F32)
        sc = S.sb("r_sc", [128, 16], F32)
        sel = S.sb("r_sel", [128, 16], F32)
        eq = S.sb("r_eq", [128, 16], F32)
        sm = S.sb("r_sm", [128, 16], F32)
        m1 = S.sb("r_m1", [128, 4], F32)
        m2 = S.sb("r_m2", [128, 4], F32)
        gm = S.sb("r_gm", [128, 2], F32)
        cmb = [S.sb("cmb%d" % i_, [128, 16], F32) for i_ in range(2)]
        V4 = lambda a_: a_[:].rearrange("p (g k) -> p g k", g=4)
        dv = lambda fn, rd, wr: S.add("dve", fn, reads=rd, writes=wr)
        xmbs = [xmb, S.sb("xmbm1", [128, D], BF16)]
        pipe = Pipe(2)

        def make_item(ti, t):
            i = ti % 2
            xt = xts[i]
            r = 1 if t < NT_C else 0
            tok = slice(t * 128, (t + 1) * 128)
            pos = []
            for dc in range(4):
                pos.append(cnt["po"] % 4)
                cnt["po"] += 1

            def s0():
                S.add("sp", lambda e: e.dma_start(out=mT[i][:], in_=self.ap("mTd")[:, :, tok].rearrange("j p n -> p j n")), writes=[("mT", i)], dma=True)
                S.add("sp", lambda e: e.dma_start(out=xt[:], in_=self.ap("xres")[tok, :]), writes=[("xt", i)], dma=True)
                for dc in range(4):
                    po = pos[dc]
                    for j in range(NKC):
                        S.add("pe", lambda e, j=j: e.matmul(pyo[po][:], mT[i][:, j, :], wout[:, j, dc * 512:(dc + 1) * 512],
                                                            start=(j == 0), stop=(j == NKC - 1)),
                              reads=[("mT", i), ("wout", j // 8)], writes=[("pyo", po)])
                    S.add("dve", lambda e: e.tensor_tensor(out=xm32[:, dc * 512:(dc + 1) * 512], in0=pyo[po][:], in1=mods[(r, 2)][:, dc * 512:(dc + 1) * 512], op=ALU.mult),
                          reads=[("pyo", po), ("mod", r, 2)], writes=["xm32"])
                S.add("pool", lambda e: e.tensor_tensor(out=xt[:], in0=xt[:], in1=xm32[:], op=ALU.add), reads=[("xt", i), "xm32"], writes=[("xt", i)])
                S.add("sp", lambda e: e.dma_start(out=self.ap("xres")[tok, :], in_=xt[:]), reads=[("xt", i)], dma=True)
                self.rms_modulate(S, xt, ("xt", i), ssb[:, t:t + 1], ("ss", t), junk, mods[(r, 4)], ("mod", r, 4), mods[(r, 3)], ("mod", r, 3),
                                  xm32, xmbs[i], ("xmb", i))

            def s1():
                for j in range(NKC):
                    S.add("pe", lambda e, j=j: e.transpose(out=pth[j // 8][:, j % 8, :], in_=xmbs[i][:, j * 128:(j + 1) * 128], identity=self.ident_b[:]),
                          reads=[("xmb", i)], writes=[("pth", j // 8)])
                S.add("act", lambda e: e.copy(out=hst[i][:, 0:8, :], in_=pth[0][:]), reads=[("pth", 0)], writes=[("hst", i, 0)])
                S.add("dve", lambda e: e.tensor_copy(out=hst[i][:, 8:16, :], in_=pth[1][:]), reads=[("pth", 1)], writes=[("hst", i, 1)])
                S.add("sp", lambda e: e.dma_start(out=self.ap("h2Td")[:, :, tok], in_=hst[i][:]), reads=[("hst", i, 0), ("hst", i, 1)], dma=True)
                for k in range(NKC):
                    S.add("pe", lambda e, k=k: e.matmul(prt[:, 0:16], hst[i][:, k, :], rw[:, k, :], start=(k == 0), stop=(k == NKC - 1)),
                          reads=[("hst", i, 0), ("hst", i, 1), "rw"], writes=["prt"])
                S.add("act", lambda e: e.activation(out=sc[:], in_=prt[:, 0:16], func=AF.Sigmoid), reads=["prt"], writes=["sc"])
                dv(lambda e: e.tensor_tensor(out=sel[:], in0=sc[:], in1=rb[:], op=ALU.add), ["sc", "rb"], ["sel"])
                dv(lambda e: e.tensor_reduce(out=m1[:], in_=V4(sel), axis=AX.X, op=ALU.max), ["sel"], ["m1"])
                dv(lambda e: e.tensor_tensor(out=V4(eq), in0=V4(sel), in1=m1[:].unsqueeze(2).to_broadcast([128, 4, 4]), op=ALU.is_equal), ["sel", "m1"], ["eq"])
                dv(lambda e: e.scalar_tensor_tensor(out=sm[:], in0=eq[:], scalar=-1e30, in1=sel[:], op0=ALU.mult, op1=ALU.add), ["eq", "sel"], ["sm"])
                dv(lambda e: e.tensor_reduce(out=m2[:], in_=V4(sm), axis=AX.X, op=ALU.max), ["sm"], ["m2"])
                dv(lambda e: e.tensor_tensor(out=m1[:], in0=m1[:], in1=m2[:], op=ALU.add), ["m1", "m2"], ["m1"])
                dv(lambda e: e.tensor_reduce(out=gm[:, 0:1], in_=m1[:], axis=AX.X, op=ALU.max), ["m1"], ["gm"])
                dv(lambda e: e.tensor_tensor(out=m2[:], in0=m1[:], in1=gm[:, 0:1].to_broadcast([128, 4]), op=ALU.is_equal), ["m1", "gm"], ["m2"])
                dv(lambda e: e.tensor_scalar(out=m2[:], in0=m2[:], scalar1=-1.0, scalar2=1e30, op0=ALU.add, op1=ALU.mult), ["m2"], ["m2"])
                dv(lambda e: e.tensor_tensor(out=V4(sm), in0=V4(sel), in1=m2[:].unsqueeze(2).to_broadcast([128, 4, 4]), op=ALU.add), ["sel", "m2"], ["sm"])
                dv(lambda e: e.tensor_reduce(out=gm[:, 0:1], in_=sm[:], axis=AX.X, op=ALU.max), ["sm"], ["gm"])
                dv(lambda e: e.tensor_tensor(out=eq[:], in0=sm[:], in1=gm[:, 0:1].to_broadcast([128, 16]), op=ALU.is_equal), ["sm", "gm"], ["eq"])
                dv(lambda e: e.scalar_tensor_tensor(out=sm[:], in0=eq[:], scalar=-1e30, in1=sm[:], op0=ALU.mult, op1=ALU.add), ["eq", "sm"], ["sm"])
                dv(lambda e: e.tensor_reduce(out=gm[:, 1:2], in_=sm[:], axis=AX.X, op=ALU.max), ["sm"], ["gm"])
                dv(lambda e: e.tensor_tensor(out=sel[:], in0=sm[:], in1=gm[:, 1:2].to_broadcast([128, 16]), op=ALU.is_equal), ["sm", "gm"], ["sel"])
                dv(lambda e: e.tensor_tensor(out=eq[:], in0=eq[:], in1=sel[:], op=ALU.add), ["eq", "sel"], ["eq"])
                dv(lambda e: e.tensor_tensor(out=sc[:], in0=sc[:], in1=eq[:], op=ALU.mult), ["sc", "eq"], ["sc"])
                dv(lambda e: e.tensor_reduce(out=gm[:, 0:1], in_=sc[:], axis=AX.X, op=ALU.add), ["sc"], ["gm"])
                dv(lambda e: e.reciprocal(out=gm[:, 0:1], in_=gm[:, 0:1]), ["gm"], ["gm"])
                dv(lambda e: e.tensor_scalar(out=cmb[i][:], in0=sc[:], scalar1=gm[:, 0:1], scalar2=None, op0=ALU.mult), ["sc", "gm"], [("cmb", i)])
                S.add("sp", lambda e: e.dma_start(out=self.ap("combd")[tok, :], in_=cmb[i][:]), reads=[("cmb", i)], dma=True)

            return [s0, s1]

        for ti, t in enumerate(tiles):
            pipe.push(make_item(ti, t))
        pipe.drain()
        S.emit()
        S.close()

    def phase_moe(self, l, tiles, out_name=None):
        nc = self.nc
        GSZ = 6
        ngr = (len(tiles) + GSZ - 1) // GSZ
        base, rem = divmod(len(tiles), ngr)
        groups = []
        pos_ = 0
        for gi_ in range(ngr):
            sz = base + (1 if gi_ < rem else 0)
            groups.append(tiles[pos_:pos_ + sz])
            pos_ += sz
        S = Sched(nc)
        g2b = {}
        for r in (0, 1):
            if r == 1 and tiles[0] >= NT_C:
                continue
            g2b[r] = S.sb("g2b%d" % r, [128, D], F32)
            S.add("sp", lambda e, r=r: e.dma_start(out=g2b[r][:], in_=self.bcast_rows("modrow", modoff(l, r, 5), D)), writes=[("g2b", r)], dma=True)
        h2T = S.sb("h2T", [128, NKC, GSZ * 128], BF16)
        acc = [S.sb("acc%d" % i, [128, D], F32) for i in range(GSZ)]
        comb = S.sb("comb", [128, GSZ, 16], F32)
        w1 = [S.sb("w1_%d" % i, [128, NKC, 512], BF16) for i in range(2)]
        w3 = [S.sb("w3_%d" % i, [128, NKC, 512], BF16) for i in range(2)]
        w2 = [S.sb("w2_%d" % i, [128, 4, D], BF16) for i in range(2)]
        psA = [S.ps("psA%d" % i, [128, 512], F32) for i in range(2)]
        psG = [S.ps("psG%d" % i, [128, 512], F32) for i in range(2)]
        ptr = [S.ps("ptrm%d" % i, [128, 4, 128], BF16) for i in range(2)]
        psO = [S.ps("psOm%d" % i, [128, 512], F32) for i in range(2)]
        sa = [S.sb("sa%d" % i, [128, 512], F32) for i in range(2)]
        hid = [S.sb("hid%d" % i, [128, 512], BF16) for i in range(2)]
        hidT = [S.sb("hidT%d" % i, [128, 4, 128], BF16) for i in range(2)]
        sc = S.sb("r_sc", [128, 16], F32)
        sel = S.sb("r_sel", [128, 16], F32)
        eq = S.sb("r_eq", [128, 16], F32)
        sm = S.sb("r_sm", [128, 16], F32)
        m1 = S.sb("r_m1", [128, 4], F32)
        m2 = S.sb("r_m2", [128, 4], F32)
        gm = S.sb("r_gm", [128, 2], F32)
        xtz = S.sb("xtz", [128, D], F32)
        xts2 = [xtz, xtz]
        cnt = {"w": 0, "a": 0, "h": 0, "o": 0}
        V4 = lambda a: a[:].rearrange("p (g k) -> p g k", g=4)
        dv = lambda fn, rd, wr: S.add("dve", fn, reads=rd, writes=wr)
        for grp in groups:
            ng = len(grp)
            t0 = grp[0] * 128
            S.add("sp", lambda e: e.dma_start(out=h2T[:, :, 0:ng * 128], in_=self.ap("h2Td")[:, :, t0:t0 + ng * 128]), writes=["h2T"], dma=True)
            S.add("sp", lambda e: e.dma_start(out=comb[:, 0:ng, :], in_=self.ap("combd")[t0:t0 + ng * 128, :].rearrange("(g p) e -> p g e", p=128)),
                  writes=[("comb", gi) for gi in range(GSZ)], dma=True)
            for ex in range(N_EXP):
                wi = cnt["w"] % 2
                cnt["w"] += 1
                S.add("pool", lambda e: e.dma_start(out=w1[wi][:], in_=self.ap("moe_w1")[l, ex].rearrange("(k p) f -> p k f", p=128)), writes=[("w1", wi)], dma=True)
                S.add("pool", lambda e: e.dma_start(out=w3[wi][:], in_=self.ap("moe_w3")[l, ex].rearrange("(k p) f -> p k f", p=128)), writes=[("w3", wi)], dma=True)
                S.add("pool", lambda e: e.dma_start(out=w2[wi][:], in_=self.ap("moe_w2")[l, ex].rearrange("(k p) f -> p k f", p=128)), writes=[("w2", wi)], dma=True)
                ais = []
                for gi in range(ng):
                    ais.append(cnt["a"] % 2)
                    cnt["a"] += 1

                def up(gi):
                    tk = slice(gi * 128, (gi + 1) * 128)
                    ai = ais[gi]
                    for k in range(NKC):
                        S.add("pe", lambda e, k=k: e.matmul(psA[ai][:], h2T[:, k, tk], w1[wi][:, k, :], start=(k == 0), stop=(k == NKC - 1)),
                              reads=["h2T", ("w1", wi)], writes=[("psA", ai)])
                    for k in range(NKC):
                        S.add("pe", lambda e, k=k: e.matmul(psG[ai][:], h2T[:, k, tk], w3[wi][:, k, :], start=(k == 0), stop=(k == NKC - 1)),
                              reads=["h2T", ("w3", wi)], writes=[("psG", ai)])
                    S.add("act", lambda e: e.activation(out=sa[ai][:], in_=psA[ai][:], func=AF.Silu), reads=[("psA", ai)], writes=[("sa", ai)])
                    S.add("dve", lambda e: e.scalar_tensor_tensor(out=hid[ai][:], in0=psG[ai][:], scalar=comb[:, gi, ex:ex + 1], in1=sa[ai][:],
                                                                  op0=ALU.mult, op1=ALU.mult),
                          reads=[("psG", ai), ("comb", gi), ("sa", ai)], writes=[("hid", ai)])

                def down(gi):
                    ai = ais[gi]
                    for fc in range(4):
                        S.add("pe", lambda e, fc=fc: e.transpose(out=ptr[ai][:, fc, :], in_=hid[ai][:, fc * 128:(fc + 1) * 128], identity=self.ident_b[:]),
                              reads=[("hid", ai)], writes=[("ptr", ai)])
                    S.add("act", lambda e: e.copy(out=hidT[ai][:], in_=ptr[ai][:]), reads=[("ptr", ai)], writes=[("hidT", ai)])
                    for dc in range(4):
                        oi = cnt["o"] % 2
                        cnt["o"] += 1
                        for fc in range(4):
                            S.add("pe", lambda e, fc=fc: e.matmul(psO[oi][:], hidT[ai][:, fc, :], w2[wi][:, fc, dc * 512:(dc + 1) * 512],
                                                                  start=(fc == 0), stop=(fc == 3)),
                                  reads=[("hidT", ai), ("w2", wi)], writes=[("psO", oi)])
                        if ex == 0:
                            S.add("dve", lambda e: e.tensor_copy(out=acc[gi][:, dc * 512:(dc + 1) * 512], in_=psO[oi][:]),
                                  reads=[("psO", oi)], writes=[("acc", gi, dc)])
                        else:
                            S.add("dve", lambda e: e.tensor_tensor(out=acc[gi][:, dc * 512:(dc + 1) * 512], in0=acc[gi][:, dc * 512:(dc + 1) * 512],
                                                                   in1=psO[oi][:], op=ALU.add),
                                  reads=[("psO", oi), ("acc", gi, dc)], writes=[("acc", gi, dc)])

                up(0)
                for gi in range(ng):
                    if gi + 1 < ng:
                        up(gi + 1)
                    down(gi)
            for gi, t in enumerate(grp):
                r = 1 if t < NT_C else 0
                tok = slice(t * 128, (t + 1) * 128)
                xi = 0
                xt = xts2[xi]
                S.add("sp", lambda e: e.dma_start(out=xt[:], in_=self.ap("xres")[tok, :]), writes=[("xt", xi)], dma=True)
                S.add("dve", lambda e: e.tensor_tensor(out=acc[gi][:], in0=acc[gi][:], in1=g2b[r][:], op=ALU.mult),
                      reads=[("acc", gi, dc) for dc in range(4)] + [("g2b", r)], writes=[("acc", gi, dc) for dc in range(4)])
                S.add("pool", lambda e: e.tensor_tensor(out=xt[:], in0=xt[:], in1=acc[gi][:], op=ALU.add),
                      reads=[("xt", xi)] + [("acc", gi, dc) for dc in range(4)], writes=[("xt", xi)])
                if out_name is not None and t >= NT_C:
                    S.add("sp", lambda e: e.dma_start(out=self.T[out_name].ap()[(t - NT_C) * 128:(t - NT_C + 1) * 128, :], in_=xt[:]), reads=[("xt", xi)], dma=True)
                else:
                    S.add("sp", lambda e: e.dma_start(out=self.ap("xres")[tok, :], in_=xt[:]), reads=[("xt", xi)], dma=True)
        S.emit()
        S.close()


def build(layers=(0, 1), dbg=(), phases=None, groups=None, aheads=None, final_out=False):
    nc = bass.Bass("TRN2", target_bir_lowering=False)
    Sched.GLOBAL = None
    mk = MK(nc, dbg)
    if final_out:
        mk.T["out"] = nc.dram_tensor("out", [NT_L * 128, D], F32, kind="ExternalOutput")
    mk.load_consts()
    ph = lambda p: phases is None or p in phases
    for l in layers:
        if ph("ada"):
            mk.phase_ada(l)
    for l in layers:
        if ph("mod1"):
            mk.phase_mod1(l)
        if ph("inproj"):
            mk.phase_inproj(l, groups)
        if ph("mlstm"):
            mk.phase_mlstm(l, heads=aheads)
        if ph("mla_up"):
            mk.phase_mla_up(l)
        if ph("da"):
            mk.phase_attn(l, 0, l < DEPTH - 1, heads=aheads)
        if ph("mla"):
            mk.phase_attn(l, 1, l < DEPTH - 1, heads=aheads)
        tiles = list(range(NT)) if l < DEPTH - 1 else list(range(NT_C, NT))
        if ph("merge"):
            mk.phase_merge_a(l, tiles)
            mk.phase_merge_b(l, tiles)
        if ph("moe"):
            mk.phase_moe(l, tiles, out_name=("out" if (l == DEPTH - 1 and final_out) else None))
    mk.g.close()
    if Sched.GLOBAL is not None:
        Sched.GLOBAL["stack"].close()
    return nc, mk


def _axial(n, rot):
    rows = n // 64
    r = np.repeat(np.arange(rows, dtype=np.float32), 64)
    col = np.tile(np.arange(64, dtype=np.float32), rows)
    nf = rot // 4
    inv = (10000.0 ** (-np.arange(nf, dtype=np.float32) / nf)).astype(np.float32)
    return np.concatenate([r[:, None] * inv, col[:, None] * inv], -1)


def _rope_tab(rot):
    a = _axial(NT_L * 128, rot)
    t = np.zeros((NTOK, rot), np.float32)
    t[:NT_C * 128, :rot // 2] = 1.0
    t[NT_C * 128:, :rot // 2] = np.cos(a)
    t[NT_C * 128:, rot // 2:] = np.sin(a)
    return t


def _consts():
    c = np.zeros((128, 512), np.float32)
    c[:, 0:128] = np.eye(128)
    c[:, 128:256] = np.eye(128)[::-1]
    c[:, 256:384] = np.triu(np.ones((128, 128)))
    c[:, 384:512] = np.tril(np.ones((128, 128)))
    c2 = np.zeros((4, 512), np.float32)
    for k in range(4):
        c2[k, k * 128:(k + 1) * 128] = 1.0
    return c, c2


N_CORES = 4
_CACHE = {}


def kernel(**inputs):
    if "nc" not in _CACHE:
        _CACHE["nc"] = build(final_out=True)
    nc, mk = _CACHE["nc"]
    names = [n for n in mk.T if n in INPUT_SHAPES]
    c1, c2 = _consts()
    rd = _rope_tab(128)
    rope_da2 = np.ascontiguousarray(np.concatenate([rd[:, :64], rd[:, :64], -rd[:, 64:], rd[:, 64:]], 1))
    shared = {"consts": c1, "consts2": c2, "rope_da2": rope_da2, "rope_mla": _rope_tab(64)}
    B = inputs["x"].shape[0]
    in_maps = []
    for core in range(N_CORES):
        b = core % B
        d = {}
        for n in names:
            if n == "xs":
                d[n] = np.ascontiguousarray(np.concatenate([inputs["ctx"][b], inputs["x"][b]], 0), dtype=np.float32)
            elif n == "cvec":
                d[n] = np.ascontiguousarray(np.stack([inputs["c"][b], inputs["c_ctx"]], 0), dtype=np.float32)
            elif n in shared:
                d[n] = shared[n]
            else:
                d[n] = np.ascontiguousarray(inputs[n], dtype=np.float32)
        in_maps.append(d)
    res = run_bass_kernel_spmd(nc, in_maps, core_ids=list(range(N_CORES)))
    out = np.stack([np.asarray(res.results[b]["out"], dtype=np.float32) for b in range(B)], 0)
    return out
```

```python
import contextlib
import math
import numpy as np
import concourse.bass as bass
import concourse.mybir as mybir
from concourse.bass_utils import run_bass_kernel_spmd

F32 = mybir.dt.float32
BF16 = mybir.dt.bfloat16
AF = mybir.ActivationFunctionType
ALU = mybir.AluOpType
AX = mybir.AxisListType

ENGS = ("pe", "act", "dve", "pool", "sp")
N_DMA_SEMS = 24

D = 2048
NKC = D // 128
DEPTH = 2
NT_C = 2
NT_L = 16
NT = NT_C + NT_L
NTOK = NT * 128
D_IN = 14160
OFF_MQK, OFF_MV, OFF_MO, OFF_MG = 0, 2048, 3072, 4096
OFF_DQ, OFF_DK, OFF_DV = 4112, 5136, 6160
OFF_CQ, OFF_CKV, OFF_KR, OFF_G = 7184, 7696, 7952, 8016
EPS = 1e-6
N_EXP = 16
INPUT_SHAPES = {
    "xs": [NTOK, D], "cvec": [2, D], "consts": [128, 512], "consts2": [4, 512],
    "rope_da2": [NTOK, 256], "rope_mla": [NTOK, 64],
    "w_ada": [DEPTH, D, 6 * D], "b_ada": [DEPTH, 6 * D],
    "w_in": [DEPTH, D, D_IN], "b_in": [DEPTH, D_IN],
    "m_conv_w": [DEPTH, 5, 2048], "m_conv_b": [DEPTH, 2048], "m_norm_g": [DEPTH, 1024],
    "da_q_norm_g": [DEPTH, 128], "da_k_norm_g": [DEPTH, 128], "da_lambda": [DEPTH, 4, 128],
    "da_subln_g": [DEPTH, 256], "mla_cq_norm_g": [DEPTH, 512], "mla_ckv_norm_g": [DEPTH, 256],
    "mla_w_uq": [DEPTH, 512, 1536], "mla_w_ukv": [DEPTH, 256, 2048],
    "mla_q_norm_g": [DEPTH, 192], "mla_k_norm_g": [DEPTH, 192],
    "w_branch": [DEPTH, 3, 1024, 2048], "w_out": [DEPTH, 2048, 2048],
    "moe_w1": [DEPTH, 16, 2048, 512], "moe_w3": [DEPTH, 16, 2048, 512], "moe_w2": [DEPTH, 16, 512, 2048],
    "router_w": [2048, 16], "router_bias": [16],
}


class Pipe:
    def __init__(self, nstages):
        self.n = nstages
        self.items = []

    def push(self, stages):
        self.items.append(stages)
        i = len(self.items) - 1
        for s in range(self.n):
            j = i - s
            if j >= 0 and s < len(self.items[j]):
                self.items[j][s]()

    def drain(self):
        last = len(self.items) - 1
        for extra in range(1, self.n):
            for s in range(extra, self.n):
                j = last + extra - s
                if 0 <= j <= last and s < len(self.items[j]):
                    self.items[j][s]()
        self.items = []


class Op:
    def __init__(self, eng, fn, dma):
        self.eng = eng
        self.fn = fn
        self.deps = []
        self.needed = False
        self.idx = -1
        self.dma = dma
        self.prev_dma = None
        self.cnt = 0


class Rec:
    def __init__(self):
        self.call = None

    def __getattr__(self, name):
        def f(*a, **k):
            self.call = (name, a, k)
            return self
        return f


class Sched:
    PID = 0
    GLOBAL = None

    def __init__(self, nc, same_engine_sync=True):
        self.nc = nc
        self.ops = {e: [] for e in ENGS}
        self.res = {}
        self.stack = contextlib.ExitStack()
        self.same_engine_sync = same_engine_sync
        self.dma_count = {e: 0 for e in ENGS}
        self.dma_last = {}
        self.seen = {e: {e2: -1 for e2 in ENGS} for e in ENGS}
        self.seen_dma = {e: {} for e in ENGS}
        Sched.PID += 1
        self.pid = Sched.PID

    def sb(self, name, shape, dt=F32):
        return self.stack.enter_context(self.nc.sbuf_tensor("p%d_%s" % (self.pid, name), list(shape), dt))

    def ps(self, name, shape, dt=F32):
        return self.stack.enter_context(self.nc.psum_tensor("p%d_%s" % (self.pid, name), list(shape), dt))

    def add(self, eng, fn, reads=(), writes=(), dma=False):
        rec = Rec()
        fn(rec)
        op = Op(eng, rec.call, dma)
        lst = self.ops[eng]
        op.idx = len(lst)
        deps = []
        for k in reads:
            r = self.res.get(k)
            if r is not None and r[0] is not None:
                deps.append(r[0])
        for k in writes:
            r = self.res.get(k)
            if r is not None:
                if r[0] is not None:
                    deps.append(r[0])
                deps.extend(r[1])
        seen = self.seen[eng]
        sd = self.seen_dma[eng]
        out = []
        for d in deps:
            if d is op:
                continue
            if d.eng == eng and not d.dma:
                if eng == "pe" or (not self.same_engine_sync and not dma):
                    continue
            if d.dma:
                key = (d.eng, d.dma_slot)
                if sd.get(key, -1) >= d.dma_seq:
                    continue
                sd[key] = d.dma_seq
                out.append(d)
            else:
                if seen[d.eng] >= d.idx:
                    continue
                seen[d.eng] = d.idx
                d.needed = True
                out.append(d)
        op.deps = out
        if dma:
            c = self.dma_count[eng]
            self.dma_count[eng] = c + 1
            op.dma_slot = c % N_DMA_SEMS
            op.dma_seq = c // N_DMA_SEMS
            prev = self.dma_last.get((eng, op.dma_slot))
            op.prev_dma = prev
            self.dma_last[(eng, op.dma_slot)] = op
            if prev is not None:
                sd[(eng, op.dma_slot)] = max(sd.get((eng, op.dma_slot), -1), prev.dma_seq)
        lst.append(op)
        for k in reads:
            r = self.res.setdefault(k, [None, []])
            r[1].append(op)
        for k in writes:
            self.res[k] = [op, []]
        return op

    def emit(self):
        nc = self.nc
        G = Sched.GLOBAL
        if G is None:
            G = Sched.GLOBAL = {"stack": contextlib.ExitStack(), "sems": {}, "dsems": {}, "base": {e: 0 for e in ENGS}, "dbase": {}}
        gs = G["stack"]
        for e in ENGS:
            if e not in G["sems"]:
                G["sems"][e] = gs.enter_context(nc.semaphore("gs_" + e))
        for e in ENGS:
            if self.dma_count[e] > 0:
                for j in range(N_DMA_SEMS):
                    if (e, j) not in G["dsems"]:
                        G["dsems"][(e, j)] = gs.enter_context(nc.semaphore("gd_%s_%d" % (e, j)))
                        G["dbase"][(e, j)] = 0
        sems, dsems, base, dbase = G["sems"], G["dsems"], G["base"], G["dbase"]
        final_cnt = {}
        for e in ENGS:
            c = base[e]
            last = None
            for op in self.ops[e]:
                if not op.dma:
                    last = op
            if last is not None:
                last.needed = True
            for op in self.ops[e]:
                if op.dma:
                    continue
                if op.needed:
                    c += 1
                op.cnt = c
            final_cnt[e] = c
        dval = lambda d: 16 * (dbase[(d.eng, d.dma_slot)] + d.dma_seq + 1)
        block = self.stack.enter_context(nc.Block())
        engmap = {"pe": block.tensor, "act": block.scalar, "dve": block.vector,
                  "pool": block.gpsimd, "sp": block.sync}

        def make(e):
            def body(eng):
                for op in self.ops[e]:
                    for d in op.deps:
                        if d.dma:
                            eng.wait_ge(dsems[(d.eng, d.dma_slot)], dval(d))
                        else:
                            eng.wait_ge(sems[d.eng], d.cnt)
                    if op.dma:
                        if op.prev_dma is not None:
                            eng.wait_ge(dsems[(e, op.prev_dma.dma_slot)], dval(op.prev_dma))
                        ins = getattr(eng, op.fn[0])(*op.fn[1], **op.fn[2])
                        ins.then_inc(dsems[(e, op.dma_slot)], 16)
                    else:
                        ins = getattr(eng, op.fn[0])(*op.fn[1], **op.fn[2])
                        if op.needed:
                            ins.then_inc(sems[e], 1)
                for e2 in ENGS:
                    if final_cnt[e2] > base[e2]:
                        eng.wait_ge(sems[e2], final_cnt[e2])
                for (e2, j), last in self.dma_last.items():
                    eng.wait_ge(dsems[(e2, j)], dval(last))
            return body

        for e in ENGS:
            engmap[e](make(e))
        for e in ENGS:
            base[e] = final_cnt[e]
        for (e2, j), last in self.dma_last.items():
            dbase[(e2, j)] += last.dma_seq + 1

    def close(self):
        self.stack.close()


def modoff(l, r, seg):
    return ((l * 2 + r) * 6 + seg) * D


class MK:
    def __init__(self, nc, dbg=()):
        self.nc = nc
        self.dbg = set(dbg)
        self.g = contextlib.ExitStack()
        self.T = {}
        Sx = self.scr
        Sx("modrow", [DEPTH, 2, 6 * D], F32)
        Sx("xres", [NTOK, D], F32)
        Sx("hTd", [128, NKC, NTOK], BF16)
        Sx("mqkT", [16, 128, NTOK], BF16)
        Sx("mk_tm", [NTOK, 1024], BF16)
        Sx("mv", [NTOK, 1024], BF16)
        Sx("mo", [NTOK, 1024], BF16)
        Sx("mg", [NTOK, 16], F32)
        Sx("dqT", [8, 128, NTOK], BF16)
        Sx("dkT", [8, 128, NTOK], BF16)
        Sx("dv", [NTOK, 1024], BF16)
        Sx("cqT", [4, 128, NTOK], BF16)
        Sx("ckvT", [2, 128, NTOK], BF16)
        Sx("akrT", [64, NTOK], BF16)
        Sx("gT", [48, 128, NTOK], BF16)
        Sx("aqTn", [8, 128, NTOK], BF16)
        Sx("aqTr", [8, 64, NTOK], BF16)
        Sx("akT", [8, 128, NTOK], BF16)
        Sx("av", [NTOK, 1024], BF16)
        Sx("ysT", [3, 8, 128, NTOK], BF16)
        Sx("hmf", [NTOK, 1024], F32)
        Sx("h2Td", [128, NKC, NTOK], BF16)
        Sx("mTd", [NKC, 128, NTOK], BF16)
        Sx("combd", [NTOK, 16], F32)
        Sx("hmb", [NTOK, 1024], F32)

    def tens(self, name):
        if name not in self.T:
            self.T[name] = self.nc.dram_tensor(name, list(INPUT_SHAPES[name]), F32, kind="ExternalInput")
        return self.T[name]

    def scr(self, name, shape, dt):
        kind = "ExternalOutput" if name in self.dbg else "Internal"
        self.T[name] = self.nc.dram_tensor(name, list(shape), dt, kind=kind)

    def ap(self, name):
        return self.tens(name).ap()

    def bcast_rows(self, name, offset, width, nparts=128):
        return bass.AP(self.tens(name), offset, [[0, nparts], [1, width]])

    def load_consts(self):
        nc = self.nc
        self.ident_f = self.g.enter_context(nc.sbuf_tensor("ident_f", [128, 128], F32))
        self.ident_b = self.g.enter_context(nc.sbuf_tensor("ident_b", [128, 128], BF16))
        self.eps_col = self.g.enter_context(nc.sbuf_tensor("eps_col", [128, 1], F32))
        S = Sched(nc)
        S.add("pool", lambda e: e.memset(self.eps_col[:], EPS), writes=["epsc"])
        S.add("sp", lambda e: e.dma_start(out=self.ident_f[:], in_=self.ap("consts")[:, 0:128]),
              writes=["idf"], dma=True)
        S.add("dve", lambda e: e.tensor_copy(out=self.ident_b[:], in_=self.ident_f[:]), reads=["idf"], writes=["idb"])
        for t in range(0, NT, 6):
            S.add("sp", lambda e, t=t: e.dma_start(out=self.ap("xres")[t * 128:(t + 6) * 128, :],
                                                   in_=self.ap("xs")[t * 128:(t + 6) * 128, :]), dma=True)
        S.emit()
        S.close()

    def phase_ada(self, l):
        nc = self.nc
        S = Sched(nc)
        cv = S.sb("cv", [2, D], F32)
        sv = S.sb("sv", [2, D], F32)
        sT = S.sb("sT", [128, NKC, 2], BF16)
        pT = S.ps("pT", [128, NKC, 2], F32)
        wb = [S.sb("wada%d" % i, [128, NKC, 512], BF16) for i in range(2)]
        bb = [S.sb("bada%d" % i, [2, 512], F32) for i in range(2)]
        ob = [S.sb("oada%d" % i, [2, 512], F32) for i in range(2)]
        pm = [S.ps("pm%d" % i, [128, 512], F32) for i in range(2)]
        S.add("sp", lambda e: e.dma_start(out=cv[:], in_=self.ap("cvec")), writes=["cv"], dma=True)
        S.add("act", lambda e: e.activation(out=sv[:], in_=cv[:], func=AF.Silu), reads=["cv"], writes=["sv"])
        for k in range(NKC):
            S.add("pe", lambda e, k=k: e.transpose(out=pT[:, k, :], in_=sv[:, k * 128:(k + 1) * 128],
                                                   identity=self.ident_f[0:2, 0:2]),
                  reads=["sv"], writes=["pT"])
        S.add("dve", lambda e: e.tensor_copy(out=sT[:], in_=pT[:]), reads=["pT"], writes=["sT"])
        wl = self.ap("w_ada")[l]
        nblk = 6 * D // 512
        for j in range(nblk):
            i = j % 2
            S.add("pool", lambda e, j=j, i=i: e.dma_start(
                out=wb[i][:], in_=wl[:, j * 512:(j + 1) * 512].rearrange("(k p) w -> p k w", p=128)),
                writes=[("wb", i)], dma=True)
            S.add("sp", lambda e, j=j, i=i: e.dma_start(
                out=bb[i][:], in_=self.bcast_rows("b_ada", l * 6 * D + j * 512, 512, 2)),
                writes=[("bb", i)], dma=True)
            for k in range(NKC):
                S.add("pe", lambda e, k=k, i=i: e.matmul(pm[i][0:2, :], sT[:, k, :], wb[i][:, k, :],
                                                         start=(k == 0), stop=(k == NKC - 1)),
                      reads=["sT", ("wb", i)], writes=[("pm", i)])
            S.add("dve", lambda e, i=i: e.tensor_tensor(out=ob[i][:], in0=pm[i][0:2, :], in1=bb[i][:], op=ALU.add),
                  reads=[("pm", i), ("bb", i)], writes=[("ob", i)])
            S.add("sp", lambda e, j=j, i=i: e.dma_start(out=self.ap("modrow")[l][:, j * 512:(j + 1) * 512], in_=ob[i][:]),
                  reads=[("ob", i)], dma=True)
        S.emit()
        S.close()

    def load_mod_tiles(self, S, l, segs, tag):
        out = {}
        for r in (0, 1):
            for seg, add1 in segs:
                tl = S.sb("mod%s_%d_%d" % (tag, r, seg), [128, D], F32)
                key = ("mod", r, seg)
                S.add("sp", lambda e, tl=tl, r=r, seg=seg: e.dma_start(
                    out=tl[:], in_=self.bcast_rows("modrow", modoff(l, r, seg), D)), writes=[key], dma=True)
                if add1:
                    S.add("pool", lambda e, tl=tl: e.tensor_scalar_add(tl[:], tl[:], 1.0), reads=[key], writes=[key])
                out[(r, seg)] = tl
        return out

    def rstd(self, S, ap, key, n):
        S.add("act", lambda e: e.activation(out=ap, in_=ap, func=AF.Sqrt, scale=1.0 / n, bias=self.eps_col[:, 0:1]),
              reads=[key], writes=[key])
        S.add("dve", lambda e: e.reciprocal(out=ap, in_=ap), reads=[key], writes=[key])

    def rms_modulate(self, S, xt, xkey, ss, sskey, junk, sc, sckey, sh, shkey, xm32, xmb, xmkey):
        S.add("act", lambda e: e.activation(out=junk[:], in_=xt[:], func=AF.Square, accum_out=ss),
              reads=[xkey, sskey], writes=["junk", sskey])
        self.rstd(S, ss, sskey, D)
        S.add("dve", lambda e: e.scalar_tensor_tensor(out=xm32[:], in0=xt[:], scalar=ss, in1=sc[:],
                                                      op0=ALU.mult, op1=ALU.mult),
              reads=[xkey, sskey, sckey], writes=["xm32"])
        S.add("pool", lambda e: e.tensor_tensor(out=xmb[:], in0=xm32[:], in1=sh[:], op=ALU.add),
              reads=["xm32", shkey], writes=[xmkey])

    def phase_mod1(self, l):
        nc = self.nc
        S = Sched(nc)
        mods = self.load_mod_tiles(S, l, [(1, True), (0, False)], "a")
        xts = [S.sb("xt%d" % i, [128, D], F32) for i in range(2)]
        junk = S.sb("junk", [128, D], BF16)
        xm32 = S.sb("xm32", [128, D], F32)
        xmbs = [S.sb("xmb%d" % i, [128, D], BF16) for i in range(2)]
        ssb = S.sb("ssb", [128, NT], F32)
        hst = [S.sb("hst%d" % i, [128, NKC, 128], BF16) for i in range(2)]
        pts = [S.ps("pt%d" % i, [128, 8, 128], BF16) for i in range(4)]
        S.add("dve", lambda e: e.memset(ssb[:], 0.0), writes=[("ss", t) for t in range(NT)])
        pipe = Pipe(2)

        def make_item(t):
            r = 1 if t < NT_C else 0
            i = t % 2
            xt = xts[i]

            def s0():
                S.add("sp", lambda e: e.dma_start(out=xt[:], in_=self.ap("xres")[t * 128:(t + 1) * 128, :]),
                      writes=[("xt", i)], dma=True)
                self.rms_modulate(S, xt, ("xt", i), ssb[:, t:t + 1], ("ss", t), junk,
                                  mods[(r, 1)], ("mod", r, 1), mods[(r, 0)], ("mod", r, 0), xm32, xmbs[i], ("xmb", i))

            def s1():
                for j in range(NKC):
                    pt = pts[2 * i + j // 8]
                    S.add("pe", lambda e, pt=pt, j=j: e.transpose(out=pt[:, j % 8, :], in_=xmbs[i][:, j * 128:(j + 1) * 128],
                                                                  identity=self.ident_b[:]),
                          reads=[("xmb", i)], writes=[("pt", 2 * i + j // 8)])
                S.add("act", lambda e: e.copy(out=hst[i][:, 0:8, :], in_=pts[2 * i][:]),
                      reads=[("pt", 2 * i)], writes=[("hst", i, 0)])
                S.add("dve", lambda e: e.tensor_copy(out=hst[i][:, 8:16, :], in_=pts[2 * i + 1][:]),
                      reads=[("pt", 2 * i + 1)], writes=[("hst", i, 1)])
                S.add("sp", lambda e: e.dma_start(out=self.ap("hTd")[:, :, t * 128:(t + 1) * 128], in_=hst[i][:]),
                      reads=[("hst", i, 0), ("hst", i, 1)], dma=True)

            return [s0, s1]

        for t in range(NT):
            pipe.push(make_item(t))
        pipe.drain()
        S.emit()
        S.close()

    def rows_to_cols(self, S, name, src_ap_rows, nrows, nchunks, pst, pstkey, eng="dve"):
        rt = S.sb(name + "_r", [nrows, nchunks * 128], F32)
        ct = S.sb(name + "_c", [128, nchunks, nrows], F32)
        S.add("sp", lambda e: e.dma_start(out=rt[:], in_=src_ap_rows), writes=[name + "_r"], dma=True)
        for c in range(nchunks):
            S.add("pe", lambda e, c=c: e.transpose(out=pst[:, c * nrows:(c + 1) * nrows], in_=rt[:, c * 128:(c + 1) * 128],
                                                   identity=self.ident_f[0:nrows, 0:nrows]),
                  reads=[name + "_r"], writes=[pstkey])
        S.add(eng, lambda e: e.tensor_copy(out=ct[:].rearrange("p c r -> p (c r)"), in_=pst[:, 0:nchunks * nrows]),
              reads=[pstkey], writes=[name + "_c"])
        return ct

    def phase_inproj(self, l, groups=None):
        nc = self.nc
        S = Sched(nc)
        hT = S.sb("hT", [128, NKC, NTOK], BF16)
        wbs = [S.sb("wblk%d" % i, [128, NKC, 512], BF16) for i in range(2)]
        bts = [S.sb("bt%d" % i, [128, 512], F32) for i in range(2)]
        pa = [S.ps("pa%d" % i, [128, 512], F32) for i in range(5)]
        pt = [S.ps("ptb%d" % i, [128, 8, 128], BF16) for i in range(2)]
        pmisc = S.ps("pmisc", [128, 512], F32)
        st32 = [S.sb("st32_%d" % i, [128, 512], F32) for i in range(2)]
        stb = [S.sb("stb%d" % i, [128, 512], BF16) for i in range(3)]
        NROT = 2
        tmpR = [[S.sb("tmp%d_%d" % (i, r_), [128, 512], F32) for i in range(3)] for r_ in range(NROT)]
        ss4R = [S.sb("ss4_%d" % r_, [128, 8], F32) for r_ in range(NROT)]
        rot = {"i": 0}
        tmp = tmpR[0]
        ss4 = ss4R[0]
        trs = [S.sb("trs%d" % i, [128, 4, 128], BF16) for i in range(4)]
        w_l = self.ap("w_in")[l]
        st = {"wcnt": 0, "pa": 0, "stb": 0, "st32": 0, "pt": 0, "trs": 0}
        for h in range(0, NT, 6):
            S.add("sp", lambda e, h=h: e.dma_start(out=hT[:, :, h * 128:(h + 6) * 128],
                                                   in_=self.ap("hTd")[:, :, h * 128:(h + 6) * 128]),
                  writes=[("hT", h // 6)], dma=True)
        hkeys = [("hT", i) for i in range(3)]

        plan = []
        _do = lambda g: groups is None or g in groups
        for gname, off in (("mv", OFF_MV), ("dv", OFF_DV), ("mo", OFF_MO)):
            if _do(gname):
                plan += [(off, 512, True), (off + 512, 512, True)]
        if _do("mg"):
            plan.append((OFF_MG, 16, True))
        for gname, off in (("dq", OFF_DQ), ("dk", OFF_DK)):
            if _do(gname):
                plan += [(off, 512, True), (off + 512, 512, True)]
        if _do("cq"):
            plan.append((OFF_CQ, 512, True))
        if _do("kr"):
            plan.append((OFF_CKV, 320, True))
        if _do("g"):
            plan += [(OFF_G + c0, 512, False) for c0 in range(0, 6144, 512)]
        if _do("mqk"):
            plan += [(OFF_MQK + c0, 512, False) for c0 in range(0, 2048, 512)]
        issued = {"n": 0}

        def issue_w(k):
            c0, W, bias = plan[k]
            i = k % 2
            S.add("pool", lambda e: e.dma_start(out=wbs[i][:, :, 0:W],
                                                in_=w_l[:, c0:c0 + W].rearrange("(k p) w -> p k w", p=128)),
                  writes=[("wb", i)], dma=True)
            if bias:
                S.add("sp", lambda e: e.dma_start(out=bts[i][:, 0:W], in_=self.bcast_rows("b_in", l * D_IN + c0, W)),
                      writes=[("bt", i)], dma=True)

        def load_w(c0, W, bias=True):
            k = st["wcnt"]
            st["wcnt"] += 1
            assert plan[k] == (c0, W, bias), (plan[k], c0, W, bias)
            while issued["n"] <= min(k + 1, len(plan) - 1):
                issue_w(issued["n"])
                issued["n"] += 1
            return k % 2

        def nxt(k, n):
            i = st[k] % n
            st[k] += 1
            return i

        def mm_tm(t, wi, W):
            p = nxt("pa", 5)
            for k in range(NKC):
                S.add("pe", lambda e, k=k: e.matmul(pa[p][:, 0:W], hT[:, k, t * 128:(t + 1) * 128], wbs[wi][:, k, 0:W],
                                                    start=(k == 0), stop=(k == NKC - 1)),
                      reads=[("hT", t // 6), ("wb", wi)], writes=[("pa", p)])
            return p

        def store(eng, dst_ap, src_ap, rkeys):
            S.add(eng, lambda e: e.dma_start(out=dst_ap, in_=src_ap), reads=rkeys, dma=True)

        do = lambda g: groups is None or g in groups

        for gname, off, width, dst in (("mv", OFF_MV, 1024, "mv"), ("dv", OFF_DV, 1024, "dv"), ("mo", OFF_MO, 1024, "mo")):
            if not do(gname):
                continue
            for c0 in range(0, width, 512):
                wi = load_w(off + c0, 512)
                for t in range(NT):
                    p = mm_tm(t, wi, 512)
                    b = nxt("stb", 3)
                    if gname == "mo":
                        a = nxt("st32", 2)
                        S.add("dve", lambda e, p=p, a=a: e.tensor_tensor(out=st32[a][:], in0=pa[p][:], in1=bts[wi][:], op=ALU.add),
                              reads=[("pa", p), ("bt", wi)], writes=[("st32", a)])
                        S.add("act", lambda e, a=a, b=b: e.activation(out=stb[b][:], in_=st32[a][:], func=AF.Sigmoid),
                              reads=[("st32", a)], writes=[("stb", b)])
                    else:
                        S.add("dve", lambda e, p=p, b=b: e.tensor_tensor(out=stb[b][:], in0=pa[p][:], in1=bts[wi][:], op=ALU.add),
                              reads=[("pa", p), ("bt", wi)], writes=[("stb", b)])
                    store("sp", self.ap(dst)[t * 128:(t + 1) * 128, c0:c0 + 512], stb[b][:], [("stb", b)])
        if do("mg"):
            wi = load_w(OFF_MG, 16)
            for t in range(NT):
                p = mm_tm(t, wi, 16)
                a = nxt("st32", 2)
                S.add("dve", lambda e, p=p, a=a: e.tensor_tensor(out=st32[a][:, 0:16], in0=pa[p][:, 0:16], in1=bts[wi][:, 0:16], op=ALU.add),
                      reads=[("pa", p), ("bt", wi)], writes=[("st32", a)])
                store("sp", self.ap("mg")[t * 128:(t + 1) * 128, :], st32[a][:, 0:16], [("st32", a)])

        def transposes_out(src_b, skey, ngrp, gw, dst_ap_fn):
            q = nxt("pt", 2)
            for g_ in range(ngrp):
                S.add("pe", lambda e, g_=g_: e.transpose(out=pt[q][0:gw, g_, :], in_=src_b[:, g_ * gw:(g_ + 1) * gw],
                                                         identity=self.ident_b[:]),
                      reads=[skey], writes=[("pt", q)])
            r = nxt("trs", 4)
            S.add("act", lambda e: e.copy(out=trs[r][0:gw, 0:ngrp, :], in_=pt[q][0:gw, 0:ngrp, :]),
                  reads=[("pt", q)], writes=[("trs", r)])
            store("sp", dst_ap_fn(), trs[r][0:gw, 0:ngrp, :], [("trs", r)])

        if do("kr"):
            rml = S.sb("rml", [128, NT, 64], F32)
            S.add("sp", lambda e: e.dma_start(out=rml[:], in_=self.ap("rope_mla").rearrange("(t p) c -> p t c", p=128)),
                  writes=["rml"], dma=True)

        def rope(src, skey, ngrp, half, cos_ap, sin_ap, dstb, dkey):
            x1 = src[:, :, 0:half]
            x2 = src[:, :, half:2 * half]
            cb = cos_ap.unsqueeze(1).to_broadcast([128, ngrp, half])
            sb_ = sin_ap.unsqueeze(1).to_broadcast([128, ngrp, half])
            ta = tmp[0][:, 0:ngrp * half].rearrange("p (g h) -> p g h", g=ngrp)
            tb = tmp[1][:, 0:ngrp * half].rearrange("p (g h) -> p g h", g=ngrp)
            S.add("pool", lambda e: e.tensor_tensor(out=ta, in0=x1, in1=cb, op=ALU.mult), reads=[skey, "rml"], writes=[("tmp0", rot["i"])])
            S.add("pool", lambda e: e.tensor_tensor(out=tb, in0=x2, in1=sb_, op=ALU.mult), reads=[skey, "rml"], writes=[("tmp1", rot["i"])])
            S.add("dve", lambda e: e.tensor_tensor(out=dstb[:, :, 0:half], in0=ta, in1=tb, op=ALU.subtract),
                  reads=[("tmp0", rot["i"]), ("tmp1", rot["i"])], writes=[dkey])
            S.add("pool", lambda e: e.tensor_tensor(out=ta, in0=x1, in1=sb_, op=ALU.mult), reads=[skey, "rml"], writes=[("tmp0", rot["i"])])
            S.add("dve", lambda e: e.tensor_tensor(out=tb, in0=x2, in1=cb, op=ALU.mult), reads=[skey, "rml"], writes=[("tmp1", rot["i"])])
            S.add("dve", lambda e: e.tensor_tensor(out=dstb[:, :, half:2 * half], in0=ta, in1=tb, op=ALU.add),
                  reads=[("tmp0", rot["i"]), ("tmp1", rot["i"])], writes=[dkey])

        if do("dq") or do("dk"):
            rdt = [S.sb("rdt%d" % i, [128, 256], F32) for i in range(2)]
            ones_b = S.sb("ones_b", [1, 128], BF16)
            S.add("pool", lambda e: e.memset(ones_b[:], 1.0), writes=["ones_b"])
            brow = [S.sb("brow%d" % i, [1, 512], BF16) for i in range(2)]
            junkq = S.sb("junkq", [128, 4, 128], BF16)
            stq = S.sb("stq18", [128, NT, 512], BF16)
            ssq4 = [S.sb("ssq4_%d" % i, [128, 4], F32) for i in range(4)]
            xg4 = [S.sb("xg4_%d" % i, [128, 512], F32) for i in range(2)]
            tq1 = [S.sb("tq1_%d" % i, [128, 512], F32) for i in range(2)]
            tq2 = [S.sb("tq2_%d" % i, [128, 512], F32) for i in range(1)]
            qc = {"n": 0}
        for gname, off, gain, scale, dst in (("dq", OFF_DQ, "da_q_norm_g", 128 ** -0.5, "dqT"), ("dk", OFF_DK, "da_k_norm_g", 1.0, "dkT")):
            if not do(gname):
                continue
            gb = S.sb("gb_" + gname, [128, 128], F32)
            S.add("sp", lambda e, gb=gb, gain=gain: e.dma_start(out=gb[:], in_=self.bcast_rows(gain, l * 128, 128)),
                  writes=["gb_" + gname], dma=True)
            S.add("pool", lambda e, gb=gb, scale=scale: e.tensor_scalar_mul(gb[:], gb[:], float(scale)),
                  reads=["gb_" + gname], writes=["gb_" + gname])
            for c0 in range(0, 1024, 512):
                wi = load_w(off + c0, 512)
                S.add("pool", lambda e: e.dma_start(out=brow[wi][:], in_=bass.AP(self.tens("b_in"), l * D_IN + off + c0, [[0, 1], [1, 512]])),
                      writes=[("brow", wi)], dma=True)
                for t in range(NT):
                    n_ = qc["n"]
                    qc["n"] += 1
                    r4, r3, r2 = n_ % 4, n_ % 2, 0
                    r1 = n_ % 2
                    ss = ssq4[r4]
                    xg = xg4[r3]
                    rd = rdt[r3]
                    p = nxt("pa", 5)
                    S.add("sp", lambda e: e.dma_start(out=rd[:], in_=self.ap("rope_da2")[t * 128:(t + 1) * 128, :]), writes=[("rdt", r3)], dma=True)
                    for k in range(NKC):
                        S.add("pe", lambda e, k=k: e.matmul(pa[p][:], hT[:, k, t * 128:(t + 1) * 128], wbs[wi][:, k, :],
                                                            start=(k == 0), stop=False),
                              reads=[("hT", t // 6), ("wb", wi)], writes=[("pa", p)])
                    S.add("pe", lambda e: e.matmul(pa[p][:], ones_b[:], brow[wi][:], start=False, stop=True),
                          reads=["ones_b", ("brow", wi)], writes=[("pa", p)])
                    S.add("dve", lambda e: e.memset(ss[:], 0.0), reads=[("ssq", r4)], writes=[("ssq", r4)])
                    for g_ in range(4):
                        S.add("act", lambda e, g_=g_: e.activation(out=junkq[:, g_, :], in_=pa[p][:, g_ * 128:(g_ + 1) * 128], func=AF.Square,
                                                                   accum_out=ss[:, g_:g_ + 1]),
                              reads=[("pa", p), ("ssq", r4)], writes=[("junkq", g_), ("ssqc", r4, g_)])
                    S.add("act", lambda e: e.activation(out=ss[:], in_=ss[:], func=AF.Sqrt, scale=1.0 / 128, bias=self.eps_col[:, 0:1]),
                          reads=[("ssqc", r4, g_) for g_ in range(4)] + [("ssq", r4)], writes=[("ssq", r4)] + [("ssqc", r4, g_) for g_ in range(4)])
                    x3 = xg[:].rearrange("p (g h) -> p g h", g=4)
                    S.add("dve", lambda e, x3=x3, gb=gb: e.tensor_tensor(out=x3, in0=pa[p][:].rearrange("p (g h) -> p g h", g=4),
                                                                         in1=gb[:].unsqueeze(1).to_broadcast([128, 4, 128]), op=ALU.mult),
                          reads=[("pa", p), "gb_" + gname] + [("ssqc", r4, g_) for g_ in range(4)], writes=[("xg", r3)])
                    S.add("dve", lambda e: e.reciprocal(out=ss[:], in_=ss[:]), reads=[("ssq", r4)], writes=[("ssq", r4)])
                    t1 = tq1[r1][:].rearrange("p (g h) -> p g h", g=4)
                    t2 = tq2[r2][:].rearrange("p (g h) -> p g h", g=4)
                    S.add("dve", lambda e: e.tensor_tensor(out=t1, in0=x3, in1=rd[:, 0:128].unsqueeze(1).to_broadcast([128, 4, 128]), op=ALU.mult),
                          reads=[("xg", r3), ("rdt", r3)], writes=[("tq1", r1)])
                    S.add("pool", lambda e: e.tensor_tensor(out=t2[:, :, 0:64], in0=x3[:, :, 64:128],
                                                            in1=rd[:, 128:192].unsqueeze(1).to_broadcast([128, 4, 64]), op=ALU.mult),
                          reads=[("xg", r3), ("rdt", r3)], writes=[("tq2", r2)])
                    S.add("pool", lambda e: e.tensor_tensor(out=t2[:, :, 64:128], in0=x3[:, :, 0:64],
                                                            in1=rd[:, 192:256].unsqueeze(1).to_broadcast([128, 4, 64]), op=ALU.mult),
                          reads=[("xg", r3), ("rdt", r3)], writes=[("tq2", r2)])
                    S.add("pool", lambda e: e.tensor_tensor(out=t1, in0=t1, in1=t2, op=ALU.add),
                          reads=[("tq1", r1), ("tq2", r2)], writes=[("tq1", r1)])
                    S.add("pool", lambda e: e.tensor_tensor(out=stq[:, t, :].rearrange("p (g h) -> p g h", g=4), in0=t1,
                                                            in1=ss[:].unsqueeze(2).to_broadcast([128, 4, 128]), op=ALU.mult),
                          reads=[("tq1", r1), ("ssq", r4)], writes=[("stq", t)])
                g0 = c0 // 128
                for t in range(NT):
                    transposes_out(stq[:, t, :], ("stq", t), 4, 128,
                                   lambda g0=g0, t=t, dst=dst: self.ap(dst)[g0:g0 + 4, :, t * 128:(t + 1) * 128].rearrange("g p n -> p g n"))

        def bias_mm_tile(t, wi, W, p):
            for k in range(NKC):
                S.add("pe", lambda e, k=k: e.matmul(pa[p][:, 0:W], hT[:, k, t * 128:(t + 1) * 128], wbs[wi][:, k, 0:W], start=(k == 0), stop=False),
                      reads=[("hT", t // 6), ("wb", wi)], writes=[("pa", p)])
            S.add("pe", lambda e: e.matmul(pa[p][:, 0:W], ones_b[:], brow[wi][:, 0:W], start=False, stop=True),
                  reads=["ones_b", ("brow", wi)], writes=[("pa", p)])

        if do("cq"):
            gcq = S.sb("gcq", [128, 512], F32)
            S.add("sp", lambda e: e.dma_start(out=gcq[:], in_=self.bcast_rows("mla_cq_norm_g", l * 512, 512)), writes=["gcq"], dma=True)
            wi = load_w(OFF_CQ, 512)
            S.add("pool", lambda e: e.dma_start(out=brow[wi][:], in_=bass.AP(self.tens("b_in"), l * D_IN + OFF_CQ, [[0, 1], [1, 512]])),
                  writes=[("brow", wi)], dma=True)
            for t in range(NT):
                n_ = qc["n"]
                qc["n"] += 1
                r4 = n_ % 4
                ss = ssq4[r4]
                p = nxt("pa", 5)
                bias_mm_tile(t, wi, 512, p)
                S.add("dve", lambda e: e.memset(ss[:], 0.0), reads=[("ssq", r4)], writes=[("ssq", r4)])
                S.add("act", lambda e: e.activation(out=junkq[:].rearrange("p g h -> p (g h)"), in_=pa[p][:], func=AF.Square, accum_out=ss[:, 0:1]),
                      reads=[("pa", p), ("ssq", r4)], writes=[("junkq", g_) for g_ in range(4)] + [("ssq", r4)])
                S.add("act", lambda e: e.activation(out=ss[:, 0:1], in_=ss[:, 0:1], func=AF.Sqrt, scale=1.0 / 512, bias=self.eps_col[:, 0:1]),
                      reads=[("ssq", r4)], writes=[("ssq", r4)])
                S.add("dve", lambda e: e.reciprocal(out=ss[:, 0:1], in_=ss[:, 0:1]), reads=[("ssq", r4)], writes=[("ssq", r4)])
                S.add("dve", lambda e: e.scalar_tensor_tensor(out=stq[:, t, :], in0=pa[p][:], scalar=ss[:, 0:1], in1=gcq[:], op0=ALU.mult, op1=ALU.mult),
                      reads=[("pa", p), ("ssq", r4), "gcq"], writes=[("stq", t)])
            for t in range(NT):
                transposes_out(stq[:, t, :], ("stq", t), 4, 128,
                               lambda t=t: self.ap("cqT")[:, :, t * 128:(t + 1) * 128].rearrange("g p n -> p g n"))

        if do("kr"):
            gkv = S.sb("gkv", [128, 320], F32)
            S.add("sp", lambda e: e.dma_start(out=gkv[:, 0:256], in_=self.bcast_rows("mla_ckv_norm_g", l * 256, 256)), writes=["gkv"], dma=True)
            S.add("sp", lambda e: e.dma_start(out=gkv[:, 256:320], in_=self.bcast_rows("mla_k_norm_g", l * 192 + 128, 64)), writes=["gkv"], dma=True)
            wi = load_w(OFF_CKV, 320)
            S.add("pool", lambda e: e.dma_start(out=brow[wi][:, 0:320], in_=bass.AP(self.tens("b_in"), l * D_IN + OFF_CKV, [[0, 1], [1, 320]])),
                  writes=[("brow", wi)], dma=True)
            for t in range(NT):
                n_ = qc["n"]
                qc["n"] += 1
                r4 = n_ % 4
                r2_ = n_ % 2
                ss = ssq4[r4]
                kx = xg4[r2_]
                p = nxt("pa", 5)
                bias_mm_tile(t, wi, 320, p)
                S.add("dve", lambda e: e.memset(ss[:], 0.0), reads=[("ssq", r4)], writes=[("ssq", r4)])
                S.add("act", lambda e: e.activation(out=junkq[:, 0:2, :].rearrange("p g h -> p (g h)"), in_=pa[p][:, 0:256], func=AF.Square, accum_out=ss[:, 0:1]),
                      reads=[("pa", p), ("ssq", r4)], writes=[("junkq", 0), ("junkq", 1), ("ssqa", r4)])
                S.add("act", lambda e: e.activation(out=junkq[:, 2, 0:64], in_=pa[p][:, 256:320], func=AF.Square, accum_out=ss[:, 1:2]),
                      reads=[("pa", p), ("ssq", r4)], writes=[("junkq", 2), ("ssqb", r4)])
                S.add("act", lambda e: e.activation(out=ss[:, 0:1], in_=ss[:, 0:1], func=AF.Sqrt, scale=1.0 / 256, bias=self.eps_col[:, 0:1]),
                      reads=[("ssqa", r4), ("ssqb", r4), ("ssq", r4)], writes=[("ssq", r4), ("ssqa", r4)])
                S.add("act", lambda e: e.activation(out=ss[:, 1:2], in_=ss[:, 1:2], func=AF.Sqrt, scale=1.0 / 64, bias=self.eps_col[:, 0:1]),
                      reads=[("ssq", r4), ("ssqb", r4)], writes=[("ssq", r4), ("ssqb", r4)])
                S.add("dve", lambda e: e.reciprocal(out=ss[:, 0:2], in_=ss[:, 0:2]), reads=[("ssq", r4)], writes=[("ssq", r4)])
                S.add("dve", lambda e: e.scalar_tensor_tensor(out=stq[:, t, 0:256], in0=pa[p][:, 0:256], scalar=ss[:, 0:1], in1=gkv[:, 0:256],
                                                              op0=ALU.mult, op1=ALU.mult),
                      reads=[("pa", p), ("ssq", r4), "gkv"], writes=[("stq", t)])
                S.add("dve", lambda e: e.scalar_tensor_tensor(out=kx[:, 0:64], in0=pa[p][:, 256:320], scalar=ss[:, 1:2], in1=gkv[:, 256:320],
                                                              op0=ALU.mult, op1=ALU.mult),
                      reads=[("pa", p), ("ssq", r4), "gkv"], writes=[("xg", r2_)])
                x1, x2 = kx[:, 0:32], kx[:, 32:64]
                cs, sn = rml[:, t, 0:32], rml[:, t, 32:64]
                ta, tb_ = tq1[0][:, 0:32], tq2[0][:, 0:32]
                pk = lambda fn, rd_, wr_: S.add("pool", fn, reads=rd_, writes=wr_)
                pk(lambda e: e.tensor_tensor(out=ta, in0=x1, in1=cs, op=ALU.mult), [("xg", r2_), "rml"], [("tq1", 0)])
                pk(lambda e: e.tensor_tensor(out=tb_, in0=x2, in1=sn, op=ALU.mult), [("xg", r2_), "rml"], [("tq2", 0)])
                pk(lambda e: e.tensor_tensor(out=stq[:, t, 256:288], in0=ta, in1=tb_, op=ALU.subtract), [("tq1", 0), ("tq2", 0)], [("stq", t)])
                pk(lambda e: e.tensor_tensor(out=ta, in0=x1, in1=sn, op=ALU.mult), [("xg", r2_), "rml"], [("tq1", 0)])
                pk(lambda e: e.tensor_tensor(out=tb_, in0=x2, in1=cs, op=ALU.mult), [("xg", r2_), "rml"], [("tq2", 0)])
                pk(lambda e: e.tensor_tensor(out=stq[:, t, 288:320], in0=ta, in1=tb_, op=ALU.add), [("tq1", 0), ("tq2", 0)], [("stq", t)])
            for t in range(NT):
                transposes_out(stq[:, t, :], ("stq", t), 2, 128,
                               lambda t=t: self.ap("ckvT")[:, :, t * 128:(t + 1) * 128].rearrange("g p n -> p g n"))
                q = nxt("pt", 2)
                S.add("pe", lambda e, q=q: e.transpose(out=pt[q][0:64, 0, :], in_=stq[:, t, 256:320], identity=self.ident_b[:]),
                      reads=[("stq", t)], writes=[("pt", q)])
                r = nxt("trs", 4)
                S.add("act", lambda e, r=r, q=q: e.copy(out=trs[r][0:64, 0, :], in_=pt[q][0:64, 0, :]), reads=[("pt", q)], writes=[("trs", r)])
                store("sp", self.ap("akrT")[:, t * 128:(t + 1) * 128], trs[r][0:64, 0, :], [("trs", r)])

        tb = [(0, 512), (512, 512), (1024, 512), (1536, 512), (2048, 256)]

        def mm_fm(wi, j, t0, n):
            p = nxt("pa", 5)
            for k in range(NKC):
                S.add("pe", lambda e, k=k: e.matmul(pa[p][:, 0:n], wbs[wi][:, k, j * 128:(j + 1) * 128], hT[:, k, t0:t0 + n],
                                                    start=(k == 0), stop=(k == NKC - 1)),
                      reads=hkeys + [("wb", wi)], writes=[("pa", p)])
            return p

        if do("g"):
            bcg = self.rows_to_cols(S, "bcg", bass.AP(self.tens("b_in"), l * D_IN + OFF_G, [[128, 48], [1, 128]]), 48, 1, pmisc, "pmisc")
            for c0 in range(0, 6144, 512):
                wi = load_w(OFF_G + c0, 512, bias=False)
                for j in range(4):
                    ch = c0 // 128 + j
                    for (t0, n) in tb:
                        p = mm_fm(wi, j, t0, n)
                        b = nxt("stb", 3)
                        S.add("act", lambda e, p=p, b=b, n=n, ch=ch: e.activation(out=stb[b][:, 0:n], in_=pa[p][:, 0:n], func=AF.Sigmoid,
                                                                                  bias=bcg[:, 0, ch:ch + 1]),
                              reads=[("pa", p), "bcg_c"], writes=[("stb", b)])
                        store("sp", self.ap("gT")[ch, :, t0:t0 + n], stb[b][:, 0:n], [("stb", b)])

        if do("mqk"):
            bcq = self.rows_to_cols(S, "bcq", bass.AP(self.tens("b_in"), l * D_IN + OFF_MQK, [[128, 16], [1, 128]]), 16, 1, pmisc, "pmisc")
            cvb = self.rows_to_cols(S, "cvb", bass.AP(self.tens("m_conv_b"), l * 2048, [[128, 16], [1, 128]]), 16, 1, pmisc, "pmisc")
            cvw = self.rows_to_cols(S, "cvw", self.ap("m_conv_w")[l], 5, 16, pmisc, "pmisc")
            pre = [S.sb("pre%d" % i, [128, 2304 + 8], BF16) for i in range(2)]
            sl = [S.sb("sil%d" % i, [128, 2304], BF16) for i in range(2)]
            mks = [S.sb("mks%d" % i, [128, 6, 128], BF16) for i in range(2)]
            dg = [S.sb("dg%d" % i, [128, 5, 128], BF16) for i in range(2)]
            for i in range(2):
                S.add("pool", lambda e, i=i: e.memset(pre[i][:], 0.0), writes=[("pre", i)])
            oblocks = [(0, 256, 2)] + [(256 + 512 * j, 512, 262 + 512 * j) for j in range(4)]
            for c0 in range(0, 2048, 512):
                wi = load_w(OFF_MQK + c0, 512, bias=False)
                for j in range(4):
                    ch = c0 // 128 + j
                    i = ch % 2
                    for jj in range(5):
                        S.add("dve", lambda e, jj=jj: e.tensor_scalar(out=dg[i][:, jj, :], in0=self.ident_b[:], scalar1=cvw[:, ch, jj:jj + 1],
                                                                       scalar2=None, op0=ALU.mult),
                              reads=["cvw_c"], writes=[("dg", i)])
                    for (t0, n) in tb:
                        p = mm_fm(wi, j, t0, n)
                        po = (2 + t0) if t0 < 256 else (262 + t0 - 256)
                        if t0 == 0:
                            S.add("act", lambda e, p=p: e.activation(out=pre[i][:, 2:258], in_=pa[p][:, 0:256], func=AF.Identity,
                                                                     bias=bcq[:, 0, ch:ch + 1]),
                                  reads=[("pa", p), "bcq_c"], writes=[("pre", i)])
                            S.add("act", lambda e, p=p: e.activation(out=pre[i][:, 262:518], in_=pa[p][:, 256:512], func=AF.Identity,
                                                                     bias=bcq[:, 0, ch:ch + 1]),
                                  reads=[("pa", p), "bcq_c"], writes=[("pre", i)])
                        elif (t0 // 512) % 2 == 1:
                            S.add("act", lambda e, p=p, po=po, n=n: e.activation(out=pre[i][:, po:po + n], in_=pa[p][:, 0:n],
                                                                                 func=AF.Identity, bias=bcq[:, 0, ch:ch + 1]),
                                  reads=[("pa", p), "bcq_c"], writes=[("pre", i)])
                        else:
                            S.add("dve", lambda e, p=p, po=po, n=n: e.tensor_scalar(out=pre[i][:, po:po + n], in0=pa[p][:, 0:n],
                                                                                    scalar1=bcq[:, 0, ch:ch + 1], scalar2=None, op0=ALU.add),
                                  reads=[("pa", p), "bcq_c"], writes=[("pre", i)])
                    for (ts, n, po) in oblocks:
                        p = nxt("pa", 5)
                        for jj in range(5):
                            S.add("pe", lambda e, jj=jj: e.matmul(pa[p][:, 0:n], dg[i][:, jj, :], pre[i][:, po - 2 + jj:po - 2 + jj + n],
                                                                  start=(jj == 0), stop=(jj == 4)),
                                  reads=[("dg", i), ("pre", i)], writes=[("pa", p)])
                        S.add("act", lambda e, p=p, ts=ts, n=n: e.activation(out=sl[i][:, ts:ts + n], in_=pa[p][:, 0:n], func=AF.Silu,
                                                                             bias=cvb[:, 0, ch:ch + 1]),
                              reads=[("pa", p), "cvb_c"], writes=[("sl", i)])
                    if ch < 8:
                        S.add("pool", lambda e, i=i: e.tensor_scalar_mul(sl[i][:], sl[i][:], 1.0 / 16.0), reads=[("sl", i)], writes=[("sl", i)])
                    store("sp", self.ap("mqkT")[ch], sl[i][:], [("sl", i)])
                    if ch >= 8:
                        for t3 in range(0, NT, 6):
                            q = nxt("pt", 2)
                            for tt in range(6):
                                t = t3 + tt
                                S.add("pe", lambda e, q=q, tt=tt, t=t, i=i: e.transpose(out=pt[q][:, tt, :], in_=sl[i][:, t * 128:(t + 1) * 128],
                                                                                        identity=self.ident_b[:]),
                                      reads=[("sl", i)], writes=[("pt", q)])
                            m = nxt("trs", 2)
                            S.add("dve", lambda e, q=q, m=m: e.tensor_copy(out=mks[m][:], in_=pt[q][:, 0:6, :]), reads=[("pt", q)], writes=[("mks", m)])
                            store("sp", self.ap("mk_tm")[t3 * 128:(t3 + 6) * 128, (ch - 8) * 128:(ch - 7) * 128].rearrange("(t p) c -> p t c", p=128),
                                  mks[m][:], [("mks", m)])
        S.emit()
        S.close()


    def phase_mla_up(self, l):
        nc = self.nc
        S = Sched(nc)
        cqT = S.sb("cqTs", [128, 4, NTOK], BF16)
        ckvT = S.sb("ckvTs", [128, 2, NTOK], BF16)
        wuq = S.sb("wuq", [128, 4, 1536], BF16)
        wukv = S.sb("wukv", [128, 2, 2048], BF16)
        gq = S.sb("gq", [128, 192], F32)
        gk = S.sb("gk", [128, 128], F32)
        rml = S.sb("rml2", [128, NT, 64], F32)
        pq = [S.ps("pq%d" % i, [128, 512], F32) for i in range(4)]
        pt = [S.ps("ptm%d" % i, [128, 8, 128], BF16) for i in range(3)]
        st = [S.sb("stq%d" % i, [128, 2048], F32) for i in range(2)]
        sq = S.sb("sqq", [128, 2048], F32)
        qb = [S.sb("qb%d" % i, [128, 2048], BF16) for i in range(2)]
        ssq = S.sb("ssq", [128, 16], F32)
        tmpa = S.sb("tmpa", [128, 256], F32)
        tmpb = S.sb("tmpb", [128, 256], F32)
        trn = [S.sb("trn%d" % i, [128, 8, 128], BF16) for i in range(2)]
        trr = [S.sb("trr%d" % i, [64, 8, 128], BF16) for i in range(2)]
        S.add("sp", lambda e: e.dma_start(out=cqT[:], in_=self.ap("cqT").rearrange("g p n -> p g n")), writes=["cqT"], dma=True)
        S.add("sp", lambda e: e.dma_start(out=ckvT[:], in_=self.ap("ckvT").rearrange("g p n -> p g n")), writes=["ckvT"], dma=True)
        S.add("pool", lambda e: e.dma_start(out=wuq[:], in_=self.ap("mla_w_uq")[l].rearrange("(k p) w -> p k w", p=128)), writes=["wuq"], dma=True)
        S.add("pool", lambda e: e.dma_start(out=wukv[:], in_=self.ap("mla_w_ukv")[l].rearrange("(k p) w -> p k w", p=128)), writes=["wukv"], dma=True)
        S.add("sp", lambda e: e.dma_start(out=gq[:], in_=self.bcast_rows("mla_q_norm_g", l * 192, 192)), writes=["gq"], dma=True)
        S.add("sp", lambda e: e.dma_start(out=gk[:], in_=self.bcast_rows("mla_k_norm_g", l * 192, 128)), writes=["gk"], dma=True)
        S.add("pool", lambda e: e.tensor_scalar_mul(gq[:], gq[:], 192.0 ** -0.5), reads=["gq"], writes=["gq"])
        S.add("sp", lambda e: e.dma_start(out=rml[:], in_=self.ap("rope_mla").rearrange("(t p) c -> p t c", p=128)), writes=["rml"], dma=True)
        pipeu = Pipe(2)

        def make_q(t):
            i = 0
            tok = slice(t * 128, (t + 1) * 128)
            s3 = st[i][:, 0:1536].rearrange("p (h d) -> p h d", h=8)
            q3 = sq[:, 0:1536].rearrange("p (h d) -> p h d", h=8)
            b3 = qb[i][:, 0:1536].rearrange("p (h d) -> p h d", h=8)

            def s0():
                    for cb in range(3):
                        for k in range(4):
                            S.add("pe", lambda e, cb=cb, k=k: e.matmul(pq[cb][:], cqT[:, k, tok], wuq[:, k, cb * 512:(cb + 1) * 512],
                                                                       start=(k == 0), stop=(k == 3)),
                                  reads=["cqT", "wuq"], writes=[("pq", cb)])
                        eng = ("act", "dve", "act")[cb]
                        if eng == "act":
                            S.add("act", lambda e, cb=cb: e.copy(out=st[i][:, cb * 512:(cb + 1) * 512], in_=pq[cb][:]),
                                  reads=[("pq", cb)], writes=[("st", i)])
                        else:
                            S.add("dve", lambda e, cb=cb: e.tensor_copy(out=st[i][:, cb * 512:(cb + 1) * 512], in_=pq[cb][:]),
                                  reads=[("pq", cb)], writes=[("st", i)])
                    s3 = st[i][:, 0:1536].rearrange("p (h d) -> p h d", h=8)
                    q3 = sq[:, 0:1536].rearrange("p (h d) -> p h d", h=8)
                    b3 = qb[i][:, 0:1536].rearrange("p (h d) -> p h d", h=8)
                    S.add("pool", lambda e: e.tensor_tensor(out=sq[:, 0:1536], in0=st[i][:, 0:1536], in1=st[i][:, 0:1536], op=ALU.mult),
                          reads=[("st", i)], writes=["sq"])
                    S.add("dve", lambda e: e.tensor_reduce(out=ssq[:, 0:8], in_=q3[:, :, 0:128], axis=AX.X, op=ALU.add), reads=["sq"], writes=["ssq"])
                    S.add("dve", lambda e: e.tensor_reduce(out=ssq[:, 8:16], in_=q3[:, :, 128:192], axis=AX.X, op=ALU.add), reads=["sq"], writes=["ssq"])
                    self.rstd(S, ssq[:, 0:8], "ssq", 128)
                    self.rstd(S, ssq[:, 8:16], "ssq", 64)
                    S.add("pool", lambda e: e.tensor_tensor(out=s3[:, :, 0:128], in0=s3[:, :, 0:128],
                                                            in1=ssq[:, 0:8].unsqueeze(2).to_broadcast([128, 8, 128]), op=ALU.mult),
                          reads=[("st", i), "ssq"], writes=[("st", i)])
                    S.add("pool", lambda e: e.tensor_tensor(out=s3[:, :, 128:192], in0=s3[:, :, 128:192],
                                                            in1=ssq[:, 8:16].unsqueeze(2).to_broadcast([128, 8, 64]), op=ALU.mult),
                          reads=[("st", i), "ssq"], writes=[("st", i)])
                    S.add("dve", lambda e: e.tensor_tensor(out=s3, in0=s3, in1=gq[:].unsqueeze(1).to_broadcast([128, 8, 192]), op=ALU.mult),
                          reads=[("st", i), "gq"], writes=[("st", i)])
                    S.add("act", lambda e: e.copy(out=b3[:, :, 0:128], in_=s3[:, :, 0:128]), reads=[("st", i)], writes=[("qb", i)])
                    x1 = s3[:, :, 128:160]
                    x2 = s3[:, :, 160:192]
                    cb_ = rml[:, t, 0:32].unsqueeze(1).to_broadcast([128, 8, 32])
                    sb_ = rml[:, t, 32:64].unsqueeze(1).to_broadcast([128, 8, 32])
                    ta = tmpa[:].rearrange("p (g h) -> p g h", g=8)
                    tb_ = tmpb[:].rearrange("p (g h) -> p g h", g=8)
                    S.add("pool", lambda e: e.tensor_tensor(out=ta, in0=x1, in1=cb_, op=ALU.mult), reads=[("st", i), "rml"], writes=["tmpa"])
                    S.add("pool", lambda e: e.tensor_tensor(out=tb_, in0=x2, in1=sb_, op=ALU.mult), reads=[("st", i), "rml"], writes=["tmpb"])
                    S.add("dve", lambda e: e.tensor_tensor(out=b3[:, :, 128:160], in0=ta, in1=tb_, op=ALU.subtract), reads=["tmpa", "tmpb"], writes=[("qb", i)])
                    S.add("pool", lambda e: e.tensor_tensor(out=ta, in0=x1, in1=sb_, op=ALU.mult), reads=[("st", i), "rml"], writes=["tmpa"])
                    S.add("pool", lambda e: e.tensor_tensor(out=tb_, in0=x2, in1=cb_, op=ALU.mult), reads=[("st", i), "rml"], writes=["tmpb"])
                    S.add("dve", lambda e: e.tensor_tensor(out=b3[:, :, 160:192], in0=ta, in1=tb_, op=ALU.add), reads=["tmpa", "tmpb"], writes=[("qb", i)])

            def s1():
                    for h in range(8):
                        S.add("pe", lambda e, h=h: e.transpose(out=pt[0][:, h, :], in_=qb[i][:, h * 192:h * 192 + 128], identity=self.ident_b[:]),
                              reads=[("qb", i)], writes=[("pt", 0)])
                    for h in range(8):
                        S.add("pe", lambda e, h=h: e.transpose(out=pt[1][0:64, h, :], in_=qb[i][:, h * 192 + 128:(h + 1) * 192], identity=self.ident_b[:]),
                              reads=[("qb", i)], writes=[("pt", 1)])
                    S.add("act", lambda e: e.copy(out=trn[i][:], in_=pt[0][:]), reads=[("pt", 0)], writes=[("trn", i)])
                    S.add("dve", lambda e: e.tensor_copy(out=trr[i][:], in_=pt[1][0:64, :, :]), reads=[("pt", 1)], writes=[("trr", i)])
                    S.add("sp", lambda e: e.dma_start(out=self.ap("aqTn")[:, :, tok].rearrange("g p n -> p g n"), in_=trn[i][:]), reads=[("trn", i)], dma=True)
                    S.add("sp", lambda e: e.dma_start(out=self.ap("aqTr")[:, :, tok].rearrange("g p n -> p g n"), in_=trr[i][:]), reads=[("trr", i)], dma=True)

            return [s0, s1]

        def make_kv(t):
            i = 1
            tok = slice(t * 128, (t + 1) * 128)
            k3 = st[i][:].rearrange("p (h d) -> p h d", h=8)
            kq3 = sq[:].rearrange("p (h d) -> p h d", h=8)
            kb3 = qb[i][:].rearrange("p (h d) -> p h d", h=8)

            def s0():
                    for cb in range(4):
                        for k in range(2):
                            S.add("pe", lambda e, cb=cb, k=k: e.matmul(pq[cb][:], ckvT[:, k, tok], wukv[:, k, cb * 512:(cb + 1) * 512],
                                                                       start=(k == 0), stop=(k == 1)),
                                  reads=["ckvT", "wukv"], writes=[("pq", cb)])
                        if cb % 2 == 0:
                            S.add("act", lambda e, cb=cb: e.copy(out=st[i][:, cb * 512:(cb + 1) * 512], in_=pq[cb][:]),
                                  reads=[("pq", cb)], writes=[("st", i)])
                        else:
                            S.add("dve", lambda e, cb=cb: e.tensor_copy(out=st[i][:, cb * 512:(cb + 1) * 512], in_=pq[cb][:]),
                                  reads=[("pq", cb)], writes=[("st", i)])
                    k3 = st[i][:].rearrange("p (h d) -> p h d", h=8)
                    kq3 = sq[:].rearrange("p (h d) -> p h d", h=8)
                    kb3 = qb[i][:].rearrange("p (h d) -> p h d", h=8)
                    S.add("pool", lambda e: e.tensor_tensor(out=kq3[:, :, 0:128], in0=k3[:, :, 0:128], in1=k3[:, :, 0:128], op=ALU.mult),
                          reads=[("st", i)], writes=["sq"])
                    S.add("dve", lambda e: e.tensor_reduce(out=ssq[:, 0:8], in_=kq3[:, :, 0:128], axis=AX.X, op=ALU.add), reads=["sq"], writes=["ssq"])
                    self.rstd(S, ssq[:, 0:8], "ssq", 128)
                    S.add("pool", lambda e: e.tensor_tensor(out=k3[:, :, 0:128], in0=k3[:, :, 0:128],
                                                            in1=ssq[:, 0:8].unsqueeze(2).to_broadcast([128, 8, 128]), op=ALU.mult),
                          reads=[("st", i), "ssq"], writes=[("st", i)])
                    S.add("dve", lambda e: e.tensor_tensor(out=kb3[:, :, 0:128], in0=k3[:, :, 0:128],
                                                           in1=gk[:].unsqueeze(1).to_broadcast([128, 8, 128]), op=ALU.mult),
                          reads=[("st", i), "gk"], writes=[("qb", i)])
                    S.add("act", lambda e: e.copy(out=kb3[:, :, 128:256], in_=k3[:, :, 128:256]), reads=[("st", i)], writes=[("qb", i)])

            def s1():
                    for h in range(8):
                        S.add("pe", lambda e, h=h: e.transpose(out=pt[2][:, h, :], in_=qb[i][:, h * 256:h * 256 + 128], identity=self.ident_b[:]),
                              reads=[("qb", i)], writes=[("pt", 2)])
                    S.add("act", lambda e: e.copy(out=trn[i][:], in_=pt[2][:]), reads=[("pt", 2)], writes=[("trn", i)])
                    S.add("sp", lambda e: e.dma_start(out=self.ap("akT")[:, :, tok].rearrange("g p n -> p g n"), in_=trn[i][:]), reads=[("trn", i)], dma=True)
                    S.add("sp", lambda e: e.dma_start(out=self.ap("av")[tok, :].rearrange("p (h d) -> p h d", h=8), in_=kb3[:, :, 128:256]),
                          reads=[("qb", i)], dma=True)

            return [s0, s1]

        for t in range(NT):
            pipeu.push(make_q(t))
            pipeu.push(make_kv(t))
        pipeu.drain()
        S.emit()
        S.close()

    def phase_attn(self, l, kind, with_ctx, heads=None):
        nc = self.nc
        S = Sched(nc)
        lam_init = 0.8 - 0.6 * math.exp(-0.3 * l)
        if kind == 0:
            nh, dv, nset = 4, 256, 2
        else:
            nh, dv, nset = 8, 128, 1
        qT = [[S.sb("aq%d_%d" % (i, s), [128, NTOK], BF16) for s in range(nset)] for i in range(2)]
        kT = [[S.sb("ak%d_%d" % (i, s), [128, NTOK], BF16) for s in range(nset)] for i in range(2)]
        if kind == 1:
            qTr = [S.sb("aqr%d" % i, [64, NTOK], BF16) for i in range(2)]
            kTr = S.sb("akr", [64, NTOK], BF16)
            S.add("sp", lambda e: e.dma_start(out=kTr[:], in_=self.ap("akrT")), writes=["kTr"], dma=True)
        va = [S.sb("va%d" % i, [128, NT, dv + 1], BF16) for i in range(2)]
        psS = [S.ps("psS%d" % i, [128, 512], F32) for i in range(3)]
        psO = [S.ps("psO%d" % i, [128, 512], F32) for i in range(4)]
        ptr = S.ps("ptr", [128, 8, 128], BF16)
        Pb = [S.sb("Pb%d" % i, [128, 512], BF16) for i in range(3)]
        rr = S.sb("rr", [128, 4], F32)
        t1 = S.sb("t1", [128, 256], F32)
        ob = S.sb("ob", [128, 256], F32)
        obb = [S.sb("obb%d" % i, [128, 256], BF16) for i in range(2)]
        ssn = S.sb("ssn", [128, 1], F32)
        junk = S.sb("junka", [128, 256], BF16)
        yst = [S.sb("yst%d" % i, [128, 2, 128], BF16) for i in range(2)]
        for i in range(2):
            S.add("pool", lambda e, i=i: e.memset(va[i][:, :, dv:dv + 1], 1.0), writes=[("va", i)])
        S.add("pool", lambda e: e.memset(ssn[:], 0.0), writes=["ssn"])
        if kind == 0:
            lamb = S.sb("lamb", [128, 512], F32)
            lamt = S.sb("lamt", [128, 256], F32)
            lamc = S.sb("lamc", [128, 2], F32)
            gsub = S.sb("gsub", [128, 256], F32)
            S.add("sp", lambda e: e.dma_start(out=lamb[:], in_=self.bcast_rows("da_lambda", l * 512, 512)), writes=["lamb"], dma=True)
            S.add("sp", lambda e: e.dma_start(out=gsub[:], in_=self.bcast_rows("da_subln_g", l * 256, 256)), writes=["gsub"], dma=True)
            S.add("pool", lambda e: e.tensor_scalar_mul(gsub[:], gsub[:], float(1.0 - lam_init)), reads=["gsub"], writes=["gsub"])
            l4 = lamb[:].rearrange("p (a b d) -> p a b d", a=2, b=2)
            S.add("dve", lambda e: e.tensor_tensor(out=lamt[:].rearrange("p (a d) -> p a d", a=2), in0=l4[:, :, 0, :], in1=l4[:, :, 1, :], op=ALU.mult),
                  reads=["lamb"], writes=["lamt"])
            S.add("dve", lambda e: e.tensor_reduce(out=lamc[:], in_=lamt[:].rearrange("p (a d) -> p a d", a=2), axis=AX.X, op=ALU.add),
                  reads=["lamt"], writes=["lamc"])
            S.add("act", lambda e: e.activation(out=lamc[:], in_=lamc[:], func=AF.Exp), reads=["lamc"], writes=["lamc"])
            S.add("dve", lambda e: e.tensor_tensor(out=lamc[:, 0:1], in0=lamc[:, 1:2], in1=lamc[:, 0:1], op=ALU.subtract), reads=["lamc"], writes=["lamc"])
            S.add("dve", lambda e: e.tensor_scalar_add(lamc[:, 0:1], lamc[:, 0:1], float(-lam_init)), reads=["lamc"], writes=["lamc"])
        st = {"P": 0, "O": 0, "y": 0}
        QBL = 256 if kind == 0 else 512
        qblocks = [(256 + j * QBL, QBL, [t for t in range(NT)]) for j in range(2048 // QBL)]
        if with_ctx:
            qblocks.append((0, 256, [0, 1]))
        for h in (range(nh) if heads is None else heads):
            i = h % 2
            for s in range(nset):
                if kind == 0:
                    qsrc = self.ap("dqT")[2 * h + s]
                    ksrc = self.ap("dkT")[2 * h + s]
                else:
                    qsrc = self.ap("aqTn")[h]
                    ksrc = self.ap("akT")[h]
                S.add("sp", lambda e, s=s, qsrc=qsrc: e.dma_start(out=qT[i][s][:], in_=qsrc), writes=[("qT", i, s)], dma=True)
                S.add("sp", lambda e, s=s, ksrc=ksrc: e.dma_start(out=kT[i][s][:], in_=ksrc), writes=[("kT", i, s)], dma=True)
            if kind == 1:
                S.add("sp", lambda e: e.dma_start(out=qTr[i][:], in_=self.ap("aqTr")[h]), writes=[("qTr", i)], dma=True)
            vsrc = self.ap("dv" if kind == 0 else "av")[:, h * dv:(h + 1) * dv].rearrange("(t p) d -> p t d", p=128)
            S.add("sp", lambda e, vsrc=vsrc: e.dma_start(out=va[i][:, :, 0:dv], in_=vsrc), writes=[("va", i)], dma=True)
            for (q0, QB, ktiles) in qblocks:
                nsub = QB // 128
                if kind == 0:
                    Oi = lambda s, sub: s * 2 + sub
                else:
                    Oi = lambda s, sub: sub
                pidx = []
                for n in range(len(ktiles)):
                    pidx.append((st["P"] % 3, st["P"] % 3))
                    st["P"] += 1

                def scores(n):
                    ps_i, pb = pidx[n]
                    kt = ktiles[n]
                    ks = slice(kt * 128, (kt + 1) * 128)
                    for s in range(nset):
                        if kind == 0:
                            S.add("pe", lambda e, s=s: e.matmul(psS[ps_i][:, s * QB:(s + 1) * QB], kT[i][s][:, ks], qT[i][s][:, q0:q0 + QB],
                                                                start=True, stop=True),
                                  reads=[("kT", i, s), ("qT", i, s)], writes=[("psS", ps_i, s)])
                        else:
                            S.add("pe", lambda e: e.matmul(psS[ps_i][:, 0:QB], kT[i][0][:, ks], qT[i][0][:, q0:q0 + QB], start=True, stop=False),
                                  reads=[("kT", i, 0), ("qT", i, 0)], writes=[("psS", ps_i, 0)])
                            S.add("pe", lambda e: e.matmul(psS[ps_i][:, 0:QB], kTr[:, ks], qTr[i][:, q0:q0 + QB], start=False, stop=True),
                                  reads=["kTr", ("qTr", i)], writes=[("psS", ps_i, 0)])

                scores(0)
                if len(ktiles) > 1:
                    scores(1)
                for n, kt in enumerate(ktiles):
                    ps_i, pb = pidx[n]
                    if n + 2 < len(ktiles):
                        scores(n + 2)
                    S.add("act", lambda e: e.activation(out=Pb[pb][:, 0:nset * QB], in_=psS[ps_i][:, 0:nset * QB], func=AF.Exp),
                          reads=[("psS", ps_i, s) for s in range(nset)], writes=[("Pb", pb)])
                    for s in range(nset):
                        for sub in range(nsub):
                            o = Oi(s, sub)
                            S.add("pe", lambda e, s=s, sub=sub, o=o: e.matmul(psO[o][:, 0:dv + 1], Pb[pb][:, s * QB + sub * 128:s * QB + (sub + 1) * 128],
                                                                              va[i][:, kt, :], start=(n == 0), stop=(n == len(ktiles) - 1)),
                                  reads=[("Pb", pb), ("va", i)], writes=[("psO", o)])
                for sub in range(nsub):
                    tq = q0 + sub * 128
                    y = st["y"] % 2
                    st["y"] += 1
                    if kind == 0:
                        o1, o2 = Oi(0, sub), Oi(1, sub)
                        S.add("dve", lambda e: e.reciprocal(out=rr[:, 0:1], in_=psO[o1][:, dv:dv + 1]), reads=[("psO", o1)], writes=["rr"])
                        S.add("dve", lambda e: e.reciprocal(out=rr[:, 1:2], in_=psO[o2][:, dv:dv + 1]), reads=[("psO", o2)], writes=["rr"])
                        S.add("dve", lambda e: e.tensor_tensor(out=rr[:, 1:2], in0=rr[:, 1:2], in1=lamc[:, 0:1], op=ALU.mult),
                              reads=["rr", "lamc"], writes=["rr"])
                        S.add("act", lambda e: e.activation(out=t1[:], in_=psO[o1][:, 0:dv], func=AF.Copy, scale=rr[:, 0:1]),
                              reads=[("psO", o1), "rr"], writes=["t1"])
                        S.add("dve", lambda e: e.scalar_tensor_tensor(out=ob[:], in0=psO[o2][:, 0:dv], scalar=rr[:, 1:2], in1=t1[:],
                                                                      op0=ALU.mult, op1=ALU.add),
                              reads=[("psO", o2), "rr", "t1"], writes=["ob"])
                        S.add("act", lambda e: e.activation(out=junk[:], in_=ob[:], func=AF.Square, accum_out=ssn[:, 0:1]),
                              reads=["ob", "ssn"], writes=["junk", "ssn"])
                        self.rstd(S, ssn[:, 0:1], "ssn", 256)
                        S.add("dve", lambda e: e.scalar_tensor_tensor(out=obb[y][:], in0=ob[:], scalar=ssn[:, 0:1], in1=gsub[:],
                                                                      op0=ALU.mult, op1=ALU.mult),
                              reads=["ob", "ssn", "gsub"], writes=[("obb", y)])
                        S.add("pool", lambda e: e.memset(ssn[:], 0.0), reads=["ssn"], writes=["ssn"])
                        nch = 2
                    else:
                        o1 = Oi(0, sub)
                        S.add("dve", lambda e: e.reciprocal(out=rr[:, 0:1], in_=psO[o1][:, dv:dv + 1]), reads=[("psO", o1)], writes=["rr"])
                        S.add("act", lambda e: e.activation(out=obb[y][:, 0:128], in_=psO[o1][:, 0:dv], func=AF.Copy, scale=rr[:, 0:1]),
                              reads=[("psO", o1), "rr"], writes=[("obb", y)])
                        nch = 1
                    for c in range(nch):
                        S.add("pe", lambda e, c=c: e.transpose(out=ptr[:, c, :], in_=obb[y][:, c * 128:(c + 1) * 128], identity=self.ident_b[:]),
                              reads=[("obb", y)], writes=["ptr"])
                    S.add("dve", lambda e: e.tensor_copy(out=yst[y][:, 0:nch, :], in_=ptr[:, 0:nch, :]), reads=["ptr"], writes=[("yst", y)])
                    br = 1 if kind == 0 else 2
                    S.add("sp", lambda e: e.dma_start(out=self.ap("ysT")[br, h * nch:(h + 1) * nch, :, tq:tq + 128].rearrange("c p n -> p c n"),
                                                      in_=yst[y][:, 0:nch, :]), reads=[("yst", y)], dma=True)
        S.emit()
        S.close()

    def phase_mlstm(self, l, heads=None):
        nc = self.nc
        NEG = {}
        pos_of = {0: lambda t: t, 1: lambda t: (1 - t) if t < 2 else 2 + (NT - 1 - t)}
        tile_at = {0: lambda p: p, 1: lambda p: (1 - p) if p < 2 else NT - 1 - (p - 2)}
        g2 = contextlib.ExitStack()
        UT = [g2.enter_context(nc.sbuf_tensor("UT%d_%d" % (l, d), [128, NT, 12], F32)) for d in range(2)]
        KB = [g2.enter_context(nc.sbuf_tensor("KB%d_%d" % (l, d), [128, 4, NT], F32)) for d in range(2)]
        S = Sched(nc)
        cj = S.sb("cj", [128, 128], F32)
        sel = S.sb("sel", [4, 512], F32)
        G = S.sb("G", [128, NT, 16], F32)
        IG = S.sb("IG", [128, NT, 8], F32)
        LF = S.sb("LF", [128, NT, 8], F32)
        ones = S.sb("ones", [128, 1], F32)
        zr = S.sb("zr", [4, NTOK], F32)
        S.add("sp", lambda e: e.dma_start(out=cj[:], in_=self.ap("consts")[:, 128:256]), writes=["cj"], dma=True)
        S.add("sp", lambda e: e.dma_start(out=sel[:], in_=self.ap("consts2")), writes=["sel"], dma=True)
        S.add("sp", lambda e: e.dma_start(out=G[:], in_=self.ap("mg").rearrange("(t p) c -> p t c", p=128)), writes=["G"], dma=True)
        S.add("pool", lambda e: e.memset(ones[:], 1.0), writes=["ones"])
        S.add("pool", lambda e: e.memset(zr[:], 0.0), writes=["zr"])
        G4 = G[:].rearrange("p t (d x c) -> p t d x c", d=2, x=2)
        IG3 = IG[:].rearrange("p t (d c) -> p t d c", d=2)
        LF3 = LF[:].rearrange("p t (d c) -> p t d c", d=2)
        for d in range(2):
            S.add("dve", lambda e, d=d: e.tensor_copy(out=IG3[:, :, d, :], in_=G4[:, :, d, 0, :]), reads=["G"], writes=["IG"])
            S.add("act", lambda e, d=d: e.activation(out=LF3[:, :, d, :], in_=G4[:, :, d, 1, :], func=AF.Exp, scale=-1.0), reads=["G"], writes=["LF"])
        S.add("act", lambda e: e.activation(out=LF[:], in_=LF[:], func=AF.Ln, bias=ones[:, 0:1]), reads=["LF", "ones"], writes=["LF"])
        S.add("dve", lambda e: e.tensor_scalar_mul(LF[:], LF[:], -1.0), reads=["LF"], writes=["LF"])
        prow = [S.ps("prow%d" % i, [128, 512], F32) for i in range(2)]
        rows = {}
        for d in range(2):
            for nm in ("I", "L", "F", "A", "M", "T", "U", "U2", "E"):
                rows[(nm, d)] = S.sb("row%s%d" % (nm, d), [4, NTOK], F32)
        R = [S.sb("R%d" % d, [4, NT + 1], F32) for d in range(2)]
        KP = [S.sb("KP%d" % d, [4, NT], F32) for d in range(2)]
        cnt = 0
        for d in range(2):
            rm = self.ident_f if d == 0 else cj
            for src, nm in ((IG, "I"), (LF, "L")):
                for p0 in range(0, NT, 4):
                    pr = prow[cnt % 2]
                    cnt += 1
                    n = min(4, NT - p0)
                    for pp in range(n):
                        t = tile_at[d](p0 + pp)
                        S.add("pe", lambda e, pr=pr, pp=pp, t=t, src=src, rm=rm, d=d: e.matmul(
                            pr[0:4, pp * 128:(pp + 1) * 128], src[:, t, 4 * d:4 * d + 4], rm[:], start=True, stop=True),
                            reads=["IG", "LF", "cj"], writes=[("prow", id(pr))])
                    S.add("dve", lambda e, pr=pr, p0=p0, n=n, nm=nm, d=d: e.tensor_copy(out=rows[(nm, d)][:, p0 * 128:(p0 + n) * 128], in_=pr[0:4, 0:n * 128]),
                          reads=[("prow", id(pr))], writes=[("row", nm, d)])
            r = lambda nm: rows[(nm, d)]
            k = lambda nm: ("row", nm, d)
            v3 = lambda nm: rows[(nm, d)][:].rearrange("c (t n) -> c t n", n=128)
            S.add("dve", lambda e, d=d: e.tensor_tensor_scan(out=r("F")[:], data0=r("L")[:], data1=zr[:], initial=0.0, op0=ALU.add, op1=ALU.add),
                  reads=[k("L"), "zr"], writes=[k("F")])
            S.add("dve", lambda e, d=d: e.tensor_tensor(out=r("A")[:], in0=r("I")[:], in1=r("F")[:], op=ALU.subtract), reads=[k("I"), k("F")], writes=[k("A")])
            S.add("dve", lambda e, d=d: e.tensor_tensor_scan(out=r("M")[:], data0=r("A")[:], data1=r("A")[:], initial=0.0, op0=ALU.max, op1=ALU.max),
                  reads=[k("A")], writes=[k("M")])
            S.add("dve", lambda e, d=d: e.memset(R[d][:, 0:1], 0.0), writes=[("R", d)])
            S.add("dve", lambda e, d=d: e.tensor_copy(out=R[d][:, 1:NT + 1], in_=v3("M")[:, :, 127]), reads=[k("M")], writes=[("R", d)])
            Rc = R[d][:, 0:NT].unsqueeze(2).to_broadcast([4, NT, 128])
            Rn = R[d][:, 1:NT + 1].unsqueeze(2).to_broadcast([4, NT, 128])
            S.add("dve", lambda e, d=d: e.tensor_tensor(out=v3("T"), in0=v3("A"), in1=Rc, op=ALU.subtract), reads=[k("A"), ("R", d)], writes=[k("T")])
            S.add("act", lambda e, d=d: e.activation(out=r("U")[:], in_=r("T")[:], func=AF.Exp), reads=[k("T")], writes=[k("U")])
            S.add("dve", lambda e, d=d: e.tensor_tensor(out=v3("T"), in0=v3("A"), in1=Rn, op=ALU.subtract), reads=[k("A"), ("R", d), k("U")], writes=[k("T")])
            S.add("act", lambda e, d=d: e.activation(out=r("U2")[:], in_=r("T")[:], func=AF.Exp), reads=[k("T")], writes=[k("U2")])
            S.add("dve", lambda e, d=d: e.tensor_tensor(out=v3("T"), in0=v3("F"), in1=Rc, op=ALU.add), reads=[k("F"), ("R", d), k("U2")], writes=[k("T")])
            S.add("act", lambda e, d=d: e.activation(out=r("E")[:], in_=r("T")[:], func=AF.Exp, scale=-1.0), reads=[k("T")], writes=[k("E")])
            S.add("dve", lambda e, d=d: e.tensor_tensor(out=KP[d][:], in0=R[d][:, 0:NT], in1=R[d][:, 1:NT + 1], op=ALU.subtract), reads=[("R", d)], writes=[("KP", d)])
            S.add("act", lambda e, d=d: e.activation(out=KP[d][:], in_=KP[d][:], func=AF.Exp), reads=[("KP", d)], writes=[("KP", d)])
            pk = prow[cnt % 2]
            cnt += 1
            for c in range(4):
                S.add("pe", lambda e, c=c, pk=pk, d=d: e.matmul(pk[:, c * NT:(c + 1) * NT], sel[:, c * 128:(c + 1) * 128], KP[d][:], start=True, stop=True),
                      reads=["sel", ("KP", d)], writes=[("prow", id(pk))])
            S.add("dve", lambda e, pk=pk, d=d: e.tensor_copy(out=KB[d][:].rearrange("p c t -> p (c t)"), in_=pk[:, 0:4 * NT]),
                  reads=[("prow", id(pk))], writes=[("KB", d)])
            xs = [S.sb("xs%d_%d" % (d, i), [128, 12], F32) for i in range(2)]
            for p in range(NT):
                t = tile_at[d](p)
                pr = prow[cnt % 2]
                cnt += 1
                for qi, nm in enumerate(("U", "U2", "E")):
                    S.add("pe", lambda e, pr=pr, qi=qi, nm=nm, p=p, d=d: e.matmul(pr[:, qi * 4:(qi + 1) * 4], rows[(nm, d)][:, p * 128:(p + 1) * 128],
                                                                               self.ident_f[0:4, 0:4], start=True, stop=True),
                          reads=[("row", nm, d)], writes=[("prow", id(pr))])
                if d == 0:
                    S.add("dve", lambda e, pr=pr, t=t: e.tensor_copy(out=UT[0][:, t, :], in_=pr[:, 0:12]), reads=[("prow", id(pr))], writes=[("UT", 0)])
                else:
                    x = xs[p % 2]
                    S.add("dve", lambda e, pr=pr, x=x: e.tensor_copy(out=x[:], in_=pr[:, 0:12]), reads=[("prow", id(pr))], writes=[("xs", p % 2)])
                    pr2 = prow[cnt % 2]
                    cnt += 1
                    S.add("pe", lambda e, pr2=pr2, x=x: e.matmul(pr2[:, 0:12], cj[:], x[:], start=True, stop=True),
                          reads=[("xs", p % 2), "cj"], writes=[("prow", id(pr2))])
                    S.add("dve", lambda e, pr2=pr2, t=t: e.tensor_copy(out=UT[1][:, t, :], in_=pr2[:, 0:12]), reads=[("prow", id(pr2))], writes=[("UT", 1)])
        S.emit()
        S.close()

        S = Sched(nc)
        mask = [S.sb("mask%d" % d, [128, 128], F32) for d in range(2)]
        S.add("sp", lambda e: e.dma_start(out=mask[0][:], in_=self.ap("consts")[:, 256:384]), writes=["mask"], dma=True)
        S.add("sp", lambda e: e.dma_start(out=mask[1][:], in_=self.ap("consts")[:, 384:512]), writes=["mask"], dma=True)
        qT = [S.sb("mq%d" % i, [128, 2, NTOK], BF16) for i in range(2)]
        kT = [S.sb("mk%d" % i, [128, 2, NTOK], BF16) for i in range(2)]
        ktm = [S.sb("mkt%d" % i, [128, NT, 256], BF16) for i in range(2)]
        va = [S.sb("mva%d" % i, [128, NT, 257], BF16) for i in range(2)]
        for i in range(2):
            S.add("pool", lambda e, i=i: e.memset(va[i][:, :, 256:257], 1.0), writes=[("va", i)])
        psKQ = [S.ps("psKQ%d" % d, [128, 512], F32) for d in range(2)]
        psND = [S.ps("psND%d" % d, [128, 512], F32) for d in range(2)]
        psD = [[S.ps("psD%d_%d" % (d, c), [128, 512], F32) for c in range(2)] for d in range(2)]
        Wm = [[S.sb("Wm%d_%d" % (d, i), [128, 128], BF16) for i in range(2)] for d in range(2)]
        gv = [[S.sb("gv%d_%d" % (d, i), [128, 257], BF16) for i in range(2)] for d in range(2)]
        gv2 = [[S.sb("gw%d_%d" % (d, i), [128, 257], BF16) for i in range(2)] for d in range(2)]
        S32 = [[S.sb("S32_%d_%d" % (d, c), [128, 257], F32) for c in range(2)] for d in range(2)]
        Sbf = [[S.sb("Sbf_%d_%d" % (d, c), [128, 257], BF16) for c in range(2)] for d in range(2)]
        dn = [S.sb("dn%d" % d, [128, 1], F32) for d in range(2)]
        ho = [[S.sb("ho%d_%d" % (d, i), [128, 256], F32) for i in range(2)] for d in range(2)]
        hdst = ["hmf", "hmb"]
        for h in (range(4) if heads is None else heads):
            i = h % 2
            S.add("sp", lambda e: e.dma_start(out=qT[i][:], in_=self.ap("mqkT")[2 * h:2 * h + 2].rearrange("c p n -> p c n")), writes=[("qT", i)], dma=True)
            S.add("sp", lambda e: e.dma_start(out=kT[i][:], in_=self.ap("mqkT")[8 + 2 * h:10 + 2 * h].rearrange("c p n -> p c n")), writes=[("kT", i)], dma=True)
            S.add("sp", lambda e: e.dma_start(out=ktm[i][:], in_=self.ap("mk_tm")[:, h * 256:(h + 1) * 256].rearrange("(t p) c -> p t c", p=128)),
                  writes=[("ktm", i)], dma=True)
            S.add("sp", lambda e: e.dma_start(out=va[i][:, :, 0:256], in_=self.ap("mv")[:, h * 256:(h + 1) * 256].rearrange("(t p) c -> p t c", p=128)),
                  writes=[("va", i)], dma=True)
            for p in range(NT):
                for d in range(2):
                    t = tile_at[d](p)
                    tok = slice(t * 128, (t + 1) * 128)
                    j = p % 2
                    for c in range(2):
                        S.add("pe", lambda e, c=c: e.matmul(psKQ[d][:, 0:128], kT[i][:, c, tok], qT[i][:, c, tok], start=(c == 0), stop=(c == 1)),
                              reads=[("kT", i), ("qT", i)], writes=[("psKQ", d)])
                    S.add("dve", lambda e: e.tensor_tensor(out=Wm[d][j][:], in0=psKQ[d][:, 0:128], in1=mask[d][:], op=ALU.mult),
                          reads=[("psKQ", d), "mask"], writes=[("Wm", d, j)])
                    S.add("act", lambda e: e.activation(out=gv[d][j][:], in_=va[i][:, t, :], func=AF.Copy, scale=UT[d][:, t, h:h + 1]),
                          reads=[("va", i)], writes=[("gv", d, j)])
                    if p < NT - 1:
                        S.add("act", lambda e: e.activation(out=gv2[d][j][:], in_=va[i][:, t, :], func=AF.Copy, scale=UT[d][:, t, 4 + h:5 + h]),
                              reads=[("va", i)], writes=[("gv2", d, j)])
                    first = True
                    if p > 0:
                        for c in range(2):
                            S.add("pe", lambda e, c=c, first=first: e.matmul(psND[d][:, 0:257], qT[i][:, c, tok], Sbf[d][c][:], start=first, stop=False),
                                  reads=[("qT", i), ("Sbf", d, c)], writes=[("psND", d)])
                            first = False
                    S.add("pe", lambda e, first=first: e.matmul(psND[d][:, 0:257], Wm[d][j][:], gv[d][j][:], start=first, stop=True),
                          reads=[("Wm", d, j), ("gv", d, j)], writes=[("psND", d)])
                    S.add("act", lambda e: e.activation(out=dn[d][:], in_=psND[d][:, 256:257], func=AF.Abs), reads=[("psND", d)], writes=[("dn", d)])
                    S.add("dve", lambda e: e.tensor_tensor(out=dn[d][:], in0=dn[d][:], in1=UT[d][:, t, 8 + h:9 + h], op=ALU.max),
                          reads=[("dn", d)], writes=[("dn", d)])
                    S.add("dve", lambda e: e.reciprocal(out=dn[d][:], in_=dn[d][:]), reads=[("dn", d)], writes=[("dn", d)])
                    S.add("act", lambda e: e.activation(out=ho[d][j][:], in_=psND[d][:, 0:256], func=AF.Copy, scale=dn[d][:, 0:1]),
                          reads=[("psND", d), ("dn", d)], writes=[("ho", d, j)])
                    S.add("sp", lambda e: e.dma_start(out=self.ap(hdst[d])[tok, h * 256:(h + 1) * 256], in_=ho[d][j][:]), reads=[("ho", d, j)], dma=True)
                    if p < NT - 1:
                        for c in range(2):
                            S.add("pe", lambda e, c=c: e.matmul(psD[d][c][:, 0:257], ktm[i][:, t, c * 128:(c + 1) * 128], gv2[d][j][:], start=True, stop=True),
                                  reads=[("ktm", i), ("gv2", d, j)], writes=[("psD", d, c)])
                            if p == 0:
                                S.add("dve", lambda e, c=c: e.tensor_copy(out=S32[d][c][:], in_=psD[d][c][:, 0:257]),
                                      reads=[("psD", d, c)], writes=[("S32", d, c)])
                            else:
                                S.add("dve", lambda e, c=c: e.scalar_tensor_tensor(out=S32[d][c][:], in0=S32[d][c][:], scalar=KB[d][:, h, p:p + 1],
                                                                                   in1=psD[d][c][:, 0:257], op0=ALU.mult, op1=ALU.add),
                                      reads=[("psD", d, c), ("S32", d, c)], writes=[("S32", d, c)])
                            S.add("pool", lambda e, c=c: e.tensor_copy(out=Sbf[d][c][:], in_=S32[d][c][:]), reads=[("S32", d, c)], writes=[("Sbf", d, c)])
        S.emit()
        S.close()

        S = Sched(nc)
        gn = S.sb("gn", [128, 1024], F32)
        S.add("sp", lambda e: e.dma_start(out=gn[:], in_=self.bcast_rows("m_norm_g", l * 1024, 1024)), writes=["gn"], dma=True)
        hf = [S.sb("hf%d" % i, [128, 1024], F32) for i in range(2)]
        hb = [S.sb("hb%d" % i, [128, 1024], F32) for i in range(2)]
        ot = [S.sb("ot%d" % i, [128, 1024], BF16) for i in range(2)]
        sq = S.sb("sqm", [128, 1024], F32)
        s4 = S.sb("s4", [128, 4], F32)
        yb = [S.sb("ybm%d" % i, [128, 1024], BF16) for i in range(2)]
        ptm = S.ps("ptm3", [128, 8, 128], BF16)
        ys = [S.sb("ysm%d" % i, [128, 8, 128], BF16) for i in range(2)]
        pipe3 = Pipe(2)

        def make_item3(t):
            i = t % 2
            tok = slice(t * 128, (t + 1) * 128)
            h3 = hf[i][:].rearrange("p (h d) -> p h d", h=4)

            def s0():
                    S.add("sp", lambda e: e.dma_start(out=hf[i][:], in_=self.ap("hmf")[tok, :]), writes=[("hf", i)], dma=True)
                    S.add("sp", lambda e: e.dma_start(out=hb[i][:], in_=self.ap("hmb")[tok, :]), writes=[("hb", i)], dma=True)
                    S.add("sp", lambda e: e.dma_start(out=ot[i][:], in_=self.ap("mo")[tok, :]), writes=[("ot", i)], dma=True)
                    S.add("dve", lambda e: e.tensor_tensor(out=hf[i][:], in0=hf[i][:], in1=hb[i][:], op=ALU.add), reads=[("hf", i), ("hb", i)], writes=[("hf", i)])
                    S.add("pool", lambda e: e.tensor_tensor(out=sq[:], in0=hf[i][:], in1=hf[i][:], op=ALU.mult), reads=[("hf", i)], writes=["sq"])
                    S.add("dve", lambda e: e.tensor_reduce(out=s4[:], in_=sq[:].rearrange("p (h d) -> p h d", h=4), axis=AX.X, op=ALU.add), reads=["sq"], writes=["s4"])
                    self.rstd(S, s4[:], "s4", 256)
                    h3 = hf[i][:].rearrange("p (h d) -> p h d", h=4)
                    S.add("pool", lambda e: e.tensor_tensor(out=h3, in0=h3, in1=s4[:].unsqueeze(2).to_broadcast([128, 4, 256]), op=ALU.mult),
                          reads=[("hf", i), "s4"], writes=[("hf", i)])
                    S.add("dve", lambda e: e.tensor_tensor(out=hf[i][:], in0=hf[i][:], in1=gn[:], op=ALU.mult), reads=[("hf", i), "gn"], writes=[("hf", i)])
                    S.add("pool", lambda e: e.tensor_tensor(out=yb[i][:], in0=hf[i][:], in1=ot[i][:], op=ALU.mult), reads=[("hf", i), ("ot", i)], writes=[("yb", i)])

            def s1():
                    for c in range(8):
                        S.add("pe", lambda e, c=c: e.transpose(out=ptm[:, c, :], in_=yb[i][:, c * 128:(c + 1) * 128], identity=self.ident_b[:]),
                              reads=[("yb", i)], writes=["ptm"])
                    S.add("act", lambda e: e.copy(out=ys[i][:], in_=ptm[:]), reads=["ptm"], writes=[("ys", i)])
                    S.add("sp", lambda e: e.dma_start(out=self.ap("ysT")[0, :, :, tok].rearrange("c p n -> p c n"), in_=ys[i][:]), reads=[("ys", i)], dma=True)

            return [s0, s1]

        for t in range(NT):
            pipe3.push(make_item3(t))
        pipe3.drain()
        S.emit()
        S.close()
        g2.close()

    def phase_merge_a(self, l, tiles):
        nc = self.nc
        S = Sched(nc)
        ysall = S.sb("ysall", [128, 24, NTOK], BF16)
        for b in range(3):
            S.add("sp", lambda e, b=b: e.dma_start(out=ysall[:, b * 8:(b + 1) * 8, :], in_=self.ap("ysT")[b].rearrange("c p n -> p c n")),
                  writes=[("ys", b)], dma=True)
        wbr = [S.sb("wbr%d" % i, [128, 24, 128], BF16) for i in range(2)]
        gj = [S.sb("gj%d" % i, [128, 3, NTOK], BF16) for i in range(2)]
        mTs = [S.sb("mTs%d" % i, [128, NTOK], BF16) for i in range(2)]
        tmpm = [[S.sb("tmpm%d_%d" % (i, r_), [128, 512], F32) for i in range(3)] for r_ in range(2)]
        accm = [S.sb("accm%d" % r_, [128, 512], F32) for r_ in range(2)]
        py = [S.ps("py%d" % i, [128, 512], F32) for i in range(6)]
        tstart = tiles[0] * 128
        tend = (tiles[-1] + 1) * 128
        blocks = [(t0, min(512, tend - t0)) for t0 in range(tstart, tend, 512)]
        cnt = {"py": 0, "r": 0}
        def load_j(j):
            wi = j % 2
            S.add("pool", lambda e: e.dma_start(out=wbr[wi][:], in_=self.ap("w_branch")[l][:, :, j * 128:(j + 1) * 128].rearrange("b (c p) w -> p (b c) w", p=128)),
                  writes=[("wbr", wi)], dma=True)
            S.add("sp", lambda e: e.dma_start(out=gj[wi][:, :, tstart:tend], in_=self.ap("gT")[:, :, tstart:tend].rearrange("(b j) p n -> j p b n", b=3)[j]),
                  writes=[("gj", wi)], dma=True)

        load_j(0)
        for j in range(NKC):
            wi = j % 2
            if j + 1 < NKC:
                load_j(j + 1)
            for (t0, n) in blocks:
                rr_ = cnt["r"] % 2
                cnt["r"] += 1
                for b in range(3):
                    pi = cnt["py"] % 6
                    cnt["py"] += 1
                    for c in range(8):
                        S.add("pe", lambda e, c=c: e.matmul(py[pi][:, 0:n], wbr[wi][:, b * 8 + c, :], ysall[:, b * 8 + c, t0:t0 + n], start=(c == 0), stop=(c == 7)),
                              reads=[("wbr", wi), ("ys", b)], writes=[("py", pi)])
                    S.add("dve", lambda e: e.tensor_tensor(out=tmpm[rr_][b][:, 0:n], in0=py[pi][:, 0:n], in1=gj[wi][:, b, t0:t0 + n], op=ALU.mult),
                          reads=[("py", pi), ("gj", wi)], writes=[("tmpm", rr_, b)])
                S.add("pool", lambda e: e.tensor_tensor(out=accm[rr_][:, 0:n], in0=tmpm[rr_][0][:, 0:n], in1=tmpm[rr_][1][:, 0:n], op=ALU.add),
                      reads=[("tmpm", rr_, 0), ("tmpm", rr_, 1)], writes=[("accm", rr_)])
                S.add("pool", lambda e: e.tensor_tensor(out=mTs[wi][:, t0:t0 + n], in0=accm[rr_][:, 0:n], in1=tmpm[rr_][2][:, 0:n], op=ALU.add),
                      reads=[("accm", rr_), ("tmpm", rr_, 2)], writes=[("mTs", wi)])
            S.add("sp", lambda e: e.dma_start(out=self.ap("mTd")[j][:, tstart:tend], in_=mTs[wi][:, tstart:tend]), reads=[("mTs", wi)], dma=True)
        S.emit()
        S.close()

    def phase_merge_b(self, l, tiles):
        nc = self.nc
        S = Sched(nc)
        wout = S.sb("wout", [128, NKC, D], BF16)
        for h in range(2):
            S.add("pool", lambda e, h=h: e.dma_start(out=wout[:, h * 8:(h + 1) * 8, :],
                                                     in_=self.ap("w_out")[l][h * 1024:(h + 1) * 1024, :].rearrange("(k p) w -> p k w", p=128)),
                  writes=[("wout", h)], dma=True)
        mods = {}
        for r in (0, 1):
            for seg, add1 in ((2, False), (4, True), (3, False)):
                if r == 1 and tiles[0] >= NT_C:
                    continue
                tl = S.sb("modm_%d_%d" % (r, seg), [128, D], F32)
                S.add("sp", lambda e, tl=tl, r=r, seg=seg: e.dma_start(out=tl[:], in_=self.bcast_rows("modrow", modoff(l, r, seg), D)),
                      writes=[("mod", r, seg)], dma=True)
                if add1:
                    S.add("pool", lambda e, tl=tl: e.tensor_scalar_add(tl[:], tl[:], 1.0), reads=[("mod", r, seg)], writes=[("mod", r, seg)])
                mods[(r, seg)] = tl
        mT = [S.sb("mTb%d" % i, [128, NKC, 128], BF16) for i in range(2)]
        pyo = [S.ps("pyo%d" % i, [128, 512], F32) for i in range(4)]
        pth = [S.ps("pth%d" % i, [128, 8, 128], BF16) for i in range(2)]
        xts = [S.sb("xtm%d" % i, [128, D], F32) for i in range(2)]
        junk = S.sb("junkm", [128, D], BF16)
        xm32 = S.sb("xm32m", [128, D], F32)
        xmb = S.sb("xmbm", [128, D], BF16)
        ssb = S.sb("ssbm", [128, NT], F32)
        hst = [S.sb("hstm%d" % i, [128, NKC, 128], BF16) for i in range(2)]
        S.add("dve", lambda e: e.memset(ssb[:], 0.0), writes=[("ss", t) for t in range(NT)])
        cnt = {"po": 0}
        rw = S.sb("rw", [128, NKC, 16], BF16)
        rb = S.sb("rb", [128, 16], F32)
        S.add("pool", lambda e: e.dma_start(out=rw[:], in_=self.ap("router_w").rearrange("(k p) e -> p k e", p=128)), writes=["rw"], dma=True)
        S.add("sp", lambda e: e.dma_start(out=rb[:], in_=self.bcast_rows("router_bias", 0, 16)), writes=["rb"], dma=True)
        prt = S.ps("prt", [128, 512], F32)
        sc = S.sb("r_sc", [128, 16], F32)
        sel = S.sb("r_sel", [128, 16], F32)
        eq = S.sb("r_eq", [128, 16], F32)
        sm = S.sb("r_sm", [128, 16], F32)
        m1 = S.sb("r_m1", [128, 4], F32)
        m2 = S.sb("r_m2", [128, 4], F32)
        gm = S.sb("r_gm", [128, 2], F32)
        cmb = [S.sb("cmb%d" % i_, [128, 16], F32) for i_ in range(2)]
        V4 = lambda a_: a_[:].rearrange("p (g k) -> p g k", g=4)
        dv = lambda fn, rd, wr: S.add("dve", fn, reads=rd, writes=wr)
        xmbs = [xmb, S.sb("xmbm1", [128, D], BF16)]
        pipe = Pipe(2)

        def make_item(ti, t):
            i = ti % 2
            xt = xts[i]
            r = 1 if t < NT_C else 0
            tok = slice(t * 128, (t + 1) * 128)
            pos = []
            for dc in range(4):
                pos.append(cnt["po"] % 4)
                cnt["po"] += 1

            def s0():
                S.add("sp", lambda e: e.dma_start(out=mT[i][:], in_=self.ap("mTd")[:, :, tok].rearrange("j p n -> p j n")), writes=[("mT", i)], dma=True)
                S.add("sp", lambda e: e.dma_start(out=xt[:], in_=self.ap("xres")[tok, :]), writes=[("xt", i)], dma=True)
                for dc in range(4):
                    po = pos[dc]
                    for j in range(NKC):
                        S.add("pe", lambda e, j=j: e.matmul(pyo[po][:], mT[i][:, j, :], wout[:, j, dc * 512:(dc + 1) * 512],
                                                            start=(j == 0), stop=(j == NKC - 1)),
                              reads=[("mT", i), ("wout", j // 8)], writes=[("pyo", po)])
                    S.add("dve", lambda e: e.tensor_tensor(out=xm32[:, dc * 512:(dc + 1) * 512], in0=pyo[po][:], in1=mods[(r, 2)][:, dc * 512:(dc + 1) * 512], op=ALU.mult),
                          reads=[("pyo", po), ("mod", r, 2)], writes=["xm32"])
                S.add("pool", lambda e: e.tensor_tensor(out=xt[:], in0=xt[:], in1=xm32[:], op=ALU.add), reads=[("xt", i), "xm32"], writes=[("xt", i)])
                S.add("sp", lambda e: e.dma_start(out=self.ap("xres")[tok, :], in_=xt[:]), reads=[("xt", i)], dma=True)
                self.rms_modulate(S, xt, ("xt", i), ssb[:, t:t + 1], ("ss", t), junk, mods[(r, 4)], ("mod", r, 4), mods[(r, 3)], ("mod", r, 3),
                                  xm32, xmbs[i], ("xmb", i))

            def s1():
                for j in range(NKC):
                    S.add("pe", lambda e, j=j: e.transpose(out=pth[j // 8][:, j % 8, :], in_=xmbs[i][:, j * 128:(j + 1) * 128], identity=self.ident_b[:]),
                          reads=[("xmb", i)], writes=[("pth", j // 8)])
                S.add("act", lambda e: e.copy(out=hst[i][:, 0:8, :], in_=pth[0][:]), reads=[("pth", 0)], writes=[("hst", i, 0)])
                S.add("dve", lambda e: e.tensor_copy(out=hst[i][:, 8:16, :], in_=pth[1][:]), reads=[("pth", 1)], writes=[("hst", i, 1)])
                S.add("sp", lambda e: e.dma_start(out=self.ap("h2Td")[:, :, tok], in_=hst[i][:]), reads=[("hst", i, 0), ("hst", i, 1)], dma=True)
                for k in range(NKC):
                    S.add("pe", lambda e, k=k: e.matmul(prt[:, 0:16], hst[i][:, k, :], rw[:, k, :], start=(k == 0), stop=(k == NKC - 1)),
                          reads=[("hst", i, 0), ("hst", i, 1), "rw"], writes=["prt"])
                S.add("act", lambda e: e.activation(out=sc[:], in_=prt[:, 0:16], func=AF.Sigmoid), reads=["prt"], writes=["sc"])
                dv(lambda e: e.tensor_tensor(out=sel[:], in0=sc[:], in1=rb[:], op=ALU.add), ["sc", "rb"], ["sel"])
                dv(lambda e: e.tensor_reduce(out=m1[:], in_=V4(sel), axis=AX.X, op=ALU.max), ["sel"], ["m1"])
                dv(lambda e: e.tensor_tensor(out=V4(eq), in0=V4(sel), in1=m1[:].unsqueeze(2).to_broadcast([128, 4, 4]), op=ALU.is_equal), ["sel", "m1"], ["eq"])
                dv(lambda e: e.scalar_tensor_tensor(out=sm[:], in0=eq[:], scalar=-1e30, in1=sel[:], op0=ALU.mult, op1=ALU.add), ["eq", "sel"], ["sm"])
                dv(lambda e: e.tensor_reduce(out=m2[:], in_=V4(sm), axis=AX.X, op=ALU.max), ["sm"], ["m2"])
                dv(lambda e: e.tensor_tensor(out=m1[:], in0=m1[:], in1=m2[:], op=ALU.add), ["m1", "m2"], ["m1"])
                dv(lambda e: e.tensor_reduce(out=gm[:, 0:1], in_=m1[:], axis=AX.X, op=ALU.max), ["m1"], ["gm"])
                dv(lambda e: e.tensor_tensor(out=m2[:], in0=m1[:], in1=gm[:, 0:1].to_broadcast([128, 4]), op=ALU.is_equal), ["m1", "gm"], ["m2"])
                dv(lambda e: e.tensor_scalar(out=m2[:], in0=m2[:], scalar1=-1.0, scalar2=1e30, op0=ALU.add, op1=ALU.mult), ["m2"], ["m2"])
                dv(lambda e: e.tensor_tensor(out=V4(sm), in0=V4(sel), in1=m2[:].unsqueeze(2).to_broadcast([128, 4, 4]), op=ALU.add), ["sel", "m2"], ["sm"])
                dv(lambda e: e.tensor_reduce(out=gm[:, 0:1], in_=sm[:], axis=AX.X, op=ALU.max), ["sm"], ["gm"])
                dv(lambda e: e.tensor_tensor(out=eq[:], in0=sm[:], in1=gm[:, 0:1].to_broadcast([128, 16]), op=ALU.is_equal), ["sm", "gm"], ["eq"])
                dv(lambda e: e.scalar_tensor_tensor(out=sm[:], in0=eq[:], scalar=-1e30, in1=sm[:], op0=ALU.mult, op1=ALU.add), ["eq", "sm"], ["sm"])
                dv(lambda e: e.tensor_reduce(out=gm[:, 1:2], in_=sm[:], axis=AX.X, op=ALU.max), ["sm"], ["gm"])
                dv(lambda e: e.tensor_tensor(out=sel[:], in0=sm[:], in1=gm[:, 1:2].to_broadcast([128, 16]), op=ALU.is_equal), ["sm", "gm"], ["sel"])
                dv(lambda e: e.tensor_tensor(out=eq[:], in0=eq[:], in1=sel[:], op=ALU.add), ["eq", "sel"], ["eq"])
                dv(lambda e: e.tensor_tensor(out=sc[:], in0=sc[:], in1=eq[:], op=ALU.mult), ["sc", "eq"], ["sc"])
                dv(lambda e: e.tensor_reduce(out=gm[:, 0:1], in_=sc[:], axis=AX.X, op=ALU.add), ["sc"], ["gm"])
                dv(lambda e: e.reciprocal(out=gm[:, 0:1], in_=gm[:, 0:1]), ["gm"], ["gm"])
                dv(lambda e: e.tensor_scalar(out=cmb[i][:], in0=sc[:], scalar1=gm[:, 0:1], scalar2=None, op0=ALU.mult), ["sc", "gm"], [("cmb", i)])
                S.add("sp", lambda e: e.dma_start(out=self.ap("combd")[tok, :], in_=cmb[i][:]), reads=[("cmb", i)], dma=True)

            return [s0, s1]

        for ti, t in enumerate(tiles):
            pipe.push(make_item(ti, t))
        pipe.drain()
        S.emit()
        S.close()

    def phase_moe(self, l, tiles, out_name=None):
        nc = self.nc
        GSZ = 6
        ngr = (len(tiles) + GSZ - 1) // GSZ
        base, rem = divmod(len(tiles), ngr)
        groups = []
        pos_ = 0
        for gi_ in range(ngr):
            sz = base + (1 if gi_ < rem else 0)
            groups.append(tiles[pos_:pos_ + sz])
            pos_ += sz
        S = Sched(nc)
        g2b = {}
        for r in (0, 1):
            if r == 1 and tiles[0] >= NT_C:
                continue
            g2b[r] = S.sb("g2b%d" % r, [128, D], F32)
            S.add("sp", lambda e, r=r: e.dma_start(out=g2b[r][:], in_=self.bcast_rows("modrow", modoff(l, r, 5), D)), writes=[("g2b", r)], dma=True)
        h2T = S.sb("h2T", [128, NKC, GSZ * 128], BF16)
        acc = [S.sb("acc%d" % i, [128, D], F32) for i in range(GSZ)]
        comb = S.sb("comb", [128, GSZ, 16], F32)
        w1 = [S.sb("w1_%d" % i, [128, NKC, 512], BF16) for i in range(2)]
        w3 = [S.sb("w3_%d" % i, [128, NKC, 512], BF16) for i in range(2)]
        w2 = [S.sb("w2_%d" % i, [128, 4, D], BF16) for i in range(2)]
        psA = [S.ps("psA%d" % i, [128, 512], F32) for i in range(2)]
        psG = [S.ps("psG%d" % i, [128, 512], F32) for i in range(2)]
        ptr = [S.ps("ptrm%d" % i, [128, 4, 128], BF16) for i in range(2)]
        psO = [S.ps("psOm%d" % i, [128, 512], F32) for i in range(2)]
        sa = [S.sb("sa%d" % i, [128, 512], F32) for i in range(2)]
        hid = [S.sb("hid%d" % i, [128, 512], BF16) for i in range(2)]
        hidT = [S.sb("hidT%d" % i, [128, 4, 128], BF16) for i in range(2)]
        sc = S.sb("r_sc", [128, 16], F32)
        sel = S.sb("r_sel", [128, 16], F32)
        eq = S.sb("r_eq", [128, 16], F32)
        sm = S.sb("r_sm", [128, 16], F32)
        m1 = S.sb("r_m1", [128, 4], F32)
        m2 = S.sb("r_m2", [128, 4], F32)
        gm = S.sb("r_gm", [128, 2], F32)
        xtz = S.sb("xtz", [128, D], F32)
        xts2 = [xtz, xtz]
        cnt = {"w": 0, "a": 0, "h": 0, "o": 0}
        V4 = lambda a: a[:].rearrange("p (g k) -> p g k", g=4)
        dv = lambda fn, rd, wr: S.add("dve", fn, reads=rd, writes=wr)
        for grp in groups:
            ng = len(grp)
            t0 = grp[0] * 128
            S.add("sp", lambda e: e.dma_start(out=h2T[:, :, 0:ng * 128], in_=self.ap("h2Td")[:, :, t0:t0 + ng * 128]), writes=["h2T"], dma=True)
            S.add("sp", lambda e: e.dma_start(out=comb[:, 0:ng, :], in_=self.ap("combd")[t0:t0 + ng * 128, :].rearrange("(g p) e -> p g e", p=128)),
                  writes=[("comb", gi) for gi in range(GSZ)], dma=True)
            for ex in range(N_EXP):
                wi = cnt["w"] % 2
                cnt["w"] += 1
                S.add("pool", lambda e: e.dma_start(out=w1[wi][:], in_=self.ap("moe_w1")[l, ex].rearrange("(k p) f -> p k f", p=128)), writes=[("w1", wi)], dma=True)
                S.add("pool", lambda e: e.dma_start(out=w3[wi][:], in_=self.ap("moe_w3")[l, ex].rearrange("(k p) f -> p k f", p=128)), writes=[("w3", wi)], dma=True)
                S.add("pool", lambda e: e.dma_start(out=w2[wi][:], in_=self.ap("moe_w2")[l, ex].rearrange("(k p) f -> p k f", p=128)), writes=[("w2", wi)], dma=True)
                ais = []
                for gi in range(ng):
                    ais.append(cnt["a"] % 2)
                    cnt["a"] += 1

                def up(gi):
                    tk = slice(gi * 128, (gi + 1) * 128)
                    ai = ais[gi]
                    for k in range(NKC):
                        S.add("pe", lambda e, k=k: e.matmul(psA[ai][:], h2T[:, k, tk], w1[wi][:, k, :], start=(k == 0), stop=(k == NKC - 1)),
                              reads=["h2T", ("w1", wi)], writes=[("psA", ai)])
                    for k in range(NKC):
                        S.add("pe", lambda e, k=k: e.matmul(psG[ai][:], h2T[:, k, tk], w3[wi][:, k, :], start=(k == 0), stop=(k == NKC - 1)),
                              reads=["h2T", ("w3", wi)], writes=[("psG", ai)])
                    S.add("act", lambda e: e.activation(out=sa[ai][:], in_=psA[ai][:], func=AF.Silu), reads=[("psA", ai)], writes=[("sa", ai)])
                    S.add("dve", lambda e: e.scalar_tensor_tensor(out=hid[ai][:], in0=psG[ai][:], scalar=comb[:, gi, ex:ex + 1], in1=sa[ai][:],
                                                                  op0=ALU.mult, op1=ALU.mult),
                          reads=[("psG", ai), ("comb", gi), ("sa", ai)], writes=[("hid", ai)])

                def down(gi):
                    ai = ais[gi]
                    for fc in range(4):
                        S.add("pe", lambda e, fc=fc: e.transpose(out=ptr[ai][:, fc, :], in_=hid[ai][:, fc * 128:(fc + 1) * 128], identity=self.ident_b[:]),
                              reads=[("hid", ai)], writes=[("ptr", ai)])
                    S.add("act", lambda e: e.copy(out=hidT[ai][:], in_=ptr[ai][:]), reads=[("ptr", ai)], writes=[("hidT", ai)])
                    for dc in range(4):
                        oi = cnt["o"] % 2
                        cnt["o"] += 1
                        for fc in range(4):
                            S.add("pe", lambda e, fc=fc: e.matmul(psO[oi][:], hidT[ai][:, fc, :], w2[wi][:, fc, dc * 512:(dc + 1) * 512],
                                                                  start=(fc == 0), stop=(fc == 3)),
                                  reads=[("hidT", ai), ("w2", wi)], writes=[("psO", oi)])
                        if ex == 0:
                            S.add("dve", lambda e: e.tensor_copy(out=acc[gi][:, dc * 512:(dc + 1) * 512], in_=psO[oi][:]),
                                  reads=[("psO", oi)], writes=[("acc", gi, dc)])
                        else:
                            S.add("dve", lambda e: e.tensor_tensor(out=acc[gi][:, dc * 512:(dc + 1) * 512], in0=acc[gi][:, dc * 512:(dc + 1) * 512],
                                                                   in1=psO[oi][:], op=ALU.add),
                                  reads=[("psO", oi), ("acc", gi, dc)], writes=[("acc", gi, dc)])

                up(0)
                for gi in range(ng):
                    if gi + 1 < ng:
                        up(gi + 1)
                    down(gi)
            for gi, t in enumerate(grp):
                r = 1 if t < NT_C else 0
                tok = slice(t * 128, (t + 1) * 128)
                xi = 0
                xt = xts2[xi]
                S.add("sp", lambda e: e.dma_start(out=xt[:], in_=self.ap("xres")[tok, :]), writes=[("xt", xi)], dma=True)
                S.add("dve", lambda e: e.tensor_tensor(out=acc[gi][:], in0=acc[gi][:], in1=g2b[r][:], op=ALU.mult),
                      reads=[("acc", gi, dc) for dc in range(4)] + [("g2b", r)], writes=[("acc", gi, dc) for dc in range(4)])
                S.add("pool", lambda e: e.tensor_tensor(out=xt[:], in0=xt[:], in1=acc[gi][:], op=ALU.add),
                      reads=[("xt", xi)] + [("acc", gi, dc) for dc in range(4)], writes=[("xt", xi)])
                if out_name is not None and t >= NT_C:
                    S.add("sp", lambda e: e.dma_start(out=self.T[out_name].ap()[(t - NT_C) * 128:(t - NT_C + 1) * 128, :], in_=xt[:]), reads=[("xt", xi)], dma=True)
                else:
                    S.add("sp", lambda e: e.dma_start(out=self.ap("xres")[tok, :], in_=xt[:]), reads=[("xt", xi)], dma=True)
        S.emit()
        S.close()


def build(layers=(0, 1), dbg=(), phases=None, groups=None, aheads=None, final_out=False):
    nc = bass.Bass("TRN2", target_bir_lowering=False)
    Sched.GLOBAL = None
    mk = MK(nc, dbg)
    if final_out:
        mk.T["out"] = nc.dram_tensor("out", [NT_L * 128, D], F32, kind="ExternalOutput")
    mk.load_consts()
    ph = lambda p: phases is None or p in phases
    for l in layers:
        if ph("ada"):
            mk.phase_ada(l)
    for l in layers:
        if ph("mod1"):
            mk.phase_mod1(l)
        if ph("inproj"):
            mk.phase_inproj(l, groups)
        if ph("mlstm"):
            mk.phase_mlstm(l, heads=aheads)
        if ph("mla_up"):
            mk.phase_mla_up(l)
        if ph("da"):
            mk.phase_attn(l, 0, l < DEPTH - 1, heads=aheads)
        if ph("mla"):
            mk.phase_attn(l, 1, l < DEPTH - 1, heads=aheads)
        tiles = list(range(NT)) if l < DEPTH - 1 else list(range(NT_C, NT))
        if ph("merge"):
            mk.phase_merge_a(l, tiles)
            mk.phase_merge_b(l, tiles)
        if ph("moe"):
            mk.phase_moe(l, tiles, out_name=("out" if (l == DEPTH - 1 and final_out) else None))
    mk.g.close()
    if Sched.GLOBAL is not None:
        Sched.GLOBAL["stack"].close()
    return nc, mk


def _axial(n, rot):
    rows = n // 64
    r = np.repeat(np.arange(rows, dtype=np.float32), 64)
    col = np.tile(np.arange(64, dtype=np.float32), rows)
    nf = rot // 4
    inv = (10000.0 ** (-np.arange(nf, dtype=np.float32) / nf)).astype(np.float32)
    return np.concatenate([r[:, None] * inv, col[:, None] * inv], -1)


def _rope_tab(rot):
    a = _axial(NT_L * 128, rot)
    t = np.zeros((NTOK, rot), np.float32)
    t[:NT_C * 128, :rot // 2] = 1.0
    t[NT_C * 128:, :rot // 2] = np.cos(a)
    t[NT_C * 128:, rot // 2:] = np.sin(a)
    return t


def _consts():
    c = np.zeros((128, 512), np.float32)
    c[:, 0:128] = np.eye(128)
    c[:, 128:256] = np.eye(128)[::-1]
    c[:, 256:384] = np.triu(np.ones((128, 128)))
    c[:, 384:512] = np.tril(np.ones((128, 128)))
    c2 = np.zeros((4, 512), np.float32)
    for k in range(4):
        c2[k, k * 128:(k + 1) * 128] = 1.0
    return c, c2


N_CORES = 4
_CACHE = {}


def kernel(**inputs):
    if "nc" not in _CACHE:
        _CACHE["nc"] = build(final_out=True)
    nc, mk = _CACHE["nc"]
    names = [n for n in mk.T if n in INPUT_SHAPES]
    c1, c2 = _consts()
    rd = _rope_tab(128)
    rope_da2 = np.ascontiguousarray(np.concatenate([rd[:, :64], rd[:, :64], -rd[:, 64:], rd[:, 64:]], 1))
    shared = {"consts": c1, "consts2": c2, "rope_da2": rope_da2, "rope_mla": _rope_tab(64)}
    B = inputs["x"].shape[0]
    in_maps = []
    for core in range(N_CORES):
        b = core % B
        d = {}
        for n in names:
            if n == "xs":
                d[n] = np.ascontiguousarray(np.concatenate([inputs["ctx"][b], inputs["x"][b]], 0), dtype=np.float32)
            elif n == "cvec":
                d[n] = np.ascontiguousarray(np.stack([inputs["c"][b], inputs["c_ctx"]], 0), dtype=np.float32)
            elif n in shared:
                d[n] = shared[n]
            else:
                d[n] = np.ascontiguousarray(inputs[n], dtype=np.float32)
        in_maps.append(d)
    res = run_bass_kernel_spmd(nc, in_maps, core_ids=list(range(N_CORES)))
    out = np.stack([np.asarray(res.results[b]["out"], dtype=np.float32) for b in range(B)], 0)
    return out
```
